# Optimizing a Trainium2 kernel written in Bass

```python
import math
import jax
import jax.numpy as jnp
from jax import lax
import numpy as np

D_MODEL = 1024
BATCH = 16
SEQ = 4096
DEPTH = 1

CHUNK = 64
GLA_HEADS = 4
GLA_DK = 64
GLA_DV = 128
GLA_GATE_RANK = 16
GLA_TAU = 16.0
ATT_HEADS = 8
ATT_DH = 64
IDX_HEADS = 8
IDX_DH = 32
TOPK_MAX = 256
Q_BLOCK = CHUNK
REL_BUCKETS = 32
REL_MAX_DIST = 128
N_GROUPS = 4
EXPERTS_PER_GROUP = 8
N_EXPERTS = N_GROUPS * EXPERTS_PER_GROUP
EXPERT_TOPK = 2
D_EXPERT = 128
EPS = 1e-6

GLA_QK_W = GLA_HEADS * GLA_DK
GLA_V_W = GLA_HEADS * GLA_DV
ATT_W = ATT_HEADS * ATT_DH
IDX_Q_W = IDX_HEADS * IDX_DH
SPLITS = (GLA_QK_W, GLA_QK_W, GLA_V_W, GLA_V_W, GLA_GATE_RANK, ATT_W, ATT_W, ATT_W, IDX_Q_W, IDX_DH, IDX_HEADS, D_MODEL, D_MODEL)
D_IN = 2 * GLA_QK_W + 2 * GLA_V_W + GLA_GATE_RANK + 3 * ATT_W + IDX_Q_W + IDX_DH + IDX_HEADS + 2 * D_MODEL

kernel_name = 'hybrid_gla_dsa_hmoe_block'


def rms_norm(x, g):
    xf = x.astype(jnp.float32)
    y = xf * lax.rsqrt(jnp.mean(xf * xf, axis=-1, keepdims=True) + EPS)
    return (y * g.astype(jnp.float32)).astype(x.dtype)


def gla_mixer(q, k, v, r, a_lr, w_alpha2, b_alpha, g_out):
    b, t, _ = q.shape
    nc = t // CHUNK
    f32 = jnp.float32
    q = q.reshape(b, nc, CHUNK, GLA_HEADS, GLA_DK).astype(f32) * (GLA_DK ** -0.5)
    k = k.reshape(b, nc, CHUNK, GLA_HEADS, GLA_DK)
    v = v.reshape(b, nc, CHUNK, GLA_HEADS, GLA_DV)
    log_a = jax.nn.log_sigmoid((a_lr @ w_alpha2 + b_alpha).astype(f32)) / GLA_TAU
    log_a = log_a.reshape(b, nc, CHUNK, GLA_HEADS, GLA_DK)
    cum = jnp.cumsum(log_a, axis=2)
    total = cum[:, :, -1]
    k_dec = (k.astype(f32) * jnp.exp(total[:, :, None] - cum)).astype(k.dtype)
    u = jnp.einsum('bnchd,bnche->nbhde', k_dec, v).astype(f32)

    def step(s, inp):
        dec, u_c = inp
        s = s * dec[..., None] + u_c
        return s, s

    s0 = jnp.zeros((b, GLA_HEADS, GLA_DK, GLA_DV), f32)
    _, states = lax.scan(step, s0, (jnp.exp(total).transpose(1, 0, 2, 3), u))
    o = jnp.einsum('bnchd,nbhde->bnche', q, states)
    o = rms_norm(o, g_out)
    o = o.reshape(b, t, GLA_V_W) * jax.nn.silu(r.astype(f32))
    return o.astype(r.dtype)


def t5_bucket(rel):
    half = REL_BUCKETS // 2
    exact = half // 2
    n = jnp.abs(rel)
    nf = jnp.maximum(n, 1).astype(jnp.float32)
    large = exact + (jnp.log(nf / exact) / math.log(REL_MAX_DIST / exact) * (half - exact)).astype(jnp.int32)
    large = jnp.minimum(large, half - 1)
    return jnp.where(rel > 0, half, 0) + jnp.where(n < exact, n, large)


def dsa_mixer(q, k, v, q_idx, k_idx, w_idx, rel_bias):
    b, t = q.shape[:2]
    k_top = min(TOPK_MAX, t // 4)
    nb = t // Q_BLOCK
    key_pos = jnp.arange(t)
    k_idx_f = k_idx.astype(jnp.float32)

    def to_blocks(a):
        return a.reshape(b, nb, Q_BLOCK, *a.shape[2:]).swapaxes(0, 1)

    def block(args):
        qb, qib, wb, i = args
        s = jnp.einsum('bqhd,bsd->bqhs', qib.astype(jnp.float32), k_idx_f)
        score = jnp.einsum('bqhs,bqh->bqs', jax.nn.relu(s), wb.astype(jnp.float32))
        admissible = key_pos < (i + 1) * Q_BLOCK
        score = jnp.where(admissible[None, None, :], score, -jnp.inf)
        top_val, top_idx = lax.top_k(score, k_top)
        valid = jnp.isfinite(top_val)
        k_sel = jax.vmap(lambda kk, ii: kk[ii])(k, top_idx)
        v_sel = jax.vmap(lambda vv, ii: vv[ii])(v, top_idx)
        logits = jnp.einsum('bqhd,bqkhd->bqhk', qb, k_sel).astype(jnp.float32) * (ATT_DH ** -0.5)
        q_pos = i * Q_BLOCK + jnp.arange(Q_BLOCK)
        bias = rel_bias[t5_bucket(top_idx - q_pos[None, :, None])]
        logits = logits + bias.astype(jnp.float32).transpose(0, 1, 3, 2)
        logits = jnp.where(valid[:, :, None, :], logits, -jnp.inf)
        p = jax.nn.softmax(logits, axis=-1)
        return jnp.einsum('bqhk,bqkhd->bqhd', p.astype(v.dtype), v_sel)

    out = lax.map(block, (to_blocks(q), to_blocks(q_idx), to_blocks(w_idx), jnp.arange(nb)))
    return out.swapaxes(0, 1).reshape(b, t, ATT_W)


def hier_moe(h, w_rg, b_rg, w_re, b_re, w_gate, w_up, w_down):
    b, t, d = h.shape
    f32 = jnp.float32
    xf = h.reshape(b * t, d)
    g_logits = (xf @ w_rg + b_rg).astype(f32)
    g_sel = jnp.argmax(g_logits, axis=-1)
    g_w = jnp.take_along_axis(jax.nn.softmax(g_logits, axis=-1), g_sel[:, None], axis=-1)
    e_logits = (xf @ w_re + b_re).astype(f32).reshape(-1, N_GROUPS, EXPERTS_PER_GROUP)
    e_in_group = jnp.take_along_axis(e_logits, g_sel[:, None, None], axis=1)[:, 0]
    top_v, top_i = lax.top_k(e_in_group, EXPERT_TOPK)
    top_w = jax.nn.softmax(top_v, axis=-1) * g_w
    within = jnp.sum(jax.nn.one_hot(top_i, EXPERTS_PER_GROUP, dtype=f32) * top_w[..., None], axis=1)
    combine = (jax.nn.one_hot(g_sel, N_GROUPS, dtype=f32)[:, :, None] * within[:, None, :]).reshape(-1, N_EXPERTS)
    y = jnp.zeros((b * t, d), f32)
    for e in range(N_EXPERTS):
        hid = jax.nn.silu(xf @ w_gate[e]) * (xf @ w_up[e])
        y = y + combine[:, e:e + 1] * (hid @ w_down[e]).astype(f32)
    return y.astype(h.dtype).reshape(b, t, d)


def setup_inputs(seed: int = 0) -> dict:
    key = jax.random.key(seed)
    ks = jax.random.split(key, 20)
    f32 = jnp.float32

    def nrm(k, shape, scale):
        return jax.random.normal(k, shape, f32) * scale

    def gain(k, shape):
        return 1.0 + 0.02 * jax.random.normal(k, shape, f32)

    L = DEPTH
    return {
        'x': nrm(ks[0], (BATCH, SEQ, D_MODEL), 1.0),
        'g_mix': gain(ks[1], (L, D_MODEL)),
        'w_in': nrm(ks[2], (L, D_MODEL, D_IN), D_MODEL ** -0.5),
        'w_alpha2': nrm(ks[3], (L, GLA_GATE_RANK, GLA_QK_W), GLA_GATE_RANK ** -0.5),
        'b_alpha': nrm(ks[4], (L, GLA_QK_W), 0.1),
        'g_gla': gain(ks[5], (L, GLA_DV)),
        'w_br_gla': nrm(ks[6], (L, GLA_V_W, D_MODEL), GLA_V_W ** -0.5),
        'g_q': gain(ks[7], (L, ATT_DH)),
        'g_k': gain(ks[8], (L, ATT_DH)),
        'rel_bias': nrm(ks[9], (REL_BUCKETS, ATT_HEADS), 0.3),
        'w_br_att': nrm(ks[10], (L, ATT_W, D_MODEL), ATT_W ** -0.5),
        'w_out': nrm(ks[11], (L, D_MODEL, D_MODEL), D_MODEL ** -0.5),
        'g_ffn': gain(ks[12], (L, D_MODEL)),
        'w_rg': nrm(ks[13], (L, D_MODEL, N_GROUPS), D_MODEL ** -0.5),
        'b_rg': nrm(ks[14], (L, N_GROUPS), 0.01),
        'w_re': nrm(ks[15], (L, D_MODEL, N_EXPERTS), D_MODEL ** -0.5),
        'b_re': nrm(ks[16], (L, N_EXPERTS), 0.01),
        'w_gate': nrm(ks[17], (L, N_EXPERTS, D_MODEL, D_EXPERT), D_MODEL ** -0.5),
        'w_up': nrm(ks[18], (L, N_EXPERTS, D_MODEL, D_EXPERT), D_MODEL ** -0.5),
        'w_down': nrm(ks[19], (L, N_EXPERTS, D_EXPERT, D_MODEL), D_EXPERT ** -0.5),
    }


def reference(x, g_mix, w_in, w_alpha2, b_alpha, g_gla, w_br_gla, g_q, g_k, rel_bias, w_br_att, w_out, g_ffn, w_rg, b_rg, w_re, b_re, w_gate, w_up, w_down):
    b, t, _ = x.shape
    split_points = [int(p) for p in np.cumsum(SPLITS)[:-1]]
    for l in range(DEPTH):
        hn = rms_norm(x, g_mix[l])
        proj = hn @ w_in[l]
        (qa, ka, va, ra, alr, qb, kb, vb, qi, ki, wi, gate_a, gate_b) = jnp.split(proj, split_points, axis=-1)
        ya = gla_mixer(qa, ka, va, ra, alr, w_alpha2[l], b_alpha[l], g_gla[l]) @ w_br_gla[l]
        qb = rms_norm(qb.reshape(b, t, ATT_HEADS, ATT_DH), g_q[l])
        kb = rms_norm(kb.reshape(b, t, ATT_HEADS, ATT_DH), g_k[l])
        vb = vb.reshape(b, t, ATT_HEADS, ATT_DH)
        qi = qi.reshape(b, t, IDX_HEADS, IDX_DH)
        wi = wi * (IDX_HEADS ** -0.5 * IDX_DH ** -0.5)
        yb = dsa_mixer(qb, kb, vb, qi, ki, wi, rel_bias) @ w_br_att[l]
        mixed = jax.nn.sigmoid(gate_a) * ya + jax.nn.sigmoid(gate_b) * yb
        x = x + mixed @ w_out[l]
        x = x + hier_moe(rms_norm(x, g_ffn[l]), w_rg[l], b_rg[l], w_re[l], b_re[l], w_gate[l], w_up[l], w_down[l])
    return x
```

```python
import math
from contextlib import ExitStack
import numpy as np
import concourse.bass as bass
import concourse.mybir as mybir
from concourse.bass_utils import run_bass_kernel_spmd

F32 = mybir.dt.float32
BF16 = mybir.dt.bfloat16
ALU = mybir.AluOpType
AF = mybir.ActivationFunctionType
AX = mybir.AxisListType

D = 1024
DIN = 5432
NEXP = 32
EPS = 1e-6
C_QA, C_KA, C_VA, C_RA, C_ALR, C_QB, C_KB, C_VB, C_QI, C_KI, C_WI, C_GA, C_GB = (
    0, 256, 512, 1024, 1536, 1552, 2064, 2576, 3088, 3344, 3376, 3384, 4408)
NBIS = 16
NEG = -3.0e38


class Buf:
    __slots__ = ("name", "t", "w", "r", "dsem", "excl", "isdram")

    def __init__(self, name, t=None):
        self.name = name
        self.t = t
        self.w = {}
        self.r = {}
        self.dsem = None
        self.excl = False
        self.isdram = False

    def __getitem__(self, idx):
        return self.t[idx]


class FW:
    def __init__(self, nc, needed=None):
        self.nc = nc
        self.eng = {"pe": nc.tensor, "dve": nc.vector, "act": nc.scalar,
                    "pool": nc.gpsimd, "sp": nc.sync}
        self.sems = {}
        self.cnt = {}
        self.known = {}
        self.rank = {}
        self.rankof = {}
        self.needed = needed
        self.used = set()
        for k in self.eng:
            self.sems[k] = nc.alloc_semaphore("c_" + k)
            self.cnt[k] = 0
            self.rank[k] = 0
            self.known[k] = {}
        self.nd = 0
        self._dcount = {}
        self.uid = 0

    def sb(self, st, name, shape, dt):
        self.uid += 1
        return Buf(name, st.enter_context(self.nc.sbuf_tensor("%s_%d" % (name, self.uid), list(shape), dt)))

    def ps(self, st, name, shape, dt=F32):
        self.uid += 1
        b = Buf(name, st.enter_context(self.nc.psum_tensor("%s_%d" % (name, self.uid), list(shape), dt)))
        b.excl = True
        return b

    def dram(self, name, shape, dt, kind="Internal"):
        b = Buf(name, self.nc.dram_tensor(name, list(shape), dt, kind=kind))
        b.isdram = True
        return b

    def _dsem(self, b):
        if b.dsem is None:
            key = "d%d" % self.nd
            self.nd += 1
            self.sems[key] = self.nc.alloc_semaphore(key)
            b.dsem = key
        return b.dsem

    def _emit_wait(self, e, k, v):
        if k in self.cnt:
            self.used.add((k, v))
            val = v if self.needed is None else self.rankof[(k, v)]
        else:
            val = v
        self.eng[e].wait_ge(self.sems[k], val)

    def _waits(self, e, reads, writes, skipkey=None):
        need = {}
        for b in reads:
            for k, v in b.w.items():
                if need.get(k, 0) < v:
                    need[k] = v
        for b in writes:
            if b.isdram:
                continue
            for k, v in b.w.items():
                if k != skipkey and need.get(k, 0) < v:
                    need[k] = v
            for k, v in b.r.items():
                if need.get(k, 0) < v:
                    need[k] = v
        kn = self.known[e]
        for k, v in need.items():
            if kn.get(k, 0) >= v:
                continue
            if k == "pe" and e == "pe":
                continue
            self._emit_wait(e, k, v)
            kn[k] = v

    def _count(self, e, ins):
        self.cnt[e] += 1
        o = self.cnt[e]
        if self.needed is None:
            ins.then_inc(self.sems[e], 1)
        elif (e, o) in self.needed:
            ins.then_inc(self.sems[e], 1)
            self.rank[e] += 1
            self.rankof[(e, o)] = self.rank[e]
        return o

    def op(self, e, reads, writes, fn):
        if any(b.excl for b in reads):
            writes = list(writes) + [b for b in reads if b.excl and b not in writes]
            reads = [b for b in reads if not b.excl]
        self._waits(e, reads, writes)
        ins = fn()
        o = self._count(e, ins)
        for b in writes:
            b.w = {e: o}
            b.r = {}
        for b in reads:
            if b not in writes:
                b.r[e] = o
        return ins

    def dma(self, q, out_ap, in_ap, dst, src, **kw):
        owner = src if dst.isdram else dst
        key = self._dsem(owner)
        self._waits(q, [src], [dst], skipkey=key)
        ins = self.eng[q].dma_start(out=out_ap, in_=in_ap, **kw)
        cnt = self._dcount.get(key, 0) + 16
        self._dcount[key] = cnt
        ins.then_inc(self.sems[key], 16)
        if dst.isdram:
            dst.w[key] = cnt
        else:
            if key in dst.w and len(dst.w) == 1:
                dst.w[key] = cnt
            else:
                dst.w = {key: cnt}
            dst.r = {}
        src.r[key] = cnt
        return ins

    def barrier(self):
        sp = self.eng["sp"]
        kn = self.known["sp"]
        for k in list(self.sems.keys()):
            if k == "sp":
                continue
            v = self.cnt[k] if k in self.cnt else self._dcount.get(k, 0)
            if v > 0 and kn.get(k, 0) < v:
                self._emit_wait("sp", k, v)
                kn[k] = v
        ins = sp.nop()
        v = self._count("sp", ins)
        for e in ("pe", "dve", "act", "pool"):
            self._emit_wait(e, "sp", v)
            self.known[e]["sp"] = v
            for k, kv in kn.items():
                if self.known[e].get(k, 0) < kv:
                    self.known[e][k] = kv


def _bucket(rel):
    n = abs(rel)
    if n < 8:
        v = n
    else:
        nf = np.float32(n)
        val = np.float32(np.log(nf / np.float32(8))) / np.float32(math.log(16.0)) * np.float32(8)
        v = min(8 + int(val), 15)
    return (16 if rel > 0 else 0) + v


def _bias_onehot():
    e = np.zeros((32, 512), np.float32)
    for vi, v in enumerate((-1, 0)):
        for y in range(255):
            d = v * 128 + 127 - y
            e[_bucket(d), vi * 256 + y] = 1.0
    return e


def build(NSEQ, T, dbg=False, needed=None):
    NTOK = NSEQ * T
    KTOP = min(256, T // 4)
    NST = NTOK // 512
    STS = T // 512
    TB = min(1024, NTOK)
    nc = bass.Bass("TRN2", target_bir_lowering=False)
    fw = FW(nc, needed)

    def din(name, shape):
        return fw.dram(name, shape, F32, kind="ExternalInput")

    x = din("x", [NTOK, D])
    g_mix = din("g_mix", [1, D])
    w_in = din("w_in", [1, D, DIN])
    w_alpha2 = din("w_alpha2", [1, 16, 256])
    b_alpha = din("b_alpha", [1, 256])
    g_gla = din("g_gla", [1, 128])
    w_br_gla = din("w_br_gla", [1, 512, D])
    g_q = din("g_q", [1, 64])
    g_k = din("g_k", [1, 64])
    rel_bias = din("rel_bias", [32, 8])
    w_br_att = din("w_br_att", [1, 512, D])
    w_out = din("w_out", [1, D, D])
    g_ffn = din("g_ffn", [1, D])
    w_rg = din("w_rg", [1, D, 4])
    b_rg = din("b_rg", [1, 4])
    w_re = din("w_re", [1, D, 32])
    b_re = din("b_re", [1, 32])
    w_gate = din("w_gate", [1, NEXP, D, 128])
    w_up = din("w_up", [1, NEXP, D, 128])
    w_down = din("w_down", [1, NEXP, 128, D])
    e1h = din("e1h", [32, 512])
    out = fw.dram("out", [NTOK, D], F32, kind="ExternalOutput")

    sk = "ExternalOutput" if dbg else "Internal"
    mixA_d = fw.dram("mixA_d", [D, NTOK], F32, kind=sk)
    epb_d = fw.dram("epb_d", [D, NTOK], F32, kind=sk)
    qbT_d = fw.dram("qbT_d", [512, NTOK], BF16, kind=sk)
    kbT_d = fw.dram("kbT_d", [512, NTOK], BF16, kind=sk)
    vb_d = fw.dram("vb_d", [NTOK, 520], BF16, kind=sk)
    qiT_d = fw.dram("qiT_d", [256, NTOK], BF16, kind=sk)
    kiT_d = fw.dram("kiT_d", [32, NTOK], BF16, kind=sk)
    wi_d = fw.dram("wi_d", [NTOK, 8], F32, kind=sk)
    phi_d = fw.dram("phi_d", [8, 512], F32, kind=sk)
    dbgs = {}

    def AP(buf, offset, ap):
        return bass.AP(tensor=buf.t, offset=offset, ap=ap)

    def mm(o, lhsT, rhs, start, stop, R, W):
        fw.op("pe", R, W, lambda: nc.tensor.matmul(o, lhsT=lhsT, rhs=rhs, start=start, stop=stop))

    def tr(o, in_, ident, R, W):
        fw.op("pe", R, W, lambda: nc.tensor.transpose(out=o, in_=in_, identity=ident))

    def act(o, in_, func, R, W, **kw):
        fw.op("act", R, W, lambda: nc.scalar.activation(out=o, in_=in_, func=func, **kw))

    def ts(e, o, in0, s1, s2, op0, op1, R, W, **kw):
        eng = fw.eng[e]
        if op1 is None:
            fw.op(e, R, W, lambda: eng.tensor_scalar(out=o, in0=in0, scalar1=s1, scalar2=None, op0=op0, **kw))
        else:
            fw.op(e, R, W, lambda: eng.tensor_scalar(out=o, in0=in0, scalar1=s1, scalar2=s2, op0=op0, op1=op1, **kw))

    def tt(e, o, in0, in1, op, R, W):
        eng = fw.eng[e]
        fw.op(e, R, W, lambda: eng.tensor_tensor(out=o, in0=in0, in1=in1, op=op))

    def stt(e, o, in0, scalar, in1, op0, op1, R, W):
        eng = fw.eng[e]
        fw.op(e, R, W, lambda: eng.scalar_tensor_tensor(out=o, in0=in0, scalar=scalar, in1=in1, op0=op0, op1=op1))

    def cp(e, o, in_, R, W):
        if e == "act":
            fw.op(e, R, W, lambda: nc.scalar.copy(out=o, in_=in_))
        else:
            eng = fw.eng[e]
            fw.op(e, R, W, lambda: eng.tensor_copy(out=o, in_=in_))

    def ms(e, o, val, W):
        eng = fw.eng[e]
        fw.op(e, [], W, lambda: eng.memset(o, val))

    def rcp(o, in_, R, W):
        fw.op("dve", R, W, lambda: nc.vector.reciprocal(out=o, in_=in_))

    def sigmoid_act(o_ap, in_ap, R, W):
        act(o_ap, in_ap, AF.Exp, R, W, scale=-1.0)
        act(o_ap, o_ap, AF.Ln, W + [oneb], W, bias=oneb[:, 0:1])
        act(o_ap, o_ap, AF.Exp, W, W, scale=-1.0)

    def rstd_from(ssq_ap, o_ap, scale, epsb, R, W):
        act(o_ap, ssq_ap, AF.Ln, R + [epsb], W, scale=scale, bias=epsb[:, 0:1])
        act(o_ap, o_ap, AF.Exp, W, W, scale=-0.5)

    with ExitStack() as gst:
        identb = fw.sb(gst, "identb", [128, 128], BF16)
        identf = fw.sb(gst, "identf", [128, 128], F32)
        antib = fw.sb(gst, "antib", [128, 128], BF16)
        epsb = fw.sb(gst, "epsb", [128, 1], F32)
        oneb = fw.sb(gst, "oneb", [128, 1], F32)
        onesf = fw.sb(gst, "onesf", [128, 128], F32)
        ms("pool", epsb[:], EPS, [epsb])
        ms("pool", oneb[:], 1.0, [oneb])
        ms("pool", onesf[:], 1.0, [onesf])
        ms("pool", identf[:], 0.0, [identf])
        fw.op("pool", [identf], [identf], lambda: nc.gpsimd.affine_select(
            out=identf[:], in_=identf[:], pattern=[[-1, 128]], compare_op=ALU.not_equal,
            fill=1.0, base=0, channel_multiplier=1))
        cp("dve", identb[:], identf[:], [identf], [identb])
        ms("pool", antib[:], 0.0, [antib])
        fw.op("pool", [antib], [antib], lambda: nc.gpsimd.affine_select(
            out=antib[:], in_=antib[:], pattern=[[1, 128]], compare_op=ALU.not_equal,
            fill=1.0, base=-127, channel_multiplier=1))

        with ExitStack() as st:
            winb = fw.sb(st, "winb", [128, 8, DIN], BF16)
            wbrg = fw.sb(st, "wbrg", [128, 4, D], BF16)
            wa2 = fw.sb(st, "wa2", [17, 256], F32)
            gmixB = fw.sb(st, "gmixB", [128, D], F32)
            gglaB = fw.sb(st, "gglaB", [128, 128], F32)
            gq2 = fw.sb(st, "gq2", [128, 1], F32)
            gk2 = fw.sb(st, "gk2", [128, 1], F32)
            Umat = fw.sb(st, "Umat", [128, 128], F32)
            blk1 = fw.sb(st, "blk1", [128, 128], F32)
            win_v = w_in.t.ap().rearrange("o (kc p) n -> p (o kc) n", p=128)
            CH = 776
            for c0 in range(0, DIN, CH):
                fw.dma("pool", winb[:, :, c0:c0 + CH], win_v[:, :, c0:c0 + CH], winb, w_in)
            fw.dma("pool", wbrg[:], w_br_gla.t.ap().rearrange("o (kc p) n -> p (o kc) n", p=128), wbrg, w_br_gla)
            fw.dma("sp", wa2[0:16, :], w_alpha2.t.ap().rearrange("o k n -> (o k) n"), wa2, w_alpha2)
            fw.dma("sp", wa2[16:17, :], b_alpha.t.ap(), wa2, b_alpha)
            fw.dma("sp", gmixB[:], AP(g_mix, 0, [[0, 128], [1, D]]), gmixB, g_mix)
            fw.dma("sp", gglaB[:], AP(g_gla, 0, [[0, 128], [1, 128]]), gglaB, g_gla)
            for hh in range(2):
                fw.dma("sp", gq2[hh * 64:(hh + 1) * 64, :], AP(g_q, 0, [[1, 64], [1, 1]]), gq2, g_q)
                fw.dma("sp", gk2[hh * 64:(hh + 1) * 64, :], AP(g_k, 0, [[1, 64], [1, 1]]), gk2, g_k)
            ts("dve", gq2[:], gq2[:], 0.125, None, ALU.mult, None, [gq2], [gq2])
            ms("pool", Umat[:], 1.0, [Umat])
            fw.op("pool", [Umat], [Umat], lambda: nc.gpsimd.affine_select(
                out=Umat[:], in_=Umat[:], pattern=[[-1, 128]], compare_op=ALU.is_gt,
                fill=0.0, base=0, channel_multiplier=1))
            ms("pool", Umat[64:128, 0:64], 0.0, [Umat])
            ms("pool", blk1[:], 0.0, [blk1])
            ms("pool", blk1[0:64, 0:64], 1.0 / 64, [blk1])
            ms("pool", blk1[64:128, 64:128], 1.0 / 64, [blk1])

            if dbg == "A0":
                fw.barrier()
                return nc, fw, out
            xt = [fw.sb(st, "xt%d" % i, [128, D], F32) for i in range(2)]
            junk = fw.sb(st, "junk", [128, D], BF16)
            st1 = [fw.sb(st, "st1_%d" % i, [128, 4], F32) for i in range(2)]
            xn = [fw.sb(st, "xn%d" % i, [128, D], BF16) for i in range(2)]
            xnT = [fw.sb(st, "xnT%d" % i, [128, 8, 512], BF16) for i in range(2)]
            qAT = fw.sb(st, "qAT", [128, 2, 512], BF16)
            alrT = fw.sb(st, "alrT", [17, 512], F32)
            epa = fw.sb(st, "epa", [128, 8, 512], F32)
            sqf = fw.sb(st, "sqf", [128, 512], F32)
            rsf = fw.sb(st, "rsf", [128, 512], F32)
            stgb = [fw.sb(st, "stgb%d" % i, [128, 512], BF16) for i in range(3)]
            stgf = [fw.sb(st, "stgf%d" % i, [128, 512], F32) for i in range(3)]
            eL = fw.sb(st, "eL", [128, 256], F32)
            Lt = fw.sb(st, "Lt", [128, 256], F32)
            dcy = fw.sb(st, "dcy", [128, 256], F32)
            dec = fw.sb(st, "dec", [128, 4], F32)
            kdec = fw.sb(st, "kdec", [128, 256], BF16)
            vAb = fw.sb(st, "vAb", [128, 512], BF16)
            er = fw.sb(st, "er", [128, 512], F32)
            sr = fw.sb(st, "sr", [128, 512], F32)
            S = fw.sb(st, "S", [128, 2, 256], F32)
            Sb = fw.sb(st, "Sb", [128, 2, 256], BF16)
            ssq4 = fw.sb(st, "ssq4", [128, 4], F32)
            rs4 = fw.sb(st, "rs4", [128, 4], F32)
            og = fw.sb(st, "og", [128, 512], F32)
            of = fw.sb(st, "of", [128, 512], BF16)
            oT = fw.sb(st, "oT", [128, 4, 512], BF16)
            vbst = [fw.sb(st, "vbst%d" % i, [128, 8, 65], BF16) for i in range(2)]
            wist = [fw.sb(st, "wist%d" % i, [128, 8], F32) for i in range(2)]
            pT = fw.ps(st, "pT", [128, 8, 128], BF16)
            pB = [fw.ps(st, "pB%d" % i, [128, 512], F32) for i in range(2)]
            pC = [fw.ps(st, "pC%d" % i, [128, 512], F32) for i in range(2)]
            pX = fw.ps(st, "pX", [128, 512], F32)
            pU = fw.ps(st, "pU", [128, 512], F32)
            pO = fw.ps(st, "pO", [128, 512], F32)
            ms("dve", alrT[:], 1.0, [alrT])
            for i in range(2):
                ms("dve", vbst[i][:], 1.0, [vbst[i]])

            fm_specs = [("qA", C_QA + 128 * j, 128, j) for j in range(2)]
            fm_specs += [("alr", C_ALR, 16, 0)]
            fm_specs += [("qB", C_QB + 128 * j, 128, j) for j in range(4)]
            fm_specs += [("kB", C_KB + 128 * j, 128, j) for j in range(4)]
            fm_specs += [("qi", C_QI + 128 * j, 128, j) for j in range(2)]
            fm_specs += [("ki", C_KI, 32, 0)]
            fm_specs += [("gA", C_GA + 128 * j, 128, j) for j in range(8)]
            fm_specs += [("gB", C_GB + 128 * j, 128, j) for j in range(8)]
            bi = 0
            ci = 0
            sgi = 0
            for g in range(NST):
                tok0 = g * 512
                xs = xnT[g % 2]
                if tok0 % T == 0:
                    ms("pool", S[:], 0.0, [S])
                for t in range(4):
                    xi = xt[t % 2]
                    si = st1[t % 2]
                    xni = xn[t % 2]
                    r0 = tok0 + t * 128
                    fw.dma("sp", xi[:], x.t.ap()[r0:r0 + 128, :], xi, x)
                    act(junk[:], xi[:], AF.Square, [xi], [junk, si], accum_out=si[:, 0:1])
                    rstd_from(si[:, 0:1], si[:, 1:2], 1.0 / D, epsb, [si], [si])
                    stt("dve", xni[:], xi[:], si[:, 1:2], gmixB[:], ALU.mult, ALU.mult, [xi, si, gmixB], [xni])
                    for kc in range(8):
                        tr(pT[:, kc, :], xni[:, kc * 128:(kc + 1) * 128], identb[:], [xni, identb], [pT])
                    cp("act", xs[:, :, t * 128:(t + 1) * 128], pT[:], [pT], [xs])
                if dbg == "A1":
                    fw.barrier()
                    return nc, fw, out
                for (kind, c0, M, j) in fm_specs:
                    pb = pB[bi % 2]
                    bi += 1
                    for kc in range(8):
                        mm(pb[0:M, :], winb[:, kc, c0:c0 + M], xs[:, kc, :], kc == 0, kc == 7, [winb, xs], [pb])
                    if kind == "qA":
                        fw.op("act", [pb], [qAT], lambda: nc.scalar.mul(out=qAT[:, j, :], in_=pb[:], mul=0.125))
                    elif kind == "alr":
                        cp("act", alrT[0:16, :], pb[0:16, :], [pb], [alrT])
                    elif kind in ("qB", "kB"):
                        gsc = gq2 if kind == "qB" else gk2
                        dd = qbT_d if kind == "qB" else kbT_d
                        act(sqf[:], pb[:], AF.Square, [pb], [sqf])
                        mm(pX[:], blk1[:], sqf[:], True, True, [blk1, sqf], [pX])
                        act(rsf[:], pX[:], AF.Ln, [pX, epsb], [rsf], bias=epsb[:, 0:1])
                        act(rsf[:], rsf[:], AF.Exp, [rsf], [rsf], scale=-0.5)
                        sg = stgb[sgi % 3]
                        sgi += 1
                        stt("dve", sg[:], pb[:], gsc[:, 0:1], rsf[:], ALU.mult, ALU.mult, [pb, gsc, rsf], [sg])
                        fw.dma("sp", dd.t.ap()[j * 128:(j + 1) * 128, tok0:tok0 + 512], sg[:], dd, sg)
                    elif kind == "qi":
                        sg = stgb[sgi % 3]
                        sgi += 1
                        cp("act", sg[:], pb[:], [pb], [sg])
                        fw.dma("sp", qiT_d.t.ap()[j * 128:(j + 1) * 128, tok0:tok0 + 512], sg[:], qiT_d, sg)
                    elif kind == "ki":
                        sg = stgb[sgi % 3]
                        sgi += 1
                        cp("act", sg[0:32, :], pb[0:32, :], [pb], [sg])
                        fw.dma("sp", kiT_d.t.ap()[:, tok0:tok0 + 512], sg[0:32, :], kiT_d, sg)
                    elif kind == "gA":
                        sigmoid_act(epa[:, j, :], pb[:], [pb], [epa])
                    elif kind == "gB":
                        sf = stgf[sgi % 3]
                        sgi += 1
                        sigmoid_act(sf[:], pb[:], [pb], [sf])
                        fw.dma("sp", epb_d.t.ap()[j * 128:(j + 1) * 128, tok0:tok0 + 512], sf[:], epb_d, sf)
                if dbg == "A2":
                    fw.barrier()
                    return nc, fw, out
                for t in range(4):
                    cs = slice(t * 128, (t + 1) * 128)
                    r0 = tok0 + t * 128

                    def tokmm(c0, N):
                        nonlocal ci
                        pc = pC[ci % 2]
                        ci += 1
                        for kc in range(8):
                            mm(pc[:, 0:N], xs[:, kc, cs], winb[:, kc, c0:c0 + N], kc == 0, kc == 7, [xs, winb], [pc])
                        return pc
                    pc = tokmm(C_VB, 512)
                    vs_ = vbst[t % 2]
                    cp("act", vs_[:, :, 0:64], pc[:].rearrange("p (h d) -> p h d", h=8), [pc], [vs_])
                    fw.dma("sp", vb_d.t.ap()[r0:r0 + 128, :], vs_[:].rearrange("p h d -> p (h d)"), vb_d, vs_)
                    pc = tokmm(C_WI, 8)
                    ws_ = wist[t % 2]
                    fw.op("act", [pc], [ws_], lambda: nc.scalar.mul(out=ws_[:], in_=pc[:, 0:8], mul=1.0 / 16))
                    fw.dma("sp", wi_d.t.ap()[r0:r0 + 128, :], ws_[:], wi_d, ws_)
                    mm(pX[:, 0:256], alrT[0:17, cs], wa2[0:17, :], True, True, [alrT, wa2], [pX])
                    act(eL[:], pX[:, 0:256], AF.Exp, [pX], [eL], scale=-1.0)
                    act(Lt[:], eL[:], AF.Ln, [eL, oneb], [Lt], bias=oneb[:, 0:1])
                    mm(pX[:, 256:512], Umat[:], Lt[:], True, True, [Umat, Lt], [pX])
                    act(dcy[:], pX[:, 256:512], AF.Exp, [pX], [dcy], scale=-1.0 / 16)
                    for ch in range(2):
                        for pr in range(2):
                            mm(pU[:, 500 + ch * 2 + pr:501 + ch * 2 + pr], Lt[ch * 64:(ch + 1) * 64, pr * 128:(pr + 1) * 128],
                               onesf[ch * 64:(ch + 1) * 64, 0:1], True, True, [Lt, onesf], [pU])
                    act(dec[:], pU[:, 500:504], AF.Exp, [pU], [dec], scale=-1.0 / 16)
                    pc = tokmm(C_KA, 256)
                    tt("dve", kdec[:], pc[:, 0:256], dcy[:], ALU.mult, [pc, dcy], [kdec])
                    pc = tokmm(C_VA, 512)
                    cp("act", vAb[:], pc[:], [pc], [vAb])
                    pc = tokmm(C_RA, 512)
                    sigmoid_act(er[:], pc[:], [pc], [er])
                    tt("dve", sr[:], pc[:], er[:], ALU.mult, [pc, er], [sr])
                    for ch in range(2):
                        rows = slice(ch * 64, (ch + 1) * 64)
                        for pr in range(2):
                            mm(pU[:, 0:256], kdec[rows, pr * 128:(pr + 1) * 128], vAb[rows, pr * 256:(pr + 1) * 256],
                               True, True, [kdec, vAb], [pU])
                            for hh in range(2):
                                prow = slice(hh * 64, (hh + 1) * 64)
                                stt("dve", S[prow, pr, hh * 128:(hh + 1) * 128], S[prow, pr, hh * 128:(hh + 1) * 128],
                                    dec[prow, ch * 2 + pr:ch * 2 + pr + 1], pU[prow, hh * 128:(hh + 1) * 128],
                                    ALU.mult, ALU.add, [S, dec, pU], [S])
                        cp("dve", Sb[:], S[:], [S], [Sb])
                        for pr in range(2):
                            mm(pO[rows, pr * 256:(pr + 1) * 256], qAT[:, pr, t * 128 + ch * 64:t * 128 + (ch + 1) * 64],
                               Sb[:, pr, :], True, True, [qAT, Sb], [pO])
                    act(og[:], pO[:], AF.Square, [pO], [og])
                    fw.op("dve", [og], [ssq4], lambda: nc.vector.tensor_reduce(
                        out=ssq4[:], in_=og[:].rearrange("p (h d) -> p h d", h=4), axis=AX.X, op=ALU.add))
                    rstd_from(ssq4[:], rs4[:], 1.0 / 128, epsb, [ssq4], [rs4])
                    for h in range(4):
                        stt("dve", og[:, h * 128:(h + 1) * 128], pO[:, h * 128:(h + 1) * 128], rs4[:, h:h + 1],
                            gglaB[:], ALU.mult, ALU.mult, [pO, rs4, gglaB], [og])
                    tt("dve", of[:], og[:], sr[:], ALU.mult, [og, sr], [of])
                    for h in range(4):
                        tr(pT[:, h, :], of[:, h * 128:(h + 1) * 128], identb[:], [of, identb], [pT])
                    cp("act", oT[:, :, cs], pT[:, 0:4, :], [pT], [oT])
                if dbg == "A3":
                    fw.barrier()
                    return nc, fw, out
                for oc in range(8):
                    pb = pB[bi % 2]
                    bi += 1
                    for kc in range(4):
                        mm(pb[:], wbrg[:, kc, oc * 128:(oc + 1) * 128], oT[:, kc, :], kc == 0, kc == 3, [wbrg, oT], [pb])
                    sf = stgf[sgi % 3]
                    sgi += 1
                    tt("dve", sf[:], pb[:], epa[:, oc, :], ALU.mult, [pb, epa], [sf])
                    fw.dma("sp", mixA_d.t.ap()[oc * 128:(oc + 1) * 128, tok0:tok0 + 512], sf[:], mixA_d, sf)
            fw.barrier()

        if dbg == "A":
            return nc, fw, out

        ybT_d = fw.dram("ybT_d", [512, NTOK], BF16)
        with ExitStack() as st:
            NBLK = T // 128
            rb15B = fw.sb(st, "rb15B", [128, 8], F32)
            hkb = fw.sb(st, "hkb", [128, 2, 8, 128], BF16)
            pow2 = fw.sb(st, "pow2", [128, NBIS + 2], F32)
            kbT_t = fw.sb(st, "kbT", [128, 4, T], BF16)
            vbs_t = fw.sb(st, "vbs", [128, NBLK, 520], BF16)
            ki3_t = fw.sb(st, "ki3", [96, T], BF16)
            kbTJ = [Buf("kbT%d" % j, kbT_t.t) for j in range(STS)]
            vbsJ = [Buf("vbs%d" % j, vbs_t.t) for j in range(STS)]
            ki3J = [Buf("ki3%d" % j, ki3_t.t) for j in range(STS)]
            kbT, vbs, ki3 = kbT_t, vbs_t, ki3_t
            qbs2 = [fw.sb(st, "qbs%d" % i, [128, 4, 512], BF16) for i in range(2)]
            qi32 = [fw.sb(st, "qi3%d" % i, [96, 3, 512], BF16) for i in range(2)]
            wis2 = [fw.sb(st, "wis%d" % i, [128, 4, 8], F32) for i in range(2)]
            maskT2 = [fw.sb(st, "maskT%d" % i, [128, NBLK, 512], BF16) for i in range(2)]
            diag = fw.sb(st, "diag", [128, 8, 128], BF16)
            Rb = [fw.sb(st, "Rb%d" % i, [128, 512], BF16) for i in range(2)]
            score2 = [fw.sb(st, "score%d" % i, [128, T], F32) for i in range(1)]
            maskq2 = [fw.sb(st, "maskq%d" % i, [128, T], BF16) for i in range(1)]
            bs = fw.sb(st, "bs", [128, 8], F32)
            wcols = fw.sb(st, "wcols", [128, NBIS + 2], F32)
            pex = [fw.sb(st, "pex%d" % i, [128, 512], BF16) for i in range(4)]
            ptm = [fw.sb(st, "ptm%d" % i, [128, 512], BF16) for i in range(4)]
            rec = fw.sb(st, "rec", [128, 4], F32)
            ybq = fw.sb(st, "ybq", [128, 4, 512], BF16)
            ybT = fw.sb(st, "ybT", [128, 4, 512], BF16)
            pS = [fw.ps(st, "pS%d" % i, [128, 512], F32) for i in range(2)]
            pSc = fw.ps(st, "pSc", [128, 512], F32)
            pT = fw.ps(st, "pTb", [128, 8, 128], BF16)
            pQK = [fw.ps(st, "pQK%d" % i, [128, 512], F32) for i in range(2)]
            pPV = [fw.ps(st, "pPV%d" % i, [128, 512], F32) for i in range(2)]

            with ExitStack() as st0:
                rbT = fw.sb(st0, "rbT", [32, 8], F32)
                e1s = fw.sb(st0, "e1s", [32, 512], F32)
                rb15c = fw.sb(st0, "rb15c", [8, 1], F32)
                phis = fw.sb(st0, "phis", [8, 512], F32)
                hk1 = fw.sb(st0, "hk1", [128, 128], F32)
                fw.dma("sp", rbT[:], rel_bias.t.ap(), rbT, rel_bias)
                fw.dma("sp", e1s[:], e1h.t.ap(), e1s, e1h)
                fw.dma("sp", rb15c[:], AP(rel_bias, 15 * 8, [[1, 8], [1, 1]]), rb15c, rel_bias)
                fw.dma("sp", rb15B[:], AP(rel_bias, 15 * 8, [[0, 128], [1, 8]]), rb15B, rel_bias)
                mm(pSc[0:8, :], rbT[:], e1s[:], True, True, [rbT, e1s], [pSc])
                ts("dve", phis[:], pSc[0:8, :], rb15c[:, 0:1], None, ALU.subtract, None, [pSc, rb15c], [phis])
                fw.dma("sp", phi_d.t.ap(), phis[:], phi_d, phis)
                for vi in range(2):
                    for h in range(8):
                        fw.dma("sp", hk1[:], AP(phi_d, h * 512 + vi * 256, [[1, 128], [1, 128]]), hk1, phi_d)
                        cp("dve", hkb[:, vi, h, :], hk1[:], [hk1], [hkb])
                for i in range(NBIS + 2):
                    ms("pool", pow2[:, i:i + 1], 2.0 ** (-i), [pow2])
                fw.barrier()

            cntr = {"qk": 0, "pv": 0, "bank": 0}
            bank4 = pS + pQK

            def next_bank():
                b = bank4[cntr["bank"] % 4]
                cntr["bank"] += 1
                return b

            def geo(g):
                tok0 = g * 512
                return tok0, (tok0 % T) // 512

            def loads(g):
                tok0, J = geo(g)
                qbs, qi3, wis = qbs2[g % 2], qi32[g % 2], wis2[g % 2]
                for c in range(4):
                    fw.dma("sp", kbT[:, c, J * 512:(J + 1) * 512], kbT_d.t.ap()[c * 128:(c + 1) * 128, tok0:tok0 + 512], kbTJ[J], kbT_d)
                    fw.dma("sp", qbs[:, c, :], qbT_d.t.ap()[c * 128:(c + 1) * 128, tok0:tok0 + 512], qbs, qbT_d)
                fw.dma("sp", vbs[:, 4 * J:4 * J + 4, :], vb_d.t.ap()[tok0:tok0 + 512, :].rearrange("(t p) c -> p t c", p=128), vbsJ[J], vb_d)
                for r in range(3):
                    fw.dma("sp", ki3[r * 32:(r + 1) * 32, J * 512:(J + 1) * 512], kiT_d.t.ap()[:, tok0:tok0 + 512], ki3J[J], kiT_d)
                for h in range(8):
                    fw.dma("sp", qi3[(h % 3) * 32:(h % 3) * 32 + 32, h // 3, :], qiT_d.t.ap()[h * 32:(h + 1) * 32, tok0:tok0 + 512], qi3, qiT_d)
                fw.dma("sp", wis[:], wi_d.t.ap()[tok0:tok0 + 512, :].rearrange("(t p) h -> p t h", p=128), wis, wi_d)

            def mask_chunks(g):
                tok0, J = geo(g)
                qi3, wis, maskT = qi32[g % 2], wis2[g % 2], maskT2[g % 2]

                def bis_iter(i, nk, score, maskq):
                    ts("dve", maskq[:, 0:nk], score[:, 0:nk], bs[:, 3:4], 0.0, ALU.is_ge, ALU.add, [score, bs], [maskq, bs],
                       accum_out=bs[:, 4:5])
                    ts("dve", bs[:, 5:6], bs[:, 4:5], KTOP - 0.5, 0.5, ALU.is_ge, ALU.subtract, [bs], [bs])
                    if i < NBIS:
                        stt("dve", bs[:, 3:4], bs[:, 5:6], wcols[:, i:i + 1], bs[:, 3:4], ALU.mult, ALU.add, [bs, wcols], [bs])

                def part1(t):
                    nk = (4 * J + t + 1) * 128
                    score, maskq = score2[0], maskq2[0]
                    for h in range(8):
                        ts("dve", diag[:, h, :], identb[:], wis[:, t, h:h + 1], None, ALU.mult, None, [identb, wis], [diag])
                    for s0 in range(0, nk, 512):
                        ns = min(512, nk - s0)
                        kr = [ki3J[jj] for jj in range(s0 // 512, (s0 + ns - 1) // 512 + 1)]

                        def s_mm(h):
                            ps_ = next_bank()
                            rb_ = Rb[h % 2]
                            r0 = (h % 3) * 32
                            mm(ps_[:, 0:ns], qi3[r0:r0 + 32, h // 3, t * 128:(t + 1) * 128], ki3[r0:r0 + 32, s0:s0 + ns],
                               True, True, [qi3] + kr, [ps_])
                            act(rb_[:, 0:ns], ps_[:, 0:ns], AF.Relu, [ps_], [rb_])
                        s_mm(0)
                        for h in range(8):
                            mm(pSc[:, 0:ns], diag[:, h, :], Rb[h % 2][:, 0:ns], h == 0, h == 7, [diag, Rb[h % 2]], [pSc])
                            if h + 1 < 8:
                                s_mm(h + 1)
                        cp("act", score[:, s0:s0 + ns], pSc[:, 0:ns], [pSc], [score])
                    fw.op("dve", [score], [bs], lambda: nc.vector.tensor_reduce(out=bs[:, 0:1], in_=score[:, 0:nk], axis=AX.X, op=ALU.min))
                    fw.op("dve", [score], [bs], lambda: nc.vector.tensor_reduce(out=bs[:, 1:2], in_=score[:, 0:nk], axis=AX.X, op=ALU.max))
                    ms("dve", score[0:64, nk - 64:nk], NEG, [score])
                    tt("dve", bs[:, 2:3], bs[:, 1:2], bs[:, 0:1], ALU.subtract, [bs], [bs])
                    ts("dve", wcols[:], pow2[:], bs[:, 2:3], None, ALU.mult, None, [pow2, bs], [wcols])
                    tt("dve", bs[:, 3:4], bs[:, 0:1], wcols[:, 1:2], ALU.add, [bs, wcols], [bs])
                    for i in range(1, NBIS // 2 + 1):
                        bis_iter(i, nk, score, maskq)

                def part2(t):
                    nk = (4 * J + t + 1) * 128
                    score, maskq = score2[0], maskq2[0]
                    for i in range(NBIS // 2 + 1, NBIS + 1):
                        bis_iter(i, nk, score, maskq)
                    stt("dve", bs[:, 6:7], bs[:, 5:6], wcols[:, NBIS:NBIS + 1], bs[:, 3:4], ALU.mult, ALU.add, [bs, wcols], [bs])
                    tt("dve", bs[:, 6:7], bs[:, 6:7], wcols[:, NBIS + 1:NBIS + 2], ALU.subtract, [bs, wcols], [bs])
                    ts("dve", maskq[:, 0:nk], score[:, 0:nk], bs[:, 6:7], None, ALU.is_ge, None, [score, bs], [maskq])

                def transp(t):
                    nk = (4 * J + t + 1) * 128
                    maskq = maskq2[0]
                    nb = nk // 128
                    for a0 in range(0, nb, 8):
                        na = min(8, nb - a0)
                        for a in range(a0, a0 + na):
                            tr(pT[:, a - a0, :], maskq[:, a * 128:(a + 1) * 128], identb[:], [maskq, identb], [pT])
                        cp("act", maskT[:, a0:a0 + na, t * 128:(t + 1) * 128], pT[:, 0:na, :], [pT], [maskT])

                pre = [None, lambda: part2(0), None, lambda: part2(1), None, lambda: part2(2), None, lambda: part2(3)]
                post = [lambda: part1(0), None, lambda: (transp(0), part1(1)), None,
                        lambda: (transp(1), part1(2)), None, lambda: (transp(2), part1(3)), None]
                return pre, post, (lambda: transp(3))

            def attention_head(g, h):
                tok0, J = geo(g)
                qbs, maskT = qbs2[g % 2], maskT2[g % 2]
                c = h // 2
                pb_ = (h % 2) * 64
                ppv = pPV[cntr["pv"] % 2]
                cntr["pv"] += 1
                na_tot = 4 * J + 4

                def qk_stage(a):
                    u = a - 4 * J
                    q0 = max(0, u) * 128
                    nq = 512 - q0
                    qki = cntr["qk"]
                    pq = next_bank()
                    pe_ = pex[qki % 4]
                    pt_ = ptm[qki % 4]
                    cntr["qk"] += 1
                    near = [(tq, a - 4 * J - tq) for tq in range(q0 // 128, 4) if (a - 4 * J - tq) in (-1, 0)]
                    mm(pq[:, 0:nq], kbT[pb_:pb_ + 64, c, a * 128:(a + 1) * 128], qbs[pb_:pb_ + 64, c, q0:512],
                       True, len(near) == 0, [kbTJ[a // 4], qbs], [pq])
                    for ni, (tq, v) in enumerate(near):
                        mm(pq[:, tq * 128 - q0:(tq + 1) * 128 - q0], antib[:], hkb[:, v + 1, h, :],
                           False, ni == len(near) - 1, [antib, hkb], [pq])
                    act(pe_[:, 0:nq], pq[:, 0:nq], AF.Exp, [pq, rb15B], [pe_], bias=rb15B[:, h:h + 1])
                    tt("pool", pt_[:, 0:nq], pe_[:, 0:nq], maskT[:, a, q0:512], ALU.mult, [pe_, maskT], [pt_])
                    return (a, q0, pt_)

                def pv_stage(st_):
                    a, q0, pt_ = st_
                    for tq in range(q0 // 128, 4):
                        fw.op("pe", [pt_, vbsJ[a // 4]], [ppv], lambda: nc.tensor.matmul(
                            ppv[:, tq * 65:(tq + 1) * 65], lhsT=pt_[:, tq * 128 - q0:(tq + 1) * 128 - q0], rhs=vbs[:, a, h * 65:(h + 1) * 65],
                            start=(a == 0 and tq == 0), stop=(a == 4 * J + tq), skip_group_check=True))
                pend = []
                for a in range(na_tot):
                    pend.append(qk_stage(a))
                    if len(pend) > 2:
                        pv_stage(pend.pop(0))
                while pend:
                    pv_stage(pend.pop(0))
                act(rec[:], ppv[:, 0:260].rearrange("p (t c) -> p t c", c=65)[:, :, 64], AF.Ln, [ppv], [rec])
                act(rec[:], rec[:], AF.Exp, [rec], [rec], scale=-1.0)
                for tq in range(4):
                    fw.op("act", [ppv, rec], [ybq], lambda: nc.scalar.mul(
                        out=ybq[:, tq, h * 64:(h + 1) * 64], in_=ppv[:, tq * 65:tq * 65 + 64], mul=rec[:, tq:tq + 1]))

            def finish(g):
                tok0, J = geo(g)
                for tq in range(4):
                    for c in range(4):
                        tr(pT[:, c, :], ybq[:, tq, c * 128:(c + 1) * 128], identb[:], [ybq, identb], [pT])
                    cp("act", ybT[:, :, tq * 128:(tq + 1) * 128], pT[:, 0:4, :], [pT], [ybT])
                for c in range(4):
                    fw.dma("sp", ybT_d.t.ap()[c * 128:(c + 1) * 128, tok0:tok0 + 512], ybT[:, c, :], ybT_d, ybT)

            def run_masks(g):
                pre, post, tail = mask_chunks(g)
                for h in range(8):
                    if pre[h] is not None:
                        pre[h]()
                    if post[h] is not None:
                        post[h]()
                tail()

            loads(0)
            run_masks(0)
            for g in range(NST):
                pre, post, tail = None, None, None
                defer = False
                if g + 1 < NST:
                    if geo(g + 1)[1] == 0:
                        defer = True
                    else:
                        loads(g + 1)
                        pre, post, tail = mask_chunks(g + 1)
                for h in range(8):
                    if pre is not None and pre[h] is not None:
                        pre[h]()
                    attention_head(g, h)
                    if post is not None and post[h] is not None:
                        post[h]()
                finish(g)
                if tail is not None:
                    tail()
                if defer:
                    loads(g + 1)
                    run_masks(g + 1)
            fw.barrier()

        with ExitStack() as st:
            wbra = fw.sb(st, "wbra", [128, 4, D], BF16)
            woutb = fw.sb(st, "woutb", [128, 8, D], BF16)
            fw.dma("pool", wbra[:], w_br_att.t.ap().rearrange("o (kc p) n -> p (o kc) n", p=128), wbra, w_br_att)
            fw.dma("pool", woutb[:], w_out.t.ap().rearrange("o (kc p) n -> p (o kc) n", p=128), woutb, w_out)
            ybl = [fw.sb(st, "ybl%d" % i, [128, 4, 512], BF16) for i in range(2)]
            epl = [fw.sb(st, "epl%d" % i, [128, 512], F32) for i in range(3)]
            mal = [fw.sb(st, "mal%d" % i, [128, 512], F32) for i in range(3)]
            t1f = [fw.sb(st, "t1f%d" % i, [128, 512], F32) for i in range(2)]
            mixT2 = [fw.sb(st, "mixT%d" % i, [128, 8, 512], BF16) for i in range(2)]
            xr = [fw.sb(st, "xr%d" % i, [128, D], F32) for i in range(3)]
            pQ2 = [fw.ps(st, "pQ2%d" % i, [128, 512], F32) for i in range(3)]
            pS2 = [fw.ps(st, "pS2%d" % i, [128, 512], F32) for i in range(4)]
            k3 = 0
            k4 = 0
            kx = 0
            for g in range(NST):
                tok0 = g * 512
                yb_ = ybl[g % 2]
                mixT = mixT2[g % 2]
                for c in range(4):
                    fw.dma("sp", yb_[:, c, :], ybT_d.t.ap()[c * 128:(c + 1) * 128, tok0:tok0 + 512], yb_, ybT_d)
                for oc in range(8):
                    pq = pQ2[k3 % 3]
                    ep_ = epl[k3 % 3]
                    ma_ = mal[k3 % 3]
                    tf_ = t1f[k3 % 2]
                    k3 += 1
                    fw.dma("sp", ep_[:], epb_d.t.ap()[oc * 128:(oc + 1) * 128, tok0:tok0 + 512], ep_, epb_d)
                    fw.dma("sp", ma_[:], mixA_d.t.ap()[oc * 128:(oc + 1) * 128, tok0:tok0 + 512], ma_, mixA_d)
                    for kc in range(4):
                        mm(pq[:], wbra[:, kc, oc * 128:(oc + 1) * 128], yb_[:, kc, :], kc == 0, kc == 3, [wbra, yb_], [pq])
                    tt("dve", tf_[:], pq[:], ep_[:], ALU.mult, [pq, ep_], [tf_])
                    tt("pool", mixT[:, oc, :], tf_[:], ma_[:], ALU.add, [tf_, ma_], [mixT])
                for tq in range(4):
                    r0 = tok0 + tq * 128
                    xi = xr[kx % 3]
                    kx += 1
                    fw.dma("sp", xi[:], x.t.ap()[r0:r0 + 128, :], xi, x)
                    for hf in range(2):
                        ps_ = pS2[k4 % 4]
                        k4 += 1
                        for kc in range(8):
                            mm(ps_[:], mixT[:, kc, tq * 128:(tq + 1) * 128], woutb[:, kc, hf * 512:(hf + 1) * 512],
                               kc == 0, kc == 7, [mixT, woutb], [ps_])
                        tt("dve", xi[:, hf * 512:(hf + 1) * 512], ps_[:], xi[:, hf * 512:(hf + 1) * 512], ALU.add, [ps_, xi], [xi])
                    fw.dma("sp", out.t.ap()[r0:r0 + 128, :], xi[:], out, xi)
            fw.barrier()

        if dbg == "B":
            return nc, fw, out

        with ExitStack() as st:
            NH = TB // 512
            NTB = TB // 128
            wdb = fw.sb(st, "wdb", [128, NEXP, D], BF16)
            for e0 in range(0, NEXP, 4):
                fw.dma("pool", wdb[:, e0:e0 + 4, :], w_down.t.ap().rearrange("o e k n -> k (o e) n")[:, e0:e0 + 4, :], wdb, w_down)
            gffB = fw.sb(st, "gffB", [128, D], F32)
            fw.dma("sp", gffB[:], AP(g_ffn, 0, [[0, 128], [1, D]]), gffB, g_ffn)
            wr = fw.sb(st, "wr", [128, 8, 36], F32)
            fw.dma("sp", wr[:, :, 0:4], w_rg.t.ap().rearrange("o (kc p) n -> p (o kc) n", p=128), wr, w_rg)
            fw.dma("sp", wr[:, :, 4:36], w_re.t.ap().rearrange("o (kc p) n -> p (o kc) n", p=128), wr, w_re)
            br = fw.sb(st, "br", [1, 36], F32)
            fw.dma("sp", br[:, 0:4], b_rg.t.ap(), br, b_rg)
            fw.dma("sp", br[:, 4:36], b_re.t.ap(), br, b_re)
            selA = fw.sb(st, "selA", [64, NEXP, 128], BF16)
            ms("pool", selA[:], 0.0, [selA])
            for base in (0, -32):
                fw.op("pool", [selA], [selA], lambda: nc.gpsimd.affine_select(
                    out=selA[:], in_=selA[:], pattern=[[-1, NEXP], [0, 128]], compare_op=ALU.not_equal,
                    fill=1.0, base=base, channel_multiplier=1))
            xt = [fw.sb(st, "xtc%d" % i, [128, D], F32) for i in range(2)]
            junk = fw.sb(st, "junkc", [128, D], BF16)
            st1 = [fw.sb(st, "st1c%d" % i, [128, 4], F32) for i in range(2)]
            xnf = [fw.sb(st, "xnf%d" % i, [128, D], F32) for i in range(1)]
            xfT = fw.sb(st, "xfT", [128, 8, 128], F32)
            xn2T = fw.sb(st, "xn2T", [128, 8, TB], BF16)
            lg = fw.sb(st, "lg", [128, 36], F32)
            rt = fw.sb(st, "rt", [128, 16], F32)
            ohg = fw.sb(st, "ohg", [128, 4], F32)
            esel = fw.sb(st, "esel", [128, 8], F32)
            top8 = fw.sb(st, "top8", [128, 8], F32)
            oh1 = fw.sb(st, "oh1", [128, 8], F32)
            oh2 = fw.sb(st, "oh2", [128, 8], F32)
            within = fw.sb(st, "within", [128, 8], F32)
            comb = fw.sb(st, "comb", [128, 32], F32)
            c2b = fw.sb(st, "c2b", [128, 32], BF16)
            c2f = fw.sb(st, "c2f", [128, 64], F32)
            cT2 = fw.sb(st, "cT2", [64, TB], BF16)
            wg = [fw.sb(st, "wg%d" % i, [128, 8, 128], BF16) for i in range(3)]
            wu = [fw.sb(st, "wu%d" % i, [128, 8, 128], BF16) for i in range(3)]
            eg = [fw.sb(st, "eg%d" % i, [128, 512], F32) for i in range(2)]
            t1 = [fw.sb(st, "t1_%d" % i, [128, 512], F32) for i in range(2)]
            hT = fw.sb(st, "hT", [128, NEXP, TB], BF16)
            x2l = [fw.sb(st, "x2l%d" % i, [128, D], F32) for i in range(1)]
            pTf = fw.ps(st, "pTf", [128, 4, 128], F32)
            pR = fw.ps(st, "pR", [128, 512], F32)
            pG = [fw.ps(st, "pG%d" % i, [128, 512], F32) for i in range(2)]
            pU2 = [fw.ps(st, "pU2%d" % i, [128, 512], F32) for i in range(2)]
            pBC = fw.ps(st, "pBC", [128, 512], F32)
            pD = fw.ps(st, "pD", [128, 512], F32)
            wgv = w_gate.t.ap().rearrange("o e (kc p) n -> (o e) p kc n", p=128)
            wuv = w_up.t.ap().rearrange("o e (kc p) n -> (o e) p kc n", p=128)
            if dbg == "C0":
                fw.barrier()
                return nc, fw, out
            wi_ = 0
            for blk in range(NTOK // TB):
                b0 = blk * TB
                for t in range(NTB):
                    xi = xt[t % 2]
                    si = st1[t % 2]
                    xf = xnf[0]
                    r0 = b0 + t * 128
                    cs = slice(t * 128, (t + 1) * 128)
                    fw.dma("sp", xi[:], out.t.ap()[r0:r0 + 128, :], xi, out)
                    act(junk[:], xi[:], AF.Square, [xi], [junk, si], accum_out=si[:, 0:1])
                    rstd_from(si[:, 0:1], si[:, 1:2], 1.0 / D, epsb, [si], [si])
                    stt("dve", xf[:], xi[:], si[:, 1:2], gffB[:], ALU.mult, ALU.mult, [xi, si, gffB], [xf])
                    for half in range(2):
                        for k4 in range(4):
                            kc = half * 4 + k4
                            tr(pTf[:, k4, :], xf[:, kc * 128:(kc + 1) * 128], identf[:], [xf, identf], [pTf])
                        cp("act", xfT[:, half * 4:half * 4 + 4, :], pTf[:], [pTf], [xfT])
                        cp("dve", xn2T[:, half * 4:half * 4 + 4, cs], pTf[:], [pTf], [xn2T])
                    for kc in range(8):
                        mm(pR[:, 0:36], xfT[:, kc, :], wr[:, kc, :], kc == 0, False, [xfT, wr], [pR])
                    mm(pR[:, 0:36], onesf[0:1, :], br[0:1, :], False, True, [onesf, br], [pR])
                    cp("act", lg[:], pR[:, 0:36], [pR], [lg])
                    fw.op("dve", [lg], [rt], lambda: nc.vector.tensor_reduce(out=rt[:, 0:1], in_=lg[:, 0:4], axis=AX.X, op=ALU.max))
                    ts("dve", rt[:, 1:2], rt[:, 0:1], -1.0, None, ALU.mult, None, [rt], [rt])
                    act(ohg[:], lg[:, 0:4], AF.Exp, [lg, rt], [ohg, rt], bias=rt[:, 1:2], accum_out=rt[:, 2:3])
                    rcp(rt[:, 3:4], rt[:, 2:3], [rt], [rt])
                    ts("dve", ohg[:], lg[:, 0:4], rt[:, 0:1], None, ALU.is_ge, None, [lg, rt], [ohg])
                    ts("dve", esel[:], lg[:, 4:12], ohg[:, 0:1], None, ALU.mult, None, [lg, ohg], [esel])
                    for gi in range(1, 4):
                        stt("dve", esel[:], lg[:, 4 + 8 * gi:12 + 8 * gi], ohg[:, gi:gi + 1], esel[:], ALU.mult, ALU.add,
                            [lg, ohg, esel], [esel])
                    fw.op("dve", [esel], [top8], lambda: nc.vector.max(out=top8[:], in_=esel[:]))
                    ts("dve", oh1[:], esel[:], top8[:, 0:1], None, ALU.is_equal, None, [esel, top8], [oh1])
                    ts("dve", oh2[:], esel[:], top8[:, 1:2], None, ALU.is_equal, None, [esel, top8], [oh2])
                    tt("dve", rt[:, 4:5], top8[:, 1:2], top8[:, 0:1], ALU.subtract, [top8], [rt])
                    act(rt[:, 5:6], rt[:, 4:5], AF.Exp, [rt], [rt])
                    ts("dve", rt[:, 5:6], rt[:, 5:6], 1.0, None, ALU.add, None, [rt], [rt])
                    rcp(rt[:, 6:7], rt[:, 5:6], [rt], [rt])
                    tt("dve", rt[:, 7:8], rt[:, 6:7], rt[:, 3:4], ALU.mult, [rt], [rt])
                    tt("dve", rt[:, 8:9], rt[:, 3:4], rt[:, 7:8], ALU.subtract, [rt], [rt])
                    ts("dve", within[:], oh1[:], rt[:, 7:8], None, ALU.mult, None, [oh1, rt], [within])
                    stt("dve", within[:], oh2[:], rt[:, 8:9], within[:], ALU.mult, ALU.add, [oh2, rt, within], [within])
                    for gi in range(4):
                        ts("dve", comb[:, gi * 8:(gi + 1) * 8], within[:], ohg[:, gi:gi + 1], None, ALU.mult, None,
                           [within, ohg], [comb])
                    cp("dve", c2b[:], comb[:], [comb], [c2b])
                    cp("dve", c2f[:, 0:32], c2b[:], [c2b], [c2f])
                    tt("dve", c2f[:, 32:64], comb[:], c2f[:, 0:32], ALU.subtract, [comb, c2f], [c2f])
                    tr(pTf[0:64, 0, :], c2f[:, :], identf[:], [c2f, identf], [pTf])
                    cp("act", cT2[:, cs], pTf[0:64, 0, :], [pTf], [cT2])
                if dbg == "C1":
                    fw.barrier()
                    return nc, fw, out
                for e in range(NEXP):
                    wg_ = wg[wi_ % 3]
                    wu_ = wu[wi_ % 3]
                    wi_ += 1
                    fw.dma("pool", wg_[:], wgv[e], wg_, w_gate)
                    fw.dma("pool", wu_[:], wuv[e], wu_, w_up)
                    for half in range(NH):
                        hs = slice(half * 512, (half + 1) * 512)
                        pg_ = pG[(e * NH + half) % 2]
                        pu_ = pU2[(e * NH + half) % 2]
                        eg_ = eg[(e * NH + half) % 2]
                        t1_ = t1[(e * NH + half) % 2]
                        for kc in range(8):
                            mm(pg_[:], wg_[:, kc, :], xn2T[:, kc, hs], kc == 0, kc == 7, [wg_, xn2T], [pg_])
                        for kc in range(8):
                            mm(pu_[:], wu_[:, kc, :], xn2T[:, kc, hs], kc == 0, kc == 7, [wu_, xn2T], [pu_])
                        pbc_ = pBC if (e * NH + half) % 2 == 0 else pD
                        mm(pbc_[:], selA[:, e, :], cT2[:, hs], True, True, [selA, cT2], [pbc_])
                        act(eg_[:], pg_[:], AF.Silu, [pg_], [eg_])
                        tt("dve", t1_[:], eg_[:], pu_[:], ALU.mult, [eg_, pu_], [t1_])
                        tt("dve", hT[:, e, hs], t1_[:], pbc_[:], ALU.mult, [t1_, pbc_], [hT])
                if dbg == "C2":
                    fw.barrier()
                    return nc, fw, out
                for t in range(NTB):
                    r0 = b0 + t * 128
                    xi = x2l[0]
                    fw.dma("sp", xi[:], out.t.ap()[r0:r0 + 128, :], xi, out)
                    pacc = (pD, pBC)
                    for e in range(NEXP):
                        for hf in range(2):
                            mm(pacc[hf][:], hT[:, e, t * 128:(t + 1) * 128], wdb[:, e, hf * 512:(hf + 1) * 512], e == 0, e == NEXP - 1,
                               [hT, wdb], [pacc[hf]])
                    for hf in range(2):
                        tt("dve", xi[:, hf * 512:(hf + 1) * 512], pacc[hf][:], xi[:, hf * 512:(hf + 1) * 512], ALU.add, [pacc[hf], xi], [xi])
                    fw.dma("sp", out.t.ap()[r0:r0 + 128, :], xi[:], out, xi)
            fw.barrier()
    return nc, fw, out


_E1H = None


def _inputs_for_core(inputs, c, NSEQ, T):
    global _E1H
    if _E1H is None:
        _E1H = _bias_onehot()
    m = {}
    xs = np.ascontiguousarray(inputs["x"][c * NSEQ:(c + 1) * NSEQ]).reshape(NSEQ * T, D)
    m["x"] = xs
    for k, v in inputs.items():
        if k == "x":
            continue
        m[k] = np.ascontiguousarray(np.asarray(v, dtype=np.float32))
    m["e1h"] = _E1H
    return m


def kernel(**inputs):
    x = np.asarray(inputs["x"])
    B, T, _ = x.shape
    NSEQ = B // 8
    _, fw0, _ = build(NSEQ, T)
    nc, fw, out = build(NSEQ, T, needed=fw0.used)
    in_maps = [_inputs_for_core(inputs, c, NSEQ, T) for c in range(8)]
    res = run_bass_kernel_spmd(nc, in_maps, core_ids=list(range(8)))
    outs = [np.asarray(res.results[c]["out"]).reshape(NSEQ, T, D) for c in range(8)]
    return np.concatenate(outs, axis=0).astype(np.float32)
```

```python
import math
from contextlib import ExitStack
import numpy as np
import concourse.bass as bass
import concourse.mybir as mybir
from concourse.bass_utils import run_bass_kernel_spmd

F32 = mybir.dt.float32
BF16 = mybir.dt.bfloat16
ALU = mybir.AluOpType
AF = mybir.ActivationFunctionType
AX = mybir.AxisListType

D = 1024
DIN = 5432
NEXP = 32
EPS = 1e-6
C_QA, C_KA, C_VA, C_RA, C_ALR, C_QB, C_KB, C_VB, C_QI, C_KI, C_WI, C_GA, C_GB = (
    0, 256, 512, 1024, 1536, 1552, 2064, 2576, 3088, 3344, 3376, 3384, 4408)
NBIS = 16
NEG = -3.0e38


class Buf:
    __slots__ = ("name", "t", "w", "r", "dsem", "excl", "isdram")

    def __init__(self, name, t=None):
        self.name = name
        self.t = t
        self.w = {}
        self.r = {}
        self.dsem = None
        self.excl = False
        self.isdram = False

    def __getitem__(self, idx):
        return self.t[idx]


class FW:
    def __init__(self, nc, needed=None):
        self.nc = nc
        self.eng = {"pe": nc.tensor, "dve": nc.vector, "act": nc.scalar,
                    "pool": nc.gpsimd, "sp": nc.sync}
        self.sems = {}
        self.cnt = {}
        self.known = {}
        self.rank = {}
        self.rankof = {}
        self.needed = needed
        self.used = set()
        for k in self.eng:
            self.sems[k] = nc.alloc_semaphore("c_" + k)
            self.cnt[k] = 0
            self.rank[k] = 0
            self.known[k] = {}
        self.nd = 0
        self._dcount = {}
        self.uid = 0

    def sb(self, st, name, shape, dt):
        self.uid += 1
        return Buf(name, st.enter_context(self.nc.sbuf_tensor("%s_%d" % (name, self.uid), list(shape), dt)))

    def ps(self, st, name, shape, dt=F32):
        self.uid += 1
        b = Buf(name, st.enter_context(self.nc.psum_tensor("%s_%d" % (name, self.uid), list(shape), dt)))
        b.excl = True
        return b

    def dram(self, name, shape, dt, kind="Internal"):
        b = Buf(name, self.nc.dram_tensor(name, list(shape), dt, kind=kind))
        b.isdram = True
        return b

    def _dsem(self, b):
        if b.dsem is None:
            key = "d%d" % self.nd
            self.nd += 1
            self.sems[key] = self.nc.alloc_semaphore(key)
            b.dsem = key
        return b.dsem

    def _emit_wait(self, e, k, v):
        if k in self.cnt:
            self.used.add((k, v))
            val = v if self.needed is None else self.rankof[(k, v)]
        else:
            val = v
        self.eng[e].wait_ge(self.sems[k], val)

    def _waits(self, e, reads, writes, skipkey=None):
        need = {}
        for b in reads:
            for k, v in b.w.items():
                if need.get(k, 0) < v:
                    need[k] = v
        for b in writes:
            if b.isdram:
                continue
            for k, v in b.w.items():
                if k != skipkey and need.get(k, 0) < v:
                    need[k] = v
            for k, v in b.r.items():
                if need.get(k, 0) < v:
                    need[k] = v
        kn = self.known[e]
        for k, v in need.items():
            if kn.get(k, 0) >= v:
                continue
            if k == "pe" and e == "pe":
                continue
            self._emit_wait(e, k, v)
            kn[k] = v

    def _count(self, e, ins):
        self.cnt[e] += 1
        o = self.cnt[e]
        if self.needed is None:
            ins.then_inc(self.sems[e], 1)
        elif (e, o) in self.needed:
            ins.then_inc(self.sems[e], 1)
            self.rank[e] += 1
            self.rankof[(e, o)] = self.rank[e]
        return o

    def op(self, e, reads, writes, fn):
        if any(b.excl for b in reads):
            writes = list(writes) + [b for b in reads if b.excl and b not in writes]
            reads = [b for b in reads if not b.excl]
        self._waits(e, reads, writes)
        ins = fn()
        o = self._count(e, ins)
        for b in writes:
            b.w = {e: o}
            b.r = {}
        for b in reads:
            if b not in writes:
                b.r[e] = o
        return ins

    def dma(self, q, out_ap, in_ap, dst, src, **kw):
        owner = src if dst.isdram else dst
        key = self._dsem(owner)
        self._waits(q, [src], [dst], skipkey=key)
        ins = self.eng[q].dma_start(out=out_ap, in_=in_ap, **kw)
        cnt = self._dcount.get(key, 0) + 16
        self._dcount[key] = cnt
        ins.then_inc(self.sems[key], 16)
        if dst.isdram:
            dst.w[key] = cnt
        else:
            if key in dst.w and len(dst.w) == 1:
                dst.w[key] = cnt
            else:
                dst.w = {key: cnt}
            dst.r = {}
        src.r[key] = cnt
        return ins

    def barrier(self):
        sp = self.eng["sp"]
        kn = self.known["sp"]
        for k in list(self.sems.keys()):
            if k == "sp":
                continue
            v = self.cnt[k] if k in self.cnt else self._dcount.get(k, 0)
            if v > 0 and kn.get(k, 0) < v:
                self._emit_wait("sp", k, v)
                kn[k] = v
        ins = sp.nop()
        v = self._count("sp", ins)
        for e in ("pe", "dve", "act", "pool"):
            self._emit_wait(e, "sp", v)
            self.known[e]["sp"] = v
            for k, kv in kn.items():
                if self.known[e].get(k, 0) < kv:
                    self.known[e][k] = kv


def _bucket(rel):
    n = abs(rel)
    if n < 8:
        v = n
    else:
        nf = np.float32(n)
        val = np.float32(np.log(nf / np.float32(8))) / np.float32(math.log(16.0)) * np.float32(8)
        v = min(8 + int(val), 15)
    return (16 if rel > 0 else 0) + v


def _bias_onehot():
    e = np.zeros((32, 512), np.float32)
    for vi, v in enumerate((-1, 0)):
        for y in range(255):
            d = v * 128 + 127 - y
            e[_bucket(d), vi * 256 + y] = 1.0
    return e


def build(NSEQ, T, dbg=False, needed=None):
    NTOK = NSEQ * T
    KTOP = min(256, T // 4)
    NST = NTOK // 512
    STS = T // 512
    TB = min(1024, NTOK)
    nc = bass.Bass("TRN2", target_bir_lowering=False)
    fw = FW(nc, needed)

    def din(name, shape):
        return fw.dram(name, shape, F32, kind="ExternalInput")

    x = din("x", [NTOK, D])
    g_mix = din("g_mix", [1, D])
    w_in = din("w_in", [1, D, DIN])
    w_alpha2 = din("w_alpha2", [1, 16, 256])
    b_alpha = din("b_alpha", [1, 256])
    g_gla = din("g_gla", [1, 128])
    w_br_gla = din("w_br_gla", [1, 512, D])
    g_q = din("g_q", [1, 64])
    g_k = din("g_k", [1, 64])
    rel_bias = din("rel_bias", [32, 8])
    w_br_att = din("w_br_att", [1, 512, D])
    w_out = din("w_out", [1, D, D])
    g_ffn = din("g_ffn", [1, D])
    w_rg = din("w_rg", [1, D, 4])
    b_rg = din("b_rg", [1, 4])
    w_re = din("w_re", [1, D, 32])
    b_re = din("b_re", [1, 32])
    w_gate = din("w_gate", [1, NEXP, D, 128])
    w_up = din("w_up", [1, NEXP, D, 128])
    w_down = din("w_down", [1, NEXP, 128, D])
    e1h = din("e1h", [32, 512])
    out = fw.dram("out", [NTOK, D], F32, kind="ExternalOutput")

    sk = "ExternalOutput" if dbg else "Internal"
    mixA_d = fw.dram("mixA_d", [D, NTOK], F32, kind=sk)
    epb_d = fw.dram("epb_d", [D, NTOK], F32, kind=sk)
    qbT_d = fw.dram("qbT_d", [512, NTOK], BF16, kind=sk)
    kbT_d = fw.dram("kbT_d", [512, NTOK], BF16, kind=sk)
    vb_d = fw.dram("vb_d", [NTOK, 520], BF16, kind=sk)
    qiT_d = fw.dram("qiT_d", [256, NTOK], BF16, kind=sk)
    kiT_d = fw.dram("kiT_d", [32, NTOK], BF16, kind=sk)
    wi_d = fw.dram("wi_d", [NTOK, 8], F32, kind=sk)
    phi_d = fw.dram("phi_d", [8, 512], F32, kind=sk)
    dbgs = {}

    def AP(buf, offset, ap):
        return bass.AP(tensor=buf.t, offset=offset, ap=ap)

    def mm(o, lhsT, rhs, start, stop, R, W):
        fw.op("pe", R, W, lambda: nc.tensor.matmul(o, lhsT=lhsT, rhs=rhs, start=start, stop=stop))

    def tr(o, in_, ident, R, W):
        fw.op("pe", R, W, lambda: nc.tensor.transpose(out=o, in_=in_, identity=ident))

    def act(o, in_, func, R, W, **kw):
        fw.op("act", R, W, lambda: nc.scalar.activation(out=o, in_=in_, func=func, **kw))

    def ts(e, o, in0, s1, s2, op0, op1, R, W, **kw):
        eng = fw.eng[e]
        if op1 is None:
            fw.op(e, R, W, lambda: eng.tensor_scalar(out=o, in0=in0, scalar1=s1, scalar2=None, op0=op0, **kw))
        else:
            fw.op(e, R, W, lambda: eng.tensor_scalar(out=o, in0=in0, scalar1=s1, scalar2=s2, op0=op0, op1=op1, **kw))

    def tt(e, o, in0, in1, op, R, W):
        eng = fw.eng[e]
        fw.op(e, R, W, lambda: eng.tensor_tensor(out=o, in0=in0, in1=in1, op=op))

    def stt(e, o, in0, scalar, in1, op0, op1, R, W):
        eng = fw.eng[e]
        fw.op(e, R, W, lambda: eng.scalar_tensor_tensor(out=o, in0=in0, scalar=scalar, in1=in1, op0=op0, op1=op1))

    def cp(e, o, in_, R, W):
        if e == "act":
            fw.op(e, R, W, lambda: nc.scalar.copy(out=o, in_=in_))
        else:
            eng = fw.eng[e]
            fw.op(e, R, W, lambda: eng.tensor_copy(out=o, in_=in_))

    def ms(e, o, val, W):
        eng = fw.eng[e]
        fw.op(e, [], W, lambda: eng.memset(o, val))

    def rcp(o, in_, R, W):
        fw.op("dve", R, W, lambda: nc.vector.reciprocal(out=o, in_=in_))

    def sigmoid_act(o_ap, in_ap, R, W):
        act(o_ap, in_ap, AF.Exp, R, W, scale=-1.0)
        act(o_ap, o_ap, AF.Ln, W + [oneb], W, bias=oneb[:, 0:1])
        act(o_ap, o_ap, AF.Exp, W, W, scale=-1.0)

    def rstd_from(ssq_ap, o_ap, scale, epsb, R, W):
        act(o_ap, ssq_ap, AF.Ln, R + [epsb], W, scale=scale, bias=epsb[:, 0:1])
        act(o_ap, o_ap, AF.Exp, W, W, scale=-0.5)

    with ExitStack() as gst:
        identb = fw.sb(gst, "identb", [128, 128], BF16)
        identf = fw.sb(gst, "identf", [128, 128], F32)
        antib = fw.sb(gst, "antib", [128, 128], BF16)
        epsb = fw.sb(gst, "epsb", [128, 1], F32)
        oneb = fw.sb(gst, "oneb", [128, 1], F32)
        onesf = fw.sb(gst, "onesf", [128, 128], F32)
        ms("pool", epsb[:], EPS, [epsb])
        ms("pool", oneb[:], 1.0, [oneb])
        ms("pool", onesf[:], 1.0, [onesf])
        ms("pool", identf[:], 0.0, [identf])
        fw.op("pool", [identf], [identf], lambda: nc.gpsimd.affine_select(
            out=identf[:], in_=identf[:], pattern=[[-1, 128]], compare_op=ALU.not_equal,
            fill=1.0, base=0, channel_multiplier=1))
        cp("dve", identb[:], identf[:], [identf], [identb])
        ms("pool", antib[:], 0.0, [antib])
        fw.op("pool", [antib], [antib], lambda: nc.gpsimd.affine_select(
            out=antib[:], in_=antib[:], pattern=[[1, 128]], compare_op=ALU.not_equal,
            fill=1.0, base=-127, channel_multiplier=1))

        with ExitStack() as st:
            winb = fw.sb(st, "winb", [128, 8, DIN], BF16)
            wbrg = fw.sb(st, "wbrg", [128, 4, D], BF16)
            wa2 = fw.sb(st, "wa2", [17, 256], F32)
            gmixB = fw.sb(st, "gmixB", [128, D], F32)
            gglaB = fw.sb(st, "gglaB", [128, 128], F32)
            gq2 = fw.sb(st, "gq2", [128, 1], F32)
            gk2 = fw.sb(st, "gk2", [128, 1], F32)
            Umat = fw.sb(st, "Umat", [128, 128], F32)
            blk1 = fw.sb(st, "blk1", [128, 128], F32)
            win_v = w_in.t.ap().rearrange("o (kc p) n -> p (o kc) n", p=128)
            CH = 776
            for c0 in range(0, DIN, CH):
                fw.dma("pool", winb[:, :, c0:c0 + CH], win_v[:, :, c0:c0 + CH], winb, w_in)
            fw.dma("pool", wbrg[:], w_br_gla.t.ap().rearrange("o (kc p) n -> p (o kc) n", p=128), wbrg, w_br_gla)
            fw.dma("sp", wa2[0:16, :], w_alpha2.t.ap().rearrange("o k n -> (o k) n"), wa2, w_alpha2)
            fw.dma("sp", wa2[16:17, :], b_alpha.t.ap(), wa2, b_alpha)
            fw.dma("sp", gmixB[:], AP(g_mix, 0, [[0, 128], [1, D]]), gmixB, g_mix)
            fw.dma("sp", gglaB[:], AP(g_gla, 0, [[0, 128], [1, 128]]), gglaB, g_gla)
            for hh in range(2):
                fw.dma("sp", gq2[hh * 64:(hh + 1) * 64, :], AP(g_q, 0, [[1, 64], [1, 1]]), gq2, g_q)
                fw.dma("sp", gk2[hh * 64:(hh + 1) * 64, :], AP(g_k, 0, [[1, 64], [1, 1]]), gk2, g_k)
            ts("dve", gq2[:], gq2[:], 0.125, None, ALU.mult, None, [gq2], [gq2])
            ms("pool", Umat[:], 1.0, [Umat])
            fw.op("pool", [Umat], [Umat], lambda: nc.gpsimd.affine_select(
                out=Umat[:], in_=Umat[:], pattern=[[-1, 128]], compare_op=ALU.is_gt,
                fill=0.0, base=0, channel_multiplier=1))
            ms("pool", Umat[64:128, 0:64], 0.0, [Umat])
            ms("pool", blk1[:], 0.0, [blk1])
            ms("pool", blk1[0:64, 0:64], 1.0 / 64, [blk1])
            ms("pool", blk1[64:128, 64:128], 1.0 / 64, [blk1])

            if dbg == "A0":
                fw.barrier()
                return nc, fw, out
            xt = [fw.sb(st, "xt%d" % i, [128, D], F32) for i in range(2)]
            junk = fw.sb(st, "junk", [128, D], BF16)
            st1 = [fw.sb(st, "st1_%d" % i, [128, 4], F32) for i in range(2)]
            xn = [fw.sb(st, "xn%d" % i, [128, D], BF16) for i in range(2)]
            xnT = [fw.sb(st, "xnT%d" % i, [128, 8, 512], BF16) for i in range(2)]
            qAT = fw.sb(st, "qAT", [128, 2, 512], BF16)
            alrT = fw.sb(st, "alrT", [17, 512], F32)
            epa = fw.sb(st, "epa", [128, 8, 512], F32)
            sqf = fw.sb(st, "sqf", [128, 512], F32)
            rsf = fw.sb(st, "rsf", [128, 512], F32)
            stgb = [fw.sb(st, "stgb%d" % i, [128, 512], BF16) for i in range(3)]
            stgf = [fw.sb(st, "stgf%d" % i, [128, 512], F32) for i in range(3)]
            eL = fw.sb(st, "eL", [128, 256], F32)
            Lt = fw.sb(st, "Lt", [128, 256], F32)
            dcy = fw.sb(st, "dcy", [128, 256], F32)
            dec = fw.sb(st, "dec", [128, 4], F32)
            kdec = fw.sb(st, "kdec", [128, 256], BF16)
            vAb = fw.sb(st, "vAb", [128, 512], BF16)
            er = fw.sb(st, "er", [128, 512], F32)
            sr = fw.sb(st, "sr", [128, 512], F32)
            S = fw.sb(st, "S", [128, 2, 256], F32)
            Sb = fw.sb(st, "Sb", [128, 2, 256], BF16)
            ssq4 = fw.sb(st, "ssq4", [128, 4], F32)
            rs4 = fw.sb(st, "rs4", [128, 4], F32)
            og = fw.sb(st, "og", [128, 512], F32)
            of = fw.sb(st, "of", [128, 512], BF16)
            oT = fw.sb(st, "oT", [128, 4, 512], BF16)
            vbst = [fw.sb(st, "vbst%d" % i, [128, 8, 65], BF16) for i in range(2)]
            wist = [fw.sb(st, "wist%d" % i, [128, 8], F32) for i in range(2)]
            pT = fw.ps(st, "pT", [128, 8, 128], BF16)
            pB = [fw.ps(st, "pB%d" % i, [128, 512], F32) for i in range(2)]
            pC = [fw.ps(st, "pC%d" % i, [128, 512], F32) for i in range(2)]
            pX = fw.ps(st, "pX", [128, 512], F32)
            pU = fw.ps(st, "pU", [128, 512], F32)
            pO = fw.ps(st, "pO", [128, 512], F32)
            ms("dve", alrT[:], 1.0, [alrT])
            for i in range(2):
                ms("dve", vbst[i][:], 1.0, [vbst[i]])

            fm_specs = [("qA", C_QA + 128 * j, 128, j) for j in range(2)]
            fm_specs += [("alr", C_ALR, 16, 0)]
            fm_specs += [("qB", C_QB + 128 * j, 128, j) for j in range(4)]
            fm_specs += [("kB", C_KB + 128 * j, 128, j) for j in range(4)]
            fm_specs += [("qi", C_QI + 128 * j, 128, j) for j in range(2)]
            fm_specs += [("ki", C_KI, 32, 0)]
            fm_specs += [("gA", C_GA + 128 * j, 128, j) for j in range(8)]
            fm_specs += [("gB", C_GB + 128 * j, 128, j) for j in range(8)]
            bi = 0
            ci = 0
            sgi = 0
            for g in range(NST):
                tok0 = g * 512
                xs = xnT[g % 2]
                if tok0 % T == 0:
                    ms("pool", S[:], 0.0, [S])
                for t in range(4):
                    xi = xt[t % 2]
                    si = st1[t % 2]
                    xni = xn[t % 2]
                    r0 = tok0 + t * 128
                    fw.dma("sp", xi[:], x.t.ap()[r0:r0 + 128, :], xi, x)
                    act(junk[:], xi[:], AF.Square, [xi], [junk, si], accum_out=si[:, 0:1])
                    rstd_from(si[:, 0:1], si[:, 1:2], 1.0 / D, epsb, [si], [si])
                    stt("dve", xni[:], xi[:], si[:, 1:2], gmixB[:], ALU.mult, ALU.mult, [xi, si, gmixB], [xni])
                    for kc in range(8):
                        tr(pT[:, kc, :], xni[:, kc * 128:(kc + 1) * 128], identb[:], [xni, identb], [pT])
                    cp("act", xs[:, :, t * 128:(t + 1) * 128], pT[:], [pT], [xs])
                if dbg == "A1":
                    fw.barrier()
                    return nc, fw, out
                for (kind, c0, M, j) in fm_specs:
                    pb = pB[bi % 2]
                    bi += 1
                    for kc in range(8):
                        mm(pb[0:M, :], winb[:, kc, c0:c0 + M], xs[:, kc, :], kc == 0, kc == 7, [winb, xs], [pb])
                    if kind == "qA":
                        fw.op("act", [pb], [qAT], lambda: nc.scalar.mul(out=qAT[:, j, :], in_=pb[:], mul=0.125))
                    elif kind == "alr":
                        cp("act", alrT[0:16, :], pb[0:16, :], [pb], [alrT])
                    elif kind in ("qB", "kB"):
                        gsc = gq2 if kind == "qB" else gk2
                        dd = qbT_d if kind == "qB" else kbT_d
                        act(sqf[:], pb[:], AF.Square, [pb], [sqf])
                        mm(pX[:], blk1[:], sqf[:], True, True, [blk1, sqf], [pX])
                        act(rsf[:], pX[:], AF.Ln, [pX, epsb], [rsf], bias=epsb[:, 0:1])
                        act(rsf[:], rsf[:], AF.Exp, [rsf], [rsf], scale=-0.5)
                        sg = stgb[sgi % 3]
                        sgi += 1
                        stt("dve", sg[:], pb[:], gsc[:, 0:1], rsf[:], ALU.mult, ALU.mult, [pb, gsc, rsf], [sg])
                        fw.dma("sp", dd.t.ap()[j * 128:(j + 1) * 128, tok0:tok0 + 512], sg[:], dd, sg)
                    elif kind == "qi":
                        sg = stgb[sgi % 3]
                        sgi += 1
                        cp("act", sg[:], pb[:], [pb], [sg])
                        fw.dma("sp", qiT_d.t.ap()[j * 128:(j + 1) * 128, tok0:tok0 + 512], sg[:], qiT_d, sg)
                    elif kind == "ki":
                        sg = stgb[sgi % 3]
                        sgi += 1
                        cp("act", sg[0:32, :], pb[0:32, :], [pb], [sg])
                        fw.dma("sp", kiT_d.t.ap()[:, tok0:tok0 + 512], sg[0:32, :], kiT_d, sg)
                    elif kind == "gA":
                        sigmoid_act(epa[:, j, :], pb[:], [pb], [epa])
                    elif kind == "gB":
                        sf = stgf[sgi % 3]
                        sgi += 1
                        sigmoid_act(sf[:], pb[:], [pb], [sf])
                        fw.dma("sp", epb_d.t.ap()[j * 128:(j + 1) * 128, tok0:tok0 + 512], sf[:], epb_d, sf)
                if dbg == "A2":
                    fw.barrier()
                    return nc, fw, out
                for t in range(4):
                    cs = slice(t * 128, (t + 1) * 128)
                    r0 = tok0 + t * 128

                    def tokmm(c0, N):
                        nonlocal ci
                        pc = pC[ci % 2]
                        ci += 1
                        for kc in range(8):
                            mm(pc[:, 0:N], xs[:, kc, cs], winb[:, kc, c0:c0 + N], kc == 0, kc == 7, [xs, winb], [pc])
                        return pc
                    pc = tokmm(C_VB, 512)
                    vs_ = vbst[t % 2]
                    cp("act", vs_[:, :, 0:64], pc[:].rearrange("p (h d) -> p h d", h=8), [pc], [vs_])
                    fw.dma("sp", vb_d.t.ap()[r0:r0 + 128, :], vs_[:].rearrange("p h d -> p (h d)"), vb_d, vs_)
                    pc = tokmm(C_WI, 8)
                    ws_ = wist[t % 2]
                    fw.op("act", [pc], [ws_], lambda: nc.scalar.mul(out=ws_[:], in_=pc[:, 0:8], mul=1.0 / 16))
                    fw.dma("sp", wi_d.t.ap()[r0:r0 + 128, :], ws_[:], wi_d, ws_)
                    mm(pX[:, 0:256], alrT[0:17, cs], wa2[0:17, :], True, True, [alrT, wa2], [pX])
                    act(eL[:], pX[:, 0:256], AF.Exp, [pX], [eL], scale=-1.0)
                    act(Lt[:], eL[:], AF.Ln, [eL, oneb], [Lt], bias=oneb[:, 0:1])
                    mm(pX[:, 256:512], Umat[:], Lt[:], True, True, [Umat, Lt], [pX])
                    act(dcy[:], pX[:, 256:512], AF.Exp, [pX], [dcy], scale=-1.0 / 16)
                    for ch in range(2):
                        for pr in range(2):
                            mm(pU[:, 500 + ch * 2 + pr:501 + ch * 2 + pr], Lt[ch * 64:(ch + 1) * 64, pr * 128:(pr + 1) * 128],
                               onesf[ch * 64:(ch + 1) * 64, 0:1], True, True, [Lt, onesf], [pU])
                    act(dec[:], pU[:, 500:504], AF.Exp, [pU], [dec], scale=-1.0 / 16)
                    pc = tokmm(C_KA, 256)
                    tt("dve", kdec[:], pc[:, 0:256], dcy[:], ALU.mult, [pc, dcy], [kdec])
                    pc = tokmm(C_VA, 512)
                    cp("act", vAb[:], pc[:], [pc], [vAb])
                    pc = tokmm(C_RA, 512)
                    sigmoid_act(er[:], pc[:], [pc], [er])
                    tt("dve", sr[:], pc[:], er[:], ALU.mult, [pc, er], [sr])
                    for ch in range(2):
                        rows = slice(ch * 64, (ch + 1) * 64)
                        for pr in range(2):
                            mm(pU[:, 0:256], kdec[rows, pr * 128:(pr + 1) * 128], vAb[rows, pr * 256:(pr + 1) * 256],
                               True, True, [kdec, vAb], [pU])
                            for hh in range(2):
                                prow = slice(hh * 64, (hh + 1) * 64)
                                stt("dve", S[prow, pr, hh * 128:(hh + 1) * 128], S[prow, pr, hh * 128:(hh + 1) * 128],
                                    dec[prow, ch * 2 + pr:ch * 2 + pr + 1], pU[prow, hh * 128:(hh + 1) * 128],
                                    ALU.mult, ALU.add, [S, dec, pU], [S])
                        cp("dve", Sb[:], S[:], [S], [Sb])
                        for pr in range(2):
                            mm(pO[rows, pr * 256:(pr + 1) * 256], qAT[:, pr, t * 128 + ch * 64:t * 128 + (ch + 1) * 64],
                               Sb[:, pr, :], True, True, [qAT, Sb], [pO])
                    act(og[:], pO[:], AF.Square, [pO], [og])
                    fw.op("dve", [og], [ssq4], lambda: nc.vector.tensor_reduce(
                        out=ssq4[:], in_=og[:].rearrange("p (h d) -> p h d", h=4), axis=AX.X, op=ALU.add))
                    rstd_from(ssq4[:], rs4[:], 1.0 / 128, epsb, [ssq4], [rs4])
                    for h in range(4):
                        stt("dve", og[:, h * 128:(h + 1) * 128], pO[:, h * 128:(h + 1) * 128], rs4[:, h:h + 1],
                            gglaB[:], ALU.mult, ALU.mult, [pO, rs4, gglaB], [og])
                    tt("dve", of[:], og[:], sr[:], ALU.mult, [og, sr], [of])
                    for h in range(4):
                        tr(pT[:, h, :], of[:, h * 128:(h + 1) * 128], identb[:], [of, identb], [pT])
                    cp("act", oT[:, :, cs], pT[:, 0:4, :], [pT], [oT])
                if dbg == "A3":
                    fw.barrier()
                    return nc, fw, out
                for oc in range(8):
                    pb = pB[bi % 2]
                    bi += 1
                    for kc in range(4):
                        mm(pb[:], wbrg[:, kc, oc * 128:(oc + 1) * 128], oT[:, kc, :], kc == 0, kc == 3, [wbrg, oT], [pb])
                    sf = stgf[sgi % 3]
                    sgi += 1
                    tt("dve", sf[:], pb[:], epa[:, oc, :], ALU.mult, [pb, epa], [sf])
                    fw.dma("sp", mixA_d.t.ap()[oc * 128:(oc + 1) * 128, tok0:tok0 + 512], sf[:], mixA_d, sf)
            fw.barrier()

        if dbg == "A":
            return nc, fw, out

        ybT_d = fw.dram("ybT_d", [512, NTOK], BF16)
        with ExitStack() as st:
            NBLK = T // 128
            rb15B = fw.sb(st, "rb15B", [128, 8], F32)
            hkb = fw.sb(st, "hkb", [128, 2, 8, 128], BF16)
            pow2 = fw.sb(st, "pow2", [128, NBIS + 2], F32)
            kbT_t = fw.sb(st, "kbT", [128, 4, T], BF16)
            vbs_t = fw.sb(st, "vbs", [128, NBLK, 520], BF16)
            ki3_t = fw.sb(st, "ki3", [96, T], BF16)
            kbTJ = [Buf("kbT%d" % j, kbT_t.t) for j in range(STS)]
            vbsJ = [Buf("vbs%d" % j, vbs_t.t) for j in range(STS)]
            ki3J = [Buf("ki3%d" % j, ki3_t.t) for j in range(STS)]
            kbT, vbs, ki3 = kbT_t, vbs_t, ki3_t
            qbs2 = [fw.sb(st, "qbs%d" % i, [128, 4, 512], BF16) for i in range(2)]
            qi32 = [fw.sb(st, "qi3%d" % i, [96, 3, 512], BF16) for i in range(2)]
            wis2 = [fw.sb(st, "wis%d" % i, [128, 4, 8], F32) for i in range(2)]
            maskT2 = [fw.sb(st, "maskT%d" % i, [128, NBLK, 512], BF16) for i in range(2)]
            diag = fw.sb(st, "diag", [128, 8, 128], BF16)
            Rb = [fw.sb(st, "Rb%d" % i, [128, 512], BF16) for i in range(2)]
            score2 = [fw.sb(st, "score%d" % i, [128, T], F32) for i in range(1)]
            maskq2 = [fw.sb(st, "maskq%d" % i, [128, T], BF16) for i in range(1)]
            bs = fw.sb(st, "bs", [128, 8], F32)
            wcols = fw.sb(st, "wcols", [128, NBIS + 2], F32)
            pex = [fw.sb(st, "pex%d" % i, [128, 512], BF16) for i in range(4)]
            ptm = [fw.sb(st, "ptm%d" % i, [128, 512], BF16) for i in range(4)]
            rec = fw.sb(st, "rec", [128, 4], F32)
            ybq = fw.sb(st, "ybq", [128, 4, 512], BF16)
            ybT = fw.sb(st, "ybT", [128, 4, 512], BF16)
            pS = [fw.ps(st, "pS%d" % i, [128, 512], F32) for i in range(2)]
            pSc = fw.ps(st, "pSc", [128, 512], F32)
            pT = fw.ps(st, "pTb", [128, 8, 128], BF16)
            pQK = [fw.ps(st, "pQK%d" % i, [128, 512], F32) for i in range(2)]
            pPV = [fw.ps(st, "pPV%d" % i, [128, 512], F32) for i in range(2)]

            with ExitStack() as st0:
                rbT = fw.sb(st0, "rbT", [32, 8], F32)
                e1s = fw.sb(st0, "e1s", [32, 512], F32)
                rb15c = fw.sb(st0, "rb15c", [8, 1], F32)
                phis = fw.sb(st0, "phis", [8, 512], F32)
                hk1 = fw.sb(st0, "hk1", [128, 128], F32)
                fw.dma("sp", rbT[:], rel_bias.t.ap(), rbT, rel_bias)
                fw.dma("sp", e1s[:], e1h.t.ap(), e1s, e1h)
                fw.dma("sp", rb15c[:], AP(rel_bias, 15 * 8, [[1, 8], [1, 1]]), rb15c, rel_bias)
                fw.dma("sp", rb15B[:], AP(rel_bias, 15 * 8, [[0, 128], [1, 8]]), rb15B, rel_bias)
                mm(pSc[0:8, :], rbT[:], e1s[:], True, True, [rbT, e1s], [pSc])
                ts("dve", phis[:], pSc[0:8, :], rb15c[:, 0:1], None, ALU.subtract, None, [pSc, rb15c], [phis])
                fw.dma("sp", phi_d.t.ap(), phis[:], phi_d, phis)
                for vi in range(2):
                    for h in range(8):
                        fw.dma("sp", hk1[:], AP(phi_d, h * 512 + vi * 256, [[1, 128], [1, 128]]), hk1, phi_d)
                        cp("dve", hkb[:, vi, h, :], hk1[:], [hk1], [hkb])
                for i in range(NBIS + 2):
                    ms("pool", pow2[:, i:i + 1], 2.0 ** (-i), [pow2])
                fw.barrier()

            cntr = {"qk": 0, "pv": 0, "bank": 0}
            bank4 = pS + pQK

            def next_bank():
                b = bank4[cntr["bank"] % 4]
                cntr["bank"] += 1
                return b

            def geo(g):
                tok0 = g * 512
                return tok0, (tok0 % T) // 512

            def loads(g):
                tok0, J = geo(g)
                qbs, qi3, wis = qbs2[g % 2], qi32[g % 2], wis2[g % 2]
                for c in range(4):
                    fw.dma("sp", kbT[:, c, J * 512:(J + 1) * 512], kbT_d.t.ap()[c * 128:(c + 1) * 128, tok0:tok0 + 512], kbTJ[J], kbT_d)
                    fw.dma("sp", qbs[:, c, :], qbT_d.t.ap()[c * 128:(c + 1) * 128, tok0:tok0 + 512], qbs, qbT_d)
                fw.dma("sp", vbs[:, 4 * J:4 * J + 4, :], vb_d.t.ap()[tok0:tok0 + 512, :].rearrange("(t p) c -> p t c", p=128), vbsJ[J], vb_d)
                for r in range(3):
                    fw.dma("sp", ki3[r * 32:(r + 1) * 32, J * 512:(J + 1) * 512], kiT_d.t.ap()[:, tok0:tok0 + 512], ki3J[J], kiT_d)
                for h in range(8):
                    fw.dma("sp", qi3[(h % 3) * 32:(h % 3) * 32 + 32, h // 3, :], qiT_d.t.ap()[h * 32:(h + 1) * 32, tok0:tok0 + 512], qi3, qiT_d)
                fw.dma("sp", wis[:], wi_d.t.ap()[tok0:tok0 + 512, :].rearrange("(t p) h -> p t h", p=128), wis, wi_d)

            def mask_chunks(g):
                tok0, J = geo(g)
                qi3, wis, maskT = qi32[g % 2], wis2[g % 2], maskT2[g % 2]

                def bis_iter(i, nk, score, maskq):
                    ts("dve", maskq[:, 0:nk], score[:, 0:nk], bs[:, 3:4], 0.0, ALU.is_ge, ALU.add, [score, bs], [maskq, bs],
                       accum_out=bs[:, 4:5])
                    ts("dve", bs[:, 5:6], bs[:, 4:5], KTOP - 0.5, 0.5, ALU.is_ge, ALU.subtract, [bs], [bs])
                    if i < NBIS:
                        stt("dve", bs[:, 3:4], bs[:, 5:6], wcols[:, i:i + 1], bs[:, 3:4], ALU.mult, ALU.add, [bs, wcols], [bs])

                def part1(t):
                    nk = (4 * J + t + 1) * 128
                    score, maskq = score2[0], maskq2[0]
                    for h in range(8):
                        ts("dve", diag[:, h, :], identb[:], wis[:, t, h:h + 1], None, ALU.mult, None, [identb, wis], [diag])
                    for s0 in range(0, nk, 512):
                        ns = min(512, nk - s0)
                        kr = [ki3J[jj] for jj in range(s0 // 512, (s0 + ns - 1) // 512 + 1)]

                        def s_mm(h):
                            ps_ = next_bank()
                            rb_ = Rb[h % 2]
                            r0 = (h % 3) * 32
                            mm(ps_[:, 0:ns], qi3[r0:r0 + 32, h // 3, t * 128:(t + 1) * 128], ki3[r0:r0 + 32, s0:s0 + ns],
                               True, True, [qi3] + kr, [ps_])
                            act(rb_[:, 0:ns], ps_[:, 0:ns], AF.Relu, [ps_], [rb_])
                        s_mm(0)
                        for h in range(8):
                            mm(pSc[:, 0:ns], diag[:, h, :], Rb[h % 2][:, 0:ns], h == 0, h == 7, [diag, Rb[h % 2]], [pSc])
                            if h + 1 < 8:
                                s_mm(h + 1)
                        cp("act", score[:, s0:s0 + ns], pSc[:, 0:ns], [pSc], [score])
                    fw.op("dve", [score], [bs], lambda: nc.vector.tensor_reduce(out=bs[:, 0:1], in_=score[:, 0:nk], axis=AX.X, op=ALU.min))
                    fw.op("dve", [score], [bs], lambda: nc.vector.tensor_reduce(out=bs[:, 1:2], in_=score[:, 0:nk], axis=AX.X, op=ALU.max))
                    ms("dve", score[0:64, nk - 64:nk], NEG, [score])
                    tt("dve", bs[:, 2:3], bs[:, 1:2], bs[:, 0:1], ALU.subtract, [bs], [bs])
                    ts("dve", wcols[:], pow2[:], bs[:, 2:3], None, ALU.mult, None, [pow2, bs], [wcols])
                    tt("dve", bs[:, 3:4], bs[:, 0:1], wcols[:, 1:2], ALU.add, [bs, wcols], [bs])
                    for i in range(1, NBIS // 2 + 1):
                        bis_iter(i, nk, score, maskq)

                def part2(t):
                    nk = (4 * J + t + 1) * 128
                    score, maskq = score2[0], maskq2[0]
                    for i in range(NBIS // 2 + 1, NBIS + 1):
                        bis_iter(i, nk, score, maskq)
                    stt("dve", bs[:, 6:7], bs[:, 5:6], wcols[:, NBIS:NBIS + 1], bs[:, 3:4], ALU.mult, ALU.add, [bs, wcols], [bs])
                    tt("dve", bs[:, 6:7], bs[:, 6:7], wcols[:, NBIS + 1:NBIS + 2], ALU.subtract, [bs, wcols], [bs])
                    ts("dve", maskq[:, 0:nk], score[:, 0:nk], bs[:, 6:7], None, ALU.is_ge, None, [score, bs], [maskq])

                def transp(t):
                    nk = (4 * J + t + 1) * 128
                    maskq = maskq2[0]
                    nb = nk // 128
                    for a0 in range(0, nb, 8):
                        na = min(8, nb - a0)
                        for a in range(a0, a0 + na):
                            tr(pT[:, a - a0, :], maskq[:, a * 128:(a + 1) * 128], identb[:], [maskq, identb], [pT])
                        cp("act", maskT[:, a0:a0 + na, t * 128:(t + 1) * 128], pT[:, 0:na, :], [pT], [maskT])

                pre = [lambda: part1(0), lambda: part2(0), lambda: part1(1), lambda: part2(1),
                       lambda: part1(2), lambda: part2(2), lambda: part1(3), lambda: part2(3)]
                post = [None, lambda: transp(0), None, lambda: transp(1), None, lambda: transp(2), None, None]
                return pre, post, (lambda: transp(3))

            def attention_head(g, h):
                tok0, J = geo(g)
                qbs, maskT = qbs2[g % 2], maskT2[g % 2]
                c = h // 2
                pb_ = (h % 2) * 64
                ppv = pPV[cntr["pv"] % 2]
                cntr["pv"] += 1
                na_tot = 4 * J + 4

                def qk_stage(a):
                    u = a - 4 * J
                    q0 = max(0, u) * 128
                    nq = 512 - q0
                    qki = cntr["qk"]
                    pq = next_bank()
                    pe_ = pex[qki % 4]
                    pt_ = ptm[qki % 4]
                    cntr["qk"] += 1
                    near = [(tq, a - 4 * J - tq) for tq in range(q0 // 128, 4) if (a - 4 * J - tq) in (-1, 0)]
                    mm(pq[:, 0:nq], kbT[pb_:pb_ + 64, c, a * 128:(a + 1) * 128], qbs[pb_:pb_ + 64, c, q0:512],
                       True, len(near) == 0, [kbTJ[a // 4], qbs], [pq])
                    for ni, (tq, v) in enumerate(near):
                        mm(pq[:, tq * 128 - q0:(tq + 1) * 128 - q0], antib[:], hkb[:, v + 1, h, :],
                           False, ni == len(near) - 1, [antib, hkb], [pq])
                    act(pe_[:, 0:nq], pq[:, 0:nq], AF.Exp, [pq, rb15B], [pe_], bias=rb15B[:, h:h + 1])
                    tt("pool", pt_[:, 0:nq], pe_[:, 0:nq], maskT[:, a, q0:512], ALU.mult, [pe_, maskT], [pt_])
                    return (a, q0, pt_)

                def pv_stage(st_):
                    a, q0, pt_ = st_
                    for tq in range(q0 // 128, 4):
                        fw.op("pe", [pt_, vbsJ[a // 4]], [ppv], lambda: nc.tensor.matmul(
                            ppv[:, tq * 65:(tq + 1) * 65], lhsT=pt_[:, tq * 128 - q0:(tq + 1) * 128 - q0], rhs=vbs[:, a, h * 65:(h + 1) * 65],
                            start=(a == 0 and tq == 0), stop=(a == 4 * J + tq), skip_group_check=True))
                pend = []
                for a in range(na_tot):
                    pend.append(qk_stage(a))
                    if len(pend) > 2:
                        pv_stage(pend.pop(0))
                while pend:
                    pv_stage(pend.pop(0))
                act(rec[:], ppv[:, 0:260].rearrange("p (t c) -> p t c", c=65)[:, :, 64], AF.Ln, [ppv], [rec])
                act(rec[:], rec[:], AF.Exp, [rec], [rec], scale=-1.0)
                for tq in range(4):
                    fw.op("act", [ppv, rec], [ybq], lambda: nc.scalar.mul(
                        out=ybq[:, tq, h * 64:(h + 1) * 64], in_=ppv[:, tq * 65:tq * 65 + 64], mul=rec[:, tq:tq + 1]))

            def finish(g):
                tok0, J = geo(g)
                for tq in range(4):
                    for c in range(4):
                        tr(pT[:, c, :], ybq[:, tq, c * 128:(c + 1) * 128], identb[:], [ybq, identb], [pT])
                    cp("act", ybT[:, :, tq * 128:(tq + 1) * 128], pT[:, 0:4, :], [pT], [ybT])
                for c in range(4):
                    fw.dma("sp", ybT_d.t.ap()[c * 128:(c + 1) * 128, tok0:tok0 + 512], ybT[:, c, :], ybT_d, ybT)

            def run_masks(g):
                pre, post, tail = mask_chunks(g)
                for h in range(8):
                    if pre[h] is not None:
                        pre[h]()
                    if post[h] is not None:
                        post[h]()
                tail()

            loads(0)
            run_masks(0)
            for g in range(NST):
                pre, post, tail = None, None, None
                defer = False
                if g + 1 < NST:
                    if geo(g + 1)[1] == 0:
                        defer = True
                    else:
                        loads(g + 1)
                        pre, post, tail = mask_chunks(g + 1)
                for h in range(8):
                    if pre is not None and pre[h] is not None:
                        pre[h]()
                    attention_head(g, h)
                    if post is not None and post[h] is not None:
                        post[h]()
                finish(g)
                if tail is not None:
                    tail()
                if defer:
                    loads(g + 1)
                    run_masks(g + 1)
            fw.barrier()

        with ExitStack() as st:
            wbra = fw.sb(st, "wbra", [128, 4, D], BF16)
            woutb = fw.sb(st, "woutb", [128, 8, D], BF16)
            fw.dma("pool", wbra[:], w_br_att.t.ap().rearrange("o (kc p) n -> p (o kc) n", p=128), wbra, w_br_att)
            fw.dma("pool", woutb[:], w_out.t.ap().rearrange("o (kc p) n -> p (o kc) n", p=128), woutb, w_out)
            ybl = [fw.sb(st, "ybl%d" % i, [128, 4, 512], BF16) for i in range(2)]
            epl = [fw.sb(st, "epl%d" % i, [128, 512], F32) for i in range(3)]
            mal = [fw.sb(st, "mal%d" % i, [128, 512], F32) for i in range(3)]
            t1f = [fw.sb(st, "t1f%d" % i, [128, 512], F32) for i in range(2)]
            mixT2 = [fw.sb(st, "mixT%d" % i, [128, 8, 512], BF16) for i in range(2)]
            xr = [fw.sb(st, "xr%d" % i, [128, D], F32) for i in range(3)]
            pQ2 = [fw.ps(st, "pQ2%d" % i, [128, 512], F32) for i in range(3)]
            pS2 = [fw.ps(st, "pS2%d" % i, [128, 512], F32) for i in range(4)]
            k3 = 0
            k4 = 0
            kx = 0
            for g in range(NST):
                tok0 = g * 512
                yb_ = ybl[g % 2]
                mixT = mixT2[g % 2]
                for c in range(4):
                    fw.dma("sp", yb_[:, c, :], ybT_d.t.ap()[c * 128:(c + 1) * 128, tok0:tok0 + 512], yb_, ybT_d)
                for oc in range(8):
                    pq = pQ2[k3 % 3]
                    ep_ = epl[k3 % 3]
                    ma_ = mal[k3 % 3]
                    tf_ = t1f[k3 % 2]
                    k3 += 1
                    fw.dma("sp", ep_[:], epb_d.t.ap()[oc * 128:(oc + 1) * 128, tok0:tok0 + 512], ep_, epb_d)
                    fw.dma("sp", ma_[:], mixA_d.t.ap()[oc * 128:(oc + 1) * 128, tok0:tok0 + 512], ma_, mixA_d)
                    for kc in range(4):
                        mm(pq[:], wbra[:, kc, oc * 128:(oc + 1) * 128], yb_[:, kc, :], kc == 0, kc == 3, [wbra, yb_], [pq])
                    tt("dve", tf_[:], pq[:], ep_[:], ALU.mult, [pq, ep_], [tf_])
                    tt("pool", mixT[:, oc, :], tf_[:], ma_[:], ALU.add, [tf_, ma_], [mixT])
                for tq in range(4):
                    r0 = tok0 + tq * 128
                    xi = xr[kx % 3]
                    kx += 1
                    fw.dma("sp", xi[:], x.t.ap()[r0:r0 + 128, :], xi, x)
                    for hf in range(2):
                        ps_ = pS2[k4 % 4]
                        k4 += 1
                        for kc in range(8):
                            mm(ps_[:], mixT[:, kc, tq * 128:(tq + 1) * 128], woutb[:, kc, hf * 512:(hf + 1) * 512],
                               kc == 0, kc == 7, [mixT, woutb], [ps_])
                        tt("dve", xi[:, hf * 512:(hf + 1) * 512], ps_[:], xi[:, hf * 512:(hf + 1) * 512], ALU.add, [ps_, xi], [xi])
                    fw.dma("sp", out.t.ap()[r0:r0 + 128, :], xi[:], out, xi)
            fw.barrier()

        if dbg == "B":
            return nc, fw, out

        with ExitStack() as st:
            NH = TB // 512
            NTB = TB // 128
            wdb = fw.sb(st, "wdb", [128, NEXP, D], BF16)
            for e0 in range(0, NEXP, 4):
                fw.dma("pool", wdb[:, e0:e0 + 4, :], w_down.t.ap().rearrange("o e k n -> k (o e) n")[:, e0:e0 + 4, :], wdb, w_down)
            gffB = fw.sb(st, "gffB", [128, D], F32)
            fw.dma("sp", gffB[:], AP(g_ffn, 0, [[0, 128], [1, D]]), gffB, g_ffn)
            wr = fw.sb(st, "wr", [128, 8, 36], F32)
            fw.dma("sp", wr[:, :, 0:4], w_rg.t.ap().rearrange("o (kc p) n -> p (o kc) n", p=128), wr, w_rg)
            fw.dma("sp", wr[:, :, 4:36], w_re.t.ap().rearrange("o (kc p) n -> p (o kc) n", p=128), wr, w_re)
            br = fw.sb(st, "br", [1, 36], F32)
            fw.dma("sp", br[:, 0:4], b_rg.t.ap(), br, b_rg)
            fw.dma("sp", br[:, 4:36], b_re.t.ap(), br, b_re)
            selA = fw.sb(st, "selA", [64, NEXP, 128], BF16)
            ms("pool", selA[:], 0.0, [selA])
            for base in (0, -32):
                fw.op("pool", [selA], [selA], lambda: nc.gpsimd.affine_select(
                    out=selA[:], in_=selA[:], pattern=[[-1, NEXP], [0, 128]], compare_op=ALU.not_equal,
                    fill=1.0, base=base, channel_multiplier=1))
            xt = [fw.sb(st, "xtc%d" % i, [128, D], F32) for i in range(2)]
            junk = fw.sb(st, "junkc", [128, D], BF16)
            st1 = [fw.sb(st, "st1c%d" % i, [128, 4], F32) for i in range(2)]
            xnf = [fw.sb(st, "xnf%d" % i, [128, D], F32) for i in range(1)]
            xfT = fw.sb(st, "xfT", [128, 8, 128], F32)
            xn2T = fw.sb(st, "xn2T", [128, 8, TB], BF16)
            lg = fw.sb(st, "lg", [128, 36], F32)
            rt = fw.sb(st, "rt", [128, 16], F32)
            ohg = fw.sb(st, "ohg", [128, 4], F32)
            esel = fw.sb(st, "esel", [128, 8], F32)
            top8 = fw.sb(st, "top8", [128, 8], F32)
            oh1 = fw.sb(st, "oh1", [128, 8], F32)
            oh2 = fw.sb(st, "oh2", [128, 8], F32)
            within = fw.sb(st, "within", [128, 8], F32)
            comb = fw.sb(st, "comb", [128, 32], F32)
            c2b = fw.sb(st, "c2b", [128, 32], BF16)
            c2f = fw.sb(st, "c2f", [128, 64], F32)
            cT2 = fw.sb(st, "cT2", [64, TB], BF16)
            wg = [fw.sb(st, "wg%d" % i, [128, 8, 128], BF16) for i in range(3)]
            wu = [fw.sb(st, "wu%d" % i, [128, 8, 128], BF16) for i in range(3)]
            eg = [fw.sb(st, "eg%d" % i, [128, 512], F32) for i in range(2)]
            t1 = [fw.sb(st, "t1_%d" % i, [128, 512], F32) for i in range(2)]
            hT = fw.sb(st, "hT", [128, NEXP, TB], BF16)
            x2l = [fw.sb(st, "x2l%d" % i, [128, D], F32) for i in range(1)]
            pTf = fw.ps(st, "pTf", [128, 4, 128], F32)
            pR = fw.ps(st, "pR", [128, 512], F32)
            pG = [fw.ps(st, "pG%d" % i, [128, 512], F32) for i in range(2)]
            pU2 = [fw.ps(st, "pU2%d" % i, [128, 512], F32) for i in range(2)]
            pBC = fw.ps(st, "pBC", [128, 512], F32)
            pD = fw.ps(st, "pD", [128, 512], F32)
            wgv = w_gate.t.ap().rearrange("o e (kc p) n -> (o e) p kc n", p=128)
            wuv = w_up.t.ap().rearrange("o e (kc p) n -> (o e) p kc n", p=128)
            if dbg == "C0":
                fw.barrier()
                return nc, fw, out
            wi_ = 0
            for blk in range(NTOK // TB):
                b0 = blk * TB
                for t in range(NTB):
                    xi = xt[t % 2]
                    si = st1[t % 2]
                    xf = xnf[0]
                    r0 = b0 + t * 128
                    cs = slice(t * 128, (t + 1) * 128)
                    fw.dma("sp", xi[:], out.t.ap()[r0:r0 + 128, :], xi, out)
                    act(junk[:], xi[:], AF.Square, [xi], [junk, si], accum_out=si[:, 0:1])
                    rstd_from(si[:, 0:1], si[:, 1:2], 1.0 / D, epsb, [si], [si])
                    stt("dve", xf[:], xi[:], si[:, 1:2], gffB[:], ALU.mult, ALU.mult, [xi, si, gffB], [xf])
                    for half in range(2):
                        for k4 in range(4):
                            kc = half * 4 + k4
                            tr(pTf[:, k4, :], xf[:, kc * 128:(kc + 1) * 128], identf[:], [xf, identf], [pTf])
                        cp("act", xfT[:, half * 4:half * 4 + 4, :], pTf[:], [pTf], [xfT])
                        cp("dve", xn2T[:, half * 4:half * 4 + 4, cs], pTf[:], [pTf], [xn2T])
                    for kc in range(8):
                        mm(pR[:, 0:36], xfT[:, kc, :], wr[:, kc, :], kc == 0, False, [xfT, wr], [pR])
                    mm(pR[:, 0:36], onesf[0:1, :], br[0:1, :], False, True, [onesf, br], [pR])
                    cp("act", lg[:], pR[:, 0:36], [pR], [lg])
                    fw.op("dve", [lg], [rt], lambda: nc.vector.tensor_reduce(out=rt[:, 0:1], in_=lg[:, 0:4], axis=AX.X, op=ALU.max))
                    ts("dve", rt[:, 1:2], rt[:, 0:1], -1.0, None, ALU.mult, None, [rt], [rt])
                    act(ohg[:], lg[:, 0:4], AF.Exp, [lg, rt], [ohg, rt], bias=rt[:, 1:2], accum_out=rt[:, 2:3])
                    rcp(rt[:, 3:4], rt[:, 2:3], [rt], [rt])
                    ts("dve", ohg[:], lg[:, 0:4], rt[:, 0:1], None, ALU.is_ge, None, [lg, rt], [ohg])
                    ts("dve", esel[:], lg[:, 4:12], ohg[:, 0:1], None, ALU.mult, None, [lg, ohg], [esel])
                    for gi in range(1, 4):
                        stt("dve", esel[:], lg[:, 4 + 8 * gi:12 + 8 * gi], ohg[:, gi:gi + 1], esel[:], ALU.mult, ALU.add,
                            [lg, ohg, esel], [esel])
                    fw.op("dve", [esel], [top8], lambda: nc.vector.max(out=top8[:], in_=esel[:]))
                    ts("dve", oh1[:], esel[:], top8[:, 0:1], None, ALU.is_equal, None, [esel, top8], [oh1])
                    ts("dve", oh2[:], esel[:], top8[:, 1:2], None, ALU.is_equal, None, [esel, top8], [oh2])
                    tt("dve", rt[:, 4:5], top8[:, 1:2], top8[:, 0:1], ALU.subtract, [top8], [rt])
                    act(rt[:, 5:6], rt[:, 4:5], AF.Exp, [rt], [rt])
                    ts("dve", rt[:, 5:6], rt[:, 5:6], 1.0, None, ALU.add, None, [rt], [rt])
                    rcp(rt[:, 6:7], rt[:, 5:6], [rt], [rt])
                    tt("dve", rt[:, 7:8], rt[:, 6:7], rt[:, 3:4], ALU.mult, [rt], [rt])
                    tt("dve", rt[:, 8:9], rt[:, 3:4], rt[:, 7:8], ALU.subtract, [rt], [rt])
                    ts("dve", within[:], oh1[:], rt[:, 7:8], None, ALU.mult, None, [oh1, rt], [within])
                    stt("dve", within[:], oh2[:], rt[:, 8:9], within[:], ALU.mult, ALU.add, [oh2, rt, within], [within])
                    for gi in range(4):
                        ts("dve", comb[:, gi * 8:(gi + 1) * 8], within[:], ohg[:, gi:gi + 1], None, ALU.mult, None,
                           [within, ohg], [comb])
                    cp("dve", c2b[:], comb[:], [comb], [c2b])
                    cp("dve", c2f[:, 0:32], c2b[:], [c2b], [c2f])
                    tt("dve", c2f[:, 32:64], comb[:], c2f[:, 0:32], ALU.subtract, [comb, c2f], [c2f])
                    tr(pTf[0:64, 0, :], c2f[:, :], identf[:], [c2f, identf], [pTf])
                    cp("act", cT2[:, cs], pTf[0:64, 0, :], [pTf], [cT2])
                if dbg == "C1":
                    fw.barrier()
                    return nc, fw, out
                for e in range(NEXP):
                    wg_ = wg[wi_ % 3]
                    wu_ = wu[wi_ % 3]
                    wi_ += 1
                    fw.dma("pool", wg_[:], wgv[e], wg_, w_gate)
                    fw.dma("pool", wu_[:], wuv[e], wu_, w_up)
                    for half in range(NH):
                        hs = slice(half * 512, (half + 1) * 512)
                        pg_ = pG[(e * NH + half) % 2]
                        pu_ = pU2[(e * NH + half) % 2]
                        eg_ = eg[(e * NH + half) % 2]
                        t1_ = t1[(e * NH + half) % 2]
                        for kc in range(8):
                            mm(pg_[:], wg_[:, kc, :], xn2T[:, kc, hs], kc == 0, kc == 7, [wg_, xn2T], [pg_])
                        for kc in range(8):
                            mm(pu_[:], wu_[:, kc, :], xn2T[:, kc, hs], kc == 0, kc == 7, [wu_, xn2T], [pu_])
                        pbc_ = pBC if (e * NH + half) % 2 == 0 else pD
                        mm(pbc_[:], selA[:, e, :], cT2[:, hs], True, True, [selA, cT2], [pbc_])
                        act(eg_[:], pg_[:], AF.Silu, [pg_], [eg_])
                        tt("dve", t1_[:], eg_[:], pu_[:], ALU.mult, [eg_, pu_], [t1_])
                        tt("dve", hT[:, e, hs], t1_[:], pbc_[:], ALU.mult, [t1_, pbc_], [hT])
                if dbg == "C2":
                    fw.barrier()
                    return nc, fw, out
                for t in range(NTB):
                    r0 = b0 + t * 128
                    xi = x2l[0]
                    fw.dma("sp", xi[:], out.t.ap()[r0:r0 + 128, :], xi, out)
                    pacc = (pD, pBC)
                    for e in range(NEXP):
                        for hf in range(2):
                            mm(pacc[hf][:], hT[:, e, t * 128:(t + 1) * 128], wdb[:, e, hf * 512:(hf + 1) * 512], e == 0, e == NEXP - 1,
                               [hT, wdb], [pacc[hf]])
                    for hf in range(2):
                        tt("dve", xi[:, hf * 512:(hf + 1) * 512], pacc[hf][:], xi[:, hf * 512:(hf + 1) * 512], ALU.add, [pacc[hf], xi], [xi])
                    fw.dma("sp", out.t.ap()[r0:r0 + 128, :], xi[:], out, xi)
            fw.barrier()
    return nc, fw, out


_E1H = None


def _inputs_for_core(inputs, c, NSEQ, T):
    global _E1H
    if _E1H is None:
        _E1H = _bias_onehot()
    m = {}
    xs = np.ascontiguousarray(inputs["x"][c * NSEQ:(c + 1) * NSEQ]).reshape(NSEQ * T, D)
    m["x"] = xs
    for k, v in inputs.items():
        if k == "x":
            continue
        m[k] = np.ascontiguousarray(np.asarray(v, dtype=np.float32))
    m["e1h"] = _E1H
    return m


def kernel(**inputs):
    x = np.asarray(inputs["x"])
    B, T, _ = x.shape
    NSEQ = B // 8
    _, fw0, _ = build(NSEQ, T)
    nc, fw, out = build(NSEQ, T, needed=fw0.used)
    in_maps = [_inputs_for_core(inputs, c, NSEQ, T) for c in range(8)]
    res = run_bass_kernel_spmd(nc, in_maps, core_ids=list(range(8)))
    outs = [np.asarray(res.results[c]["out"]).reshape(NSEQ, T, D) for c in range(8)]
    return np.concatenate(outs, axis=0).astype(np.float32)
```

```python
import math
from contextlib import ExitStack
import numpy as np
import concourse.bass as bass
import concourse.mybir as mybir
from concourse.bass_utils import run_bass_kernel_spmd

F32 = mybir.dt.float32
BF16 = mybir.dt.bfloat16
ALU = mybir.AluOpType
AF = mybir.ActivationFunctionType
AX = mybir.AxisListType

D = 1024
DIN = 5432
NEXP = 32
EPS = 1e-6
C_QA, C_KA, C_VA, C_RA, C_ALR, C_QB, C_KB, C_VB, C_QI, C_KI, C_WI, C_GA, C_GB = (
    0, 256, 512, 1024, 1536, 1552, 2064, 2576, 3088, 3344, 3376, 3384, 4408)
NBIS = 16
NEG = -3.0e38


class Buf:
    __slots__ = ("name", "t", "w", "r", "dsem", "excl", "isdram")

    def __init__(self, name, t=None):
        self.name = name
        self.t = t
        self.w = {}
        self.r = {}
        self.dsem = None
        self.excl = False
        self.isdram = False

    def __getitem__(self, idx):
        return self.t[idx]


class FW:
    def __init__(self, nc, needed=None):
        self.nc = nc
        self.eng = {"pe": nc.tensor, "dve": nc.vector, "act": nc.scalar,
                    "pool": nc.gpsimd, "sp": nc.sync}
        self.sems = {}
        self.cnt = {}
        self.known = {}
        self.rank = {}
        self.rankof = {}
        self.needed = needed
        self.used = set()
        for k in self.eng:
            self.sems[k] = nc.alloc_semaphore("c_" + k)
            self.cnt[k] = 0
            self.rank[k] = 0
            self.known[k] = {}
        self.nd = 0
        self._dcount = {}
        self.uid = 0

    def sb(self, st, name, shape, dt):
        self.uid += 1
        return Buf(name, st.enter_context(self.nc.sbuf_tensor("%s_%d" % (name, self.uid), list(shape), dt)))

    def ps(self, st, name, shape, dt=F32):
        self.uid += 1
        b = Buf(name, st.enter_context(self.nc.psum_tensor("%s_%d" % (name, self.uid), list(shape), dt)))
        b.excl = True
        return b

    def dram(self, name, shape, dt, kind="Internal"):
        b = Buf(name, self.nc.dram_tensor(name, list(shape), dt, kind=kind))
        b.isdram = True
        return b

    def _dsem(self, b):
        if b.dsem is None:
            key = "d%d" % self.nd
            self.nd += 1
            self.sems[key] = self.nc.alloc_semaphore(key)
            b.dsem = key
        return b.dsem

    def _emit_wait(self, e, k, v):
        if k in self.cnt:
            self.used.add((k, v))
            val = v if self.needed is None else self.rankof[(k, v)]
        else:
            val = v
        self.eng[e].wait_ge(self.sems[k], val)

    def _waits(self, e, reads, writes, skipkey=None):
        need = {}
        for b in reads:
            for k, v in b.w.items():
                if need.get(k, 0) < v:
                    need[k] = v
        for b in writes:
            if b.isdram:
                continue
            for k, v in b.w.items():
                if k != skipkey and need.get(k, 0) < v:
                    need[k] = v
            for k, v in b.r.items():
                if need.get(k, 0) < v:
                    need[k] = v
        kn = self.known[e]
        for k, v in need.items():
            if kn.get(k, 0) >= v:
                continue
            if k == "pe" and e == "pe":
                continue
            self._emit_wait(e, k, v)
            kn[k] = v

    def _count(self, e, ins):
        self.cnt[e] += 1
        o = self.cnt[e]
        if self.needed is None:
            ins.then_inc(self.sems[e], 1)
        elif (e, o) in self.needed:
            ins.then_inc(self.sems[e], 1)
            self.rank[e] += 1
            self.rankof[(e, o)] = self.rank[e]
        return o

    def op(self, e, reads, writes, fn):
        if any(b.excl for b in reads):
            writes = list(writes) + [b for b in reads if b.excl and b not in writes]
            reads = [b for b in reads if not b.excl]
        self._waits(e, reads, writes)
        ins = fn()
        o = self._count(e, ins)
        for b in writes:
            b.w = {e: o}
            b.r = {}
        for b in reads:
            if b not in writes:
                b.r[e] = o
        return ins

    def dma(self, q, out_ap, in_ap, dst, src, **kw):
        owner = src if dst.isdram else dst
        key = self._dsem(owner)
        self._waits(q, [src], [dst], skipkey=key)
        ins = self.eng[q].dma_start(out=out_ap, in_=in_ap, **kw)
        cnt = self._dcount.get(key, 0) + 16
        self._dcount[key] = cnt
        ins.then_inc(self.sems[key], 16)
        if dst.isdram:
            dst.w[key] = cnt
        else:
            if key in dst.w and len(dst.w) == 1:
                dst.w[key] = cnt
            else:
                dst.w = {key: cnt}
            dst.r = {}
        src.r[key] = cnt
        return ins

    def barrier(self):
        sp = self.eng["sp"]
        kn = self.known["sp"]
        for k in list(self.sems.keys()):
            if k == "sp":
                continue
            v = self.cnt[k] if k in self.cnt else self._dcount.get(k, 0)
            if v > 0 and kn.get(k, 0) < v:
                self._emit_wait("sp", k, v)
                kn[k] = v
        ins = sp.nop()
        v = self._count("sp", ins)
        for e in ("pe", "dve", "act", "pool"):
            self._emit_wait(e, "sp", v)
            self.known[e]["sp"] = v
            for k, kv in kn.items():
                if self.known[e].get(k, 0) < kv:
                    self.known[e][k] = kv


def _bucket(rel):
    n = abs(rel)
    if n < 8:
        v = n
    else:
        nf = np.float32(n)
        val = np.float32(np.log(nf / np.float32(8))) / np.float32(math.log(16.0)) * np.float32(8)
        v = min(8 + int(val), 15)
    return (16 if rel > 0 else 0) + v


def _bias_onehot():
    e = np.zeros((32, 512), np.float32)
    for vi, v in enumerate((-1, 0)):
        for y in range(255):
            d = v * 128 + 127 - y
            e[_bucket(d), vi * 256 + y] = 1.0
    return e


def build(NSEQ, T, dbg=False, needed=None):
    NTOK = NSEQ * T
    KTOP = min(256, T // 4)
    NST = NTOK // 512
    STS = T // 512
    TB = min(1024, NTOK)
    nc = bass.Bass("TRN2", target_bir_lowering=False)
    fw = FW(nc, needed)

    def din(name, shape):
        return fw.dram(name, shape, F32, kind="ExternalInput")

    x = din("x", [NTOK, D])
    g_mix = din("g_mix", [1, D])
    w_in = din("w_in", [1, D, DIN])
    w_alpha2 = din("w_alpha2", [1, 16, 256])
    b_alpha = din("b_alpha", [1, 256])
    g_gla = din("g_gla", [1, 128])
    w_br_gla = din("w_br_gla", [1, 512, D])
    g_q = din("g_q", [1, 64])
    g_k = din("g_k", [1, 64])
    rel_bias = din("rel_bias", [32, 8])
    w_br_att = din("w_br_att", [1, 512, D])
    w_out = din("w_out", [1, D, D])
    g_ffn = din("g_ffn", [1, D])
    w_rg = din("w_rg", [1, D, 4])
    b_rg = din("b_rg", [1, 4])
    w_re = din("w_re", [1, D, 32])
    b_re = din("b_re", [1, 32])
    w_gate = din("w_gate", [1, NEXP, D, 128])
    w_up = din("w_up", [1, NEXP, D, 128])
    w_down = din("w_down", [1, NEXP, 128, D])
    e1h = din("e1h", [32, 512])
    out = fw.dram("out", [NTOK, D], F32, kind="ExternalOutput")

    sk = "ExternalOutput" if dbg else "Internal"
    mixA_d = fw.dram("mixA_d", [D, NTOK], F32, kind=sk)
    epb_d = fw.dram("epb_d", [D, NTOK], F32, kind=sk)
    qbT_d = fw.dram("qbT_d", [512, NTOK], BF16, kind=sk)
    kbT_d = fw.dram("kbT_d", [512, NTOK], BF16, kind=sk)
    vb_d = fw.dram("vb_d", [NTOK, 520], BF16, kind=sk)
    qiT_d = fw.dram("qiT_d", [256, NTOK], BF16, kind=sk)
    kiT_d = fw.dram("kiT_d", [32, NTOK], BF16, kind=sk)
    wi_d = fw.dram("wi_d", [NTOK, 8], F32, kind=sk)
    phi_d = fw.dram("phi_d", [8, 512], F32, kind=sk)
    dbgs = {}

    def AP(buf, offset, ap):
        return bass.AP(tensor=buf.t, offset=offset, ap=ap)

    def mm(o, lhsT, rhs, start, stop, R, W):
        fw.op("pe", R, W, lambda: nc.tensor.matmul(o, lhsT=lhsT, rhs=rhs, start=start, stop=stop))

    def tr(o, in_, ident, R, W):
        fw.op("pe", R, W, lambda: nc.tensor.transpose(out=o, in_=in_, identity=ident))

    def act(o, in_, func, R, W, **kw):
        fw.op("act", R, W, lambda: nc.scalar.activation(out=o, in_=in_, func=func, **kw))

    def ts(e, o, in0, s1, s2, op0, op1, R, W, **kw):
        eng = fw.eng[e]
        if op1 is None:
            fw.op(e, R, W, lambda: eng.tensor_scalar(out=o, in0=in0, scalar1=s1, scalar2=None, op0=op0, **kw))
        else:
            fw.op(e, R, W, lambda: eng.tensor_scalar(out=o, in0=in0, scalar1=s1, scalar2=s2, op0=op0, op1=op1, **kw))

    def tt(e, o, in0, in1, op, R, W):
        eng = fw.eng[e]
        fw.op(e, R, W, lambda: eng.tensor_tensor(out=o, in0=in0, in1=in1, op=op))

    def stt(e, o, in0, scalar, in1, op0, op1, R, W):
        eng = fw.eng[e]
        fw.op(e, R, W, lambda: eng.scalar_tensor_tensor(out=o, in0=in0, scalar=scalar, in1=in1, op0=op0, op1=op1))

    def cp(e, o, in_, R, W):
        if e == "act":
            fw.op(e, R, W, lambda: nc.scalar.copy(out=o, in_=in_))
        else:
            eng = fw.eng[e]
            fw.op(e, R, W, lambda: eng.tensor_copy(out=o, in_=in_))

    def ms(e, o, val, W):
        eng = fw.eng[e]
        fw.op(e, [], W, lambda: eng.memset(o, val))

    def rcp(o, in_, R, W):
        fw.op("dve", R, W, lambda: nc.vector.reciprocal(out=o, in_=in_))

    def sigmoid_act(o_ap, in_ap, R, W):
        act(o_ap, in_ap, AF.Exp, R, W, scale=-1.0)
        act(o_ap, o_ap, AF.Ln, W + [oneb], W, bias=oneb[:, 0:1])
        act(o_ap, o_ap, AF.Exp, W, W, scale=-1.0)

    def rstd_from(ssq_ap, o_ap, scale, epsb, R, W):
        act(o_ap, ssq_ap, AF.Ln, R + [epsb], W, scale=scale, bias=epsb[:, 0:1])
        act(o_ap, o_ap, AF.Exp, W, W, scale=-0.5)

    with ExitStack() as gst:
        identb = fw.sb(gst, "identb", [128, 128], BF16)
        identf = fw.sb(gst, "identf", [128, 128], F32)
        antib = fw.sb(gst, "antib", [128, 128], BF16)
        epsb = fw.sb(gst, "epsb", [128, 1], F32)
        oneb = fw.sb(gst, "oneb", [128, 1], F32)
        onesf = fw.sb(gst, "onesf", [128, 128], F32)
        ms("pool", epsb[:], EPS, [epsb])
        ms("pool", oneb[:], 1.0, [oneb])
        ms("pool", onesf[:], 1.0, [onesf])
        ms("pool", identf[:], 0.0, [identf])
        fw.op("pool", [identf], [identf], lambda: nc.gpsimd.affine_select(
            out=identf[:], in_=identf[:], pattern=[[-1, 128]], compare_op=ALU.not_equal,
            fill=1.0, base=0, channel_multiplier=1))
        cp("dve", identb[:], identf[:], [identf], [identb])
        ms("pool", antib[:], 0.0, [antib])
        fw.op("pool", [antib], [antib], lambda: nc.gpsimd.affine_select(
            out=antib[:], in_=antib[:], pattern=[[1, 128]], compare_op=ALU.not_equal,
            fill=1.0, base=-127, channel_multiplier=1))

        with ExitStack() as st:
            winb = fw.sb(st, "winb", [128, 8, DIN], BF16)
            wbrg = fw.sb(st, "wbrg", [128, 4, D], BF16)
            wa2 = fw.sb(st, "wa2", [17, 256], F32)
            gmixB = fw.sb(st, "gmixB", [128, D], F32)
            gglaB = fw.sb(st, "gglaB", [128, 128], F32)
            gq2 = fw.sb(st, "gq2", [128, 1], F32)
            gk2 = fw.sb(st, "gk2", [128, 1], F32)
            Umat = fw.sb(st, "Umat", [128, 128], F32)
            blk1 = fw.sb(st, "blk1", [128, 128], F32)
            win_v = w_in.t.ap().rearrange("o (kc p) n -> p (o kc) n", p=128)
            CH = 776
            for c0 in range(0, DIN, CH):
                fw.dma("pool", winb[:, :, c0:c0 + CH], win_v[:, :, c0:c0 + CH], winb, w_in)
            fw.dma("pool", wbrg[:], w_br_gla.t.ap().rearrange("o (kc p) n -> p (o kc) n", p=128), wbrg, w_br_gla)
            fw.dma("sp", wa2[0:16, :], w_alpha2.t.ap().rearrange("o k n -> (o k) n"), wa2, w_alpha2)
            fw.dma("sp", wa2[16:17, :], b_alpha.t.ap(), wa2, b_alpha)
            fw.dma("sp", gmixB[:], AP(g_mix, 0, [[0, 128], [1, D]]), gmixB, g_mix)
            fw.dma("sp", gglaB[:], AP(g_gla, 0, [[0, 128], [1, 128]]), gglaB, g_gla)
            for hh in range(2):
                fw.dma("sp", gq2[hh * 64:(hh + 1) * 64, :], AP(g_q, 0, [[1, 64], [1, 1]]), gq2, g_q)
                fw.dma("sp", gk2[hh * 64:(hh + 1) * 64, :], AP(g_k, 0, [[1, 64], [1, 1]]), gk2, g_k)
            ts("dve", gq2[:], gq2[:], 0.125, None, ALU.mult, None, [gq2], [gq2])
            ms("pool", Umat[:], 1.0, [Umat])
            fw.op("pool", [Umat], [Umat], lambda: nc.gpsimd.affine_select(
                out=Umat[:], in_=Umat[:], pattern=[[-1, 128]], compare_op=ALU.is_gt,
                fill=0.0, base=0, channel_multiplier=1))
            ms("pool", Umat[64:128, 0:64], 0.0, [Umat])
            ms("pool", blk1[:], 0.0, [blk1])
            ms("pool", blk1[0:64, 0:64], 1.0 / 64, [blk1])
            ms("pool", blk1[64:128, 64:128], 1.0 / 64, [blk1])

            if dbg == "A0":
                fw.barrier()
                return nc, fw, out
            xt = [fw.sb(st, "xt%d" % i, [128, D], F32) for i in range(2)]
            junk = fw.sb(st, "junk", [128, D], BF16)
            st1 = [fw.sb(st, "st1_%d" % i, [128, 4], F32) for i in range(2)]
            xn = [fw.sb(st, "xn%d" % i, [128, D], BF16) for i in range(2)]
            xnT = [fw.sb(st, "xnT%d" % i, [128, 8, 512], BF16) for i in range(2)]
            qAT = fw.sb(st, "qAT", [128, 2, 512], BF16)
            alrT = fw.sb(st, "alrT", [17, 512], F32)
            epa = fw.sb(st, "epa", [128, 8, 512], F32)
            sqf = fw.sb(st, "sqf", [128, 512], F32)
            rsf = fw.sb(st, "rsf", [128, 512], F32)
            stgb = [fw.sb(st, "stgb%d" % i, [128, 512], BF16) for i in range(3)]
            stgf = [fw.sb(st, "stgf%d" % i, [128, 512], F32) for i in range(3)]
            eL = fw.sb(st, "eL", [128, 256], F32)
            Lt = fw.sb(st, "Lt", [128, 256], F32)
            dcy = fw.sb(st, "dcy", [128, 256], F32)
            dec = fw.sb(st, "dec", [128, 4], F32)
            kdec = fw.sb(st, "kdec", [128, 256], BF16)
            vAb = fw.sb(st, "vAb", [128, 512], BF16)
            er = fw.sb(st, "er", [128, 512], F32)
            sr = fw.sb(st, "sr", [128, 512], F32)
            S = fw.sb(st, "S", [128, 2, 256], F32)
            Sb = fw.sb(st, "Sb", [128, 2, 256], BF16)
            ssq4 = fw.sb(st, "ssq4", [128, 4], F32)
            rs4 = fw.sb(st, "rs4", [128, 4], F32)
            og = fw.sb(st, "og", [128, 512], F32)
            of = fw.sb(st, "of", [128, 512], BF16)
            oT = fw.sb(st, "oT", [128, 4, 512], BF16)
            vbst = [fw.sb(st, "vbst%d" % i, [128, 8, 65], BF16) for i in range(2)]
            wist = [fw.sb(st, "wist%d" % i, [128, 8], F32) for i in range(2)]
            pT = fw.ps(st, "pT", [128, 8, 128], BF16)
            pB = [fw.ps(st, "pB%d" % i, [128, 512], F32) for i in range(2)]
            pC = [fw.ps(st, "pC%d" % i, [128, 512], F32) for i in range(2)]
            pX = fw.ps(st, "pX", [128, 512], F32)
            pU = fw.ps(st, "pU", [128, 512], F32)
            pO = fw.ps(st, "pO", [128, 512], F32)
            ms("dve", alrT[:], 1.0, [alrT])
            for i in range(2):
                ms("dve", vbst[i][:], 1.0, [vbst[i]])

            fm_specs = [("qA", C_QA + 128 * j, 128, j) for j in range(2)]
            fm_specs += [("alr", C_ALR, 16, 0)]
            fm_specs += [("qB", C_QB + 128 * j, 128, j) for j in range(4)]
            fm_specs += [("kB", C_KB + 128 * j, 128, j) for j in range(4)]
            fm_specs += [("qi", C_QI + 128 * j, 128, j) for j in range(2)]
            fm_specs += [("ki", C_KI, 32, 0)]
            fm_specs += [("gA", C_GA + 128 * j, 128, j) for j in range(8)]
            fm_specs += [("gB", C_GB + 128 * j, 128, j) for j in range(8)]
            bi = 0
            ci = 0
            sgi = 0
            for g in range(NST):
                tok0 = g * 512
                xs = xnT[g % 2]
                if tok0 % T == 0:
                    ms("pool", S[:], 0.0, [S])
                for t in range(4):
                    xi = xt[t % 2]
                    si = st1[t % 2]
                    xni = xn[t % 2]
                    r0 = tok0 + t * 128
                    fw.dma("sp", xi[:], x.t.ap()[r0:r0 + 128, :], xi, x)
                    act(junk[:], xi[:], AF.Square, [xi], [junk, si], accum_out=si[:, 0:1])
                    rstd_from(si[:, 0:1], si[:, 1:2], 1.0 / D, epsb, [si], [si])
                    stt("dve", xni[:], xi[:], si[:, 1:2], gmixB[:], ALU.mult, ALU.mult, [xi, si, gmixB], [xni])
                    for kc in range(8):
                        tr(pT[:, kc, :], xni[:, kc * 128:(kc + 1) * 128], identb[:], [xni, identb], [pT])
                    cp("act", xs[:, :, t * 128:(t + 1) * 128], pT[:], [pT], [xs])
                if dbg == "A1":
                    fw.barrier()
                    return nc, fw, out
                for (kind, c0, M, j) in fm_specs:
                    pb = pB[bi % 2]
                    bi += 1
                    for kc in range(8):
                        mm(pb[0:M, :], winb[:, kc, c0:c0 + M], xs[:, kc, :], kc == 0, kc == 7, [winb, xs], [pb])
                    if kind == "qA":
                        fw.op("act", [pb], [qAT], lambda: nc.scalar.mul(out=qAT[:, j, :], in_=pb[:], mul=0.125))
                    elif kind == "alr":
                        cp("act", alrT[0:16, :], pb[0:16, :], [pb], [alrT])
                    elif kind in ("qB", "kB"):
                        gsc = gq2 if kind == "qB" else gk2
                        dd = qbT_d if kind == "qB" else kbT_d
                        act(sqf[:], pb[:], AF.Square, [pb], [sqf])
                        mm(pX[:], blk1[:], sqf[:], True, True, [blk1, sqf], [pX])
                        act(rsf[:], pX[:], AF.Ln, [pX, epsb], [rsf], bias=epsb[:, 0:1])
                        act(rsf[:], rsf[:], AF.Exp, [rsf], [rsf], scale=-0.5)
                        sg = stgb[sgi % 3]
                        sgi += 1
                        stt("dve", sg[:], pb[:], gsc[:, 0:1], rsf[:], ALU.mult, ALU.mult, [pb, gsc, rsf], [sg])
                        fw.dma("sp", dd.t.ap()[j * 128:(j + 1) * 128, tok0:tok0 + 512], sg[:], dd, sg)
                    elif kind == "qi":
                        sg = stgb[sgi % 3]
                        sgi += 1
                        cp("act", sg[:], pb[:], [pb], [sg])
                        fw.dma("sp", qiT_d.t.ap()[j * 128:(j + 1) * 128, tok0:tok0 + 512], sg[:], qiT_d, sg)
                    elif kind == "ki":
                        sg = stgb[sgi % 3]
                        sgi += 1
                        cp("act", sg[0:32, :], pb[0:32, :], [pb], [sg])
                        fw.dma("sp", kiT_d.t.ap()[:, tok0:tok0 + 512], sg[0:32, :], kiT_d, sg)
                    elif kind == "gA":
                        sigmoid_act(epa[:, j, :], pb[:], [pb], [epa])
                    elif kind == "gB":
                        sf = stgf[sgi % 3]
                        sgi += 1
                        sigmoid_act(sf[:], pb[:], [pb], [sf])
                        fw.dma("sp", epb_d.t.ap()[j * 128:(j + 1) * 128, tok0:tok0 + 512], sf[:], epb_d, sf)
                if dbg == "A2":
                    fw.barrier()
                    return nc, fw, out
                for t in range(4):
                    cs = slice(t * 128, (t + 1) * 128)
                    r0 = tok0 + t * 128

                    def tokmm(c0, N):
                        nonlocal ci
                        pc = pC[ci % 2]
                        ci += 1
                        for kc in range(8):
                            mm(pc[:, 0:N], xs[:, kc, cs], winb[:, kc, c0:c0 + N], kc == 0, kc == 7, [xs, winb], [pc])
                        return pc
                    pc = tokmm(C_VB, 512)
                    vs_ = vbst[t % 2]
                    cp("act", vs_[:, :, 0:64], pc[:].rearrange("p (h d) -> p h d", h=8), [pc], [vs_])
                    fw.dma("sp", vb_d.t.ap()[r0:r0 + 128, :], vs_[:].rearrange("p h d -> p (h d)"), vb_d, vs_)
                    pc = tokmm(C_WI, 8)
                    ws_ = wist[t % 2]
                    fw.op("act", [pc], [ws_], lambda: nc.scalar.mul(out=ws_[:], in_=pc[:, 0:8], mul=1.0 / 16))
                    fw.dma("sp", wi_d.t.ap()[r0:r0 + 128, :], ws_[:], wi_d, ws_)
                    mm(pX[:, 0:256], alrT[0:17, cs], wa2[0:17, :], True, True, [alrT, wa2], [pX])
                    act(eL[:], pX[:, 0:256], AF.Exp, [pX], [eL], scale=-1.0)
                    act(Lt[:], eL[:], AF.Ln, [eL, oneb], [Lt], bias=oneb[:, 0:1])
                    mm(pX[:, 256:512], Umat[:], Lt[:], True, True, [Umat, Lt], [pX])
                    act(dcy[:], pX[:, 256:512], AF.Exp, [pX], [dcy], scale=-1.0 / 16)
                    for ch in range(2):
                        for pr in range(2):
                            mm(pU[:, 500 + ch * 2 + pr:501 + ch * 2 + pr], Lt[ch * 64:(ch + 1) * 64, pr * 128:(pr + 1) * 128],
                               onesf[ch * 64:(ch + 1) * 64, 0:1], True, True, [Lt, onesf], [pU])
                    act(dec[:], pU[:, 500:504], AF.Exp, [pU], [dec], scale=-1.0 / 16)
                    pc = tokmm(C_KA, 256)
                    tt("dve", kdec[:], pc[:, 0:256], dcy[:], ALU.mult, [pc, dcy], [kdec])
                    pc = tokmm(C_VA, 512)
                    cp("act", vAb[:], pc[:], [pc], [vAb])
                    pc = tokmm(C_RA, 512)
                    sigmoid_act(er[:], pc[:], [pc], [er])
                    tt("dve", sr[:], pc[:], er[:], ALU.mult, [pc, er], [sr])
                    for ch in range(2):
                        rows = slice(ch * 64, (ch + 1) * 64)
                        for pr in range(2):
                            mm(pU[:, 0:256], kdec[rows, pr * 128:(pr + 1) * 128], vAb[rows, pr * 256:(pr + 1) * 256],
                               True, True, [kdec, vAb], [pU])
                            for hh in range(2):
                                prow = slice(hh * 64, (hh + 1) * 64)
                                stt("dve", S[prow, pr, hh * 128:(hh + 1) * 128], S[prow, pr, hh * 128:(hh + 1) * 128],
                                    dec[prow, ch * 2 + pr:ch * 2 + pr + 1], pU[prow, hh * 128:(hh + 1) * 128],
                                    ALU.mult, ALU.add, [S, dec, pU], [S])
                        cp("dve", Sb[:], S[:], [S], [Sb])
                        for pr in range(2):
                            mm(pO[rows, pr * 256:(pr + 1) * 256], qAT[:, pr, t * 128 + ch * 64:t * 128 + (ch + 1) * 64],
                               Sb[:, pr, :], True, True, [qAT, Sb], [pO])
                    act(og[:], pO[:], AF.Square, [pO], [og])
                    fw.op("dve", [og], [ssq4], lambda: nc.vector.tensor_reduce(
                        out=ssq4[:], in_=og[:].rearrange("p (h d) -> p h d", h=4), axis=AX.X, op=ALU.add))
                    rstd_from(ssq4[:], rs4[:], 1.0 / 128, epsb, [ssq4], [rs4])
                    for h in range(4):
                        stt("dve", og[:, h * 128:(h + 1) * 128], pO[:, h * 128:(h + 1) * 128], rs4[:, h:h + 1],
                            gglaB[:], ALU.mult, ALU.mult, [pO, rs4, gglaB], [og])
                    tt("dve", of[:], og[:], sr[:], ALU.mult, [og, sr], [of])
                    for h in range(4):
                        tr(pT[:, h, :], of[:, h * 128:(h + 1) * 128], identb[:], [of, identb], [pT])
                    cp("act", oT[:, :, cs], pT[:, 0:4, :], [pT], [oT])
                if dbg == "A3":
                    fw.barrier()
                    return nc, fw, out
                for oc in range(8):
                    pb = pB[bi % 2]
                    bi += 1
                    for kc in range(4):
                        mm(pb[:], wbrg[:, kc, oc * 128:(oc + 1) * 128], oT[:, kc, :], kc == 0, kc == 3, [wbrg, oT], [pb])
                    sf = stgf[sgi % 3]
                    sgi += 1
                    tt("dve", sf[:], pb[:], epa[:, oc, :], ALU.mult, [pb, epa], [sf])
                    fw.dma("sp", mixA_d.t.ap()[oc * 128:(oc + 1) * 128, tok0:tok0 + 512], sf[:], mixA_d, sf)
            fw.barrier()

        if dbg == "A":
            return nc, fw, out

        ybT_d = fw.dram("ybT_d", [512, NTOK], BF16)
        with ExitStack() as st:
            NBLK = T // 128
            rb15B = fw.sb(st, "rb15B", [128, 8], F32)
            hkb = fw.sb(st, "hkb", [128, 2, 8, 128], BF16)
            pow2 = fw.sb(st, "pow2", [128, NBIS + 2], F32)
            kbT_t = fw.sb(st, "kbT", [128, 4, T], BF16)
            vbs_t = fw.sb(st, "vbs", [128, NBLK, 520], BF16)
            ki3_t = fw.sb(st, "ki3", [96, T], BF16)
            kbTJ = [Buf("kbT%d" % j, kbT_t.t) for j in range(STS)]
            vbsJ = [Buf("vbs%d" % j, vbs_t.t) for j in range(STS)]
            ki3J = [Buf("ki3%d" % j, ki3_t.t) for j in range(STS)]
            kbT, vbs, ki3 = kbT_t, vbs_t, ki3_t
            qbs2 = [fw.sb(st, "qbs%d" % i, [128, 4, 512], BF16) for i in range(2)]
            qi32 = [fw.sb(st, "qi3%d" % i, [96, 3, 512], BF16) for i in range(2)]
            wis2 = [fw.sb(st, "wis%d" % i, [128, 4, 8], F32) for i in range(2)]
            maskT2 = [fw.sb(st, "maskT%d" % i, [128, NBLK, 512], BF16) for i in range(2)]
            diag = fw.sb(st, "diag", [128, 8, 128], BF16)
            Rb = [fw.sb(st, "Rb%d" % i, [128, 512], BF16) for i in range(3)]
            score2 = [fw.sb(st, "score%d" % i, [128, T], F32) for i in range(1)]
            maskq2 = [fw.sb(st, "maskq%d" % i, [128, T], BF16) for i in range(1)]
            bs = fw.sb(st, "bs", [128, 8], F32)
            wcols = fw.sb(st, "wcols", [128, NBIS + 2], F32)
            pex = [fw.sb(st, "pex%d" % i, [128, 512], BF16) for i in range(4)]
            ptm = [fw.sb(st, "ptm%d" % i, [128, 512], BF16) for i in range(4)]
            rec = fw.sb(st, "rec", [128, 4], F32)
            ybq = fw.sb(st, "ybq", [128, 4, 512], BF16)
            ybT = fw.sb(st, "ybT", [128, 4, 512], BF16)
            pS = [fw.ps(st, "pS%d" % i, [128, 512], F32) for i in range(2)]
            pSc = fw.ps(st, "pSc", [128, 512], F32)
            pT = fw.ps(st, "pTb", [128, 8, 128], BF16)
            pQK = [fw.ps(st, "pQK%d" % i, [128, 512], F32) for i in range(2)]
            pPV = [fw.ps(st, "pPV%d" % i, [128, 512], F32) for i in range(2)]

            with ExitStack() as st0:
                rbT = fw.sb(st0, "rbT", [32, 8], F32)
                e1s = fw.sb(st0, "e1s", [32, 512], F32)
                rb15c = fw.sb(st0, "rb15c", [8, 1], F32)
                phis = fw.sb(st0, "phis", [8, 512], F32)
                hk1 = fw.sb(st0, "hk1", [128, 128], F32)
                fw.dma("sp", rbT[:], rel_bias.t.ap(), rbT, rel_bias)
                fw.dma("sp", e1s[:], e1h.t.ap(), e1s, e1h)
                fw.dma("sp", rb15c[:], AP(rel_bias, 15 * 8, [[1, 8], [1, 1]]), rb15c, rel_bias)
                fw.dma("sp", rb15B[:], AP(rel_bias, 15 * 8, [[0, 128], [1, 8]]), rb15B, rel_bias)
                mm(pSc[0:8, :], rbT[:], e1s[:], True, True, [rbT, e1s], [pSc])
                ts("dve", phis[:], pSc[0:8, :], rb15c[:, 0:1], None, ALU.subtract, None, [pSc, rb15c], [phis])
                fw.dma("sp", phi_d.t.ap(), phis[:], phi_d, phis)
                for vi in range(2):
                    for h in range(8):
                        fw.dma("sp", hk1[:], AP(phi_d, h * 512 + vi * 256, [[1, 128], [1, 128]]), hk1, phi_d)
                        cp("dve", hkb[:, vi, h, :], hk1[:], [hk1], [hkb])
                for i in range(NBIS + 2):
                    ms("pool", pow2[:, i:i + 1], 2.0 ** (-i), [pow2])
                fw.barrier()

            cntr = {"qk": 0, "pv": 0, "bank": 0}
            bank4 = pS + pQK

            def next_bank():
                b = bank4[cntr["bank"] % 4]
                cntr["bank"] += 1
                return b

            def geo(g):
                tok0 = g * 512
                return tok0, (tok0 % T) // 512

            def loads(g):
                tok0, J = geo(g)
                qbs, qi3, wis = qbs2[g % 2], qi32[g % 2], wis2[g % 2]
                for c in range(4):
                    fw.dma("sp", kbT[:, c, J * 512:(J + 1) * 512], kbT_d.t.ap()[c * 128:(c + 1) * 128, tok0:tok0 + 512], kbTJ[J], kbT_d)
                    fw.dma("sp", qbs[:, c, :], qbT_d.t.ap()[c * 128:(c + 1) * 128, tok0:tok0 + 512], qbs, qbT_d)
                fw.dma("sp", vbs[:, 4 * J:4 * J + 4, :], vb_d.t.ap()[tok0:tok0 + 512, :].rearrange("(t p) c -> p t c", p=128), vbsJ[J], vb_d)
                for r in range(3):
                    fw.dma("sp", ki3[r * 32:(r + 1) * 32, J * 512:(J + 1) * 512], kiT_d.t.ap()[:, tok0:tok0 + 512], ki3J[J], kiT_d)
                for h in range(8):
                    fw.dma("sp", qi3[(h % 3) * 32:(h % 3) * 32 + 32, h // 3, :], qiT_d.t.ap()[h * 32:(h + 1) * 32, tok0:tok0 + 512], qi3, qiT_d)
                fw.dma("sp", wis[:], wi_d.t.ap()[tok0:tok0 + 512, :].rearrange("(t p) h -> p t h", p=128), wis, wi_d)

            def mask_chunks(g):
                tok0, J = geo(g)
                qi3, wis, maskT = qi32[g % 2], wis2[g % 2], maskT2[g % 2]

                def bis_iter(i, nk, score, maskq):
                    ts("dve", maskq[:, 0:nk], score[:, 0:nk], bs[:, 3:4], 0.0, ALU.is_ge, ALU.add, [score, bs], [maskq, bs],
                       accum_out=bs[:, 4:5])
                    ts("dve", bs[:, 5:6], bs[:, 4:5], KTOP - 0.5, 0.5, ALU.is_ge, ALU.subtract, [bs], [bs])
                    if i < NBIS:
                        stt("dve", bs[:, 3:4], bs[:, 5:6], wcols[:, i:i + 1], bs[:, 3:4], ALU.mult, ALU.add, [bs, wcols], [bs])

                def part1(t):
                    nk = (4 * J + t + 1) * 128
                    score, maskq = score2[0], maskq2[0]
                    for h in range(8):
                        ts("dve", diag[:, h, :], identb[:], wis[:, t, h:h + 1], None, ALU.mult, None, [identb, wis], [diag])
                    for s0 in range(0, nk, 512):
                        ns = min(512, nk - s0)
                        kr = [ki3J[jj] for jj in range(s0 // 512, (s0 + ns - 1) // 512 + 1)]

                        def s_mm(h):
                            ps_ = next_bank()
                            rb_ = Rb[h % 3]
                            r0 = (h % 3) * 32
                            mm(ps_[:, 0:ns], qi3[r0:r0 + 32, h // 3, t * 128:(t + 1) * 128], ki3[r0:r0 + 32, s0:s0 + ns],
                               True, True, [qi3] + kr, [ps_])
                            act(rb_[:, 0:ns], ps_[:, 0:ns], AF.Relu, [ps_], [rb_])
                        s_mm(0)
                        s_mm(1)
                        for h in range(8):
                            mm(pSc[:, 0:ns], diag[:, h, :], Rb[h % 3][:, 0:ns], h == 0, h == 7, [diag, Rb[h % 3]], [pSc])
                            if h + 2 < 8:
                                s_mm(h + 2)
                        cp("act", score[:, s0:s0 + ns], pSc[:, 0:ns], [pSc], [score])
                    fw.op("dve", [score], [bs], lambda: nc.vector.tensor_reduce(out=bs[:, 0:1], in_=score[:, 0:nk], axis=AX.X, op=ALU.min))
                    fw.op("dve", [score], [bs], lambda: nc.vector.tensor_reduce(out=bs[:, 1:2], in_=score[:, 0:nk], axis=AX.X, op=ALU.max))
                    ms("dve", score[0:64, nk - 64:nk], NEG, [score])
                    tt("dve", bs[:, 2:3], bs[:, 1:2], bs[:, 0:1], ALU.subtract, [bs], [bs])
                    ts("dve", wcols[:], pow2[:], bs[:, 2:3], None, ALU.mult, None, [pow2, bs], [wcols])
                    tt("dve", bs[:, 3:4], bs[:, 0:1], wcols[:, 1:2], ALU.add, [bs, wcols], [bs])
                    for i in range(1, NBIS // 2 + 1):
                        bis_iter(i, nk, score, maskq)

                def part2(t):
                    nk = (4 * J + t + 1) * 128
                    score, maskq = score2[0], maskq2[0]
                    for i in range(NBIS // 2 + 1, NBIS + 1):
                        bis_iter(i, nk, score, maskq)
                    stt("dve", bs[:, 6:7], bs[:, 5:6], wcols[:, NBIS:NBIS + 1], bs[:, 3:4], ALU.mult, ALU.add, [bs, wcols], [bs])
                    tt("dve", bs[:, 6:7], bs[:, 6:7], wcols[:, NBIS + 1:NBIS + 2], ALU.subtract, [bs, wcols], [bs])
                    ts("dve", maskq[:, 0:nk], score[:, 0:nk], bs[:, 6:7], None, ALU.is_ge, None, [score, bs], [maskq])

                def transp(t):
                    nk = (4 * J + t + 1) * 128
                    maskq = maskq2[0]
                    nb = nk // 128
                    for a0 in range(0, nb, 8):
                        na = min(8, nb - a0)
                        for a in range(a0, a0 + na):
                            tr(pT[:, a - a0, :], maskq[:, a * 128:(a + 1) * 128], identb[:], [maskq, identb], [pT])
                        cp("act", maskT[:, a0:a0 + na, t * 128:(t + 1) * 128], pT[:, 0:na, :], [pT], [maskT])

                pre = [lambda: part1(0), lambda: part2(0), lambda: part1(1), lambda: part2(1),
                       lambda: part1(2), lambda: part2(2), lambda: part1(3), lambda: part2(3)]
                post = [None, lambda: transp(0), None, lambda: transp(1), None, lambda: transp(2), None, None]
                return pre, post, (lambda: transp(3))

            def attention_head(g, h):
                tok0, J = geo(g)
                qbs, maskT = qbs2[g % 2], maskT2[g % 2]
                c = h // 2
                pb_ = (h % 2) * 64
                ppv = pPV[cntr["pv"] % 2]
                cntr["pv"] += 1
                na_tot = 4 * J + 4

                def qk_stage(a):
                    u = a - 4 * J
                    q0 = max(0, u) * 128
                    nq = 512 - q0
                    qki = cntr["qk"]
                    pq = next_bank()
                    pe_ = pex[qki % 4]
                    pt_ = ptm[qki % 4]
                    cntr["qk"] += 1
                    near = [(tq, a - 4 * J - tq) for tq in range(q0 // 128, 4) if (a - 4 * J - tq) in (-1, 0)]
                    mm(pq[:, 0:nq], kbT[pb_:pb_ + 64, c, a * 128:(a + 1) * 128], qbs[pb_:pb_ + 64, c, q0:512],
                       True, len(near) == 0, [kbTJ[a // 4], qbs], [pq])
                    for ni, (tq, v) in enumerate(near):
                        mm(pq[:, tq * 128 - q0:(tq + 1) * 128 - q0], antib[:], hkb[:, v + 1, h, :],
                           False, ni == len(near) - 1, [antib, hkb], [pq])
                    act(pe_[:, 0:nq], pq[:, 0:nq], AF.Exp, [pq, rb15B], [pe_], bias=rb15B[:, h:h + 1])
                    tt("pool", pt_[:, 0:nq], pe_[:, 0:nq], maskT[:, a, q0:512], ALU.mult, [pe_, maskT], [pt_])
                    return (a, q0, pt_)

                def pv_stage(st_):
                    a, q0, pt_ = st_
                    for tq in range(q0 // 128, 4):
                        fw.op("pe", [pt_, vbsJ[a // 4]], [ppv], lambda: nc.tensor.matmul(
                            ppv[:, tq * 65:(tq + 1) * 65], lhsT=pt_[:, tq * 128 - q0:(tq + 1) * 128 - q0], rhs=vbs[:, a, h * 65:(h + 1) * 65],
                            start=(a == 0 and tq == 0), stop=(a == 4 * J + tq), skip_group_check=True))
                pend = []
                for a in range(na_tot):
                    pend.append(qk_stage(a))
                    if len(pend) > 2:
                        pv_stage(pend.pop(0))
                while pend:
                    pv_stage(pend.pop(0))
                act(rec[:], ppv[:, 0:260].rearrange("p (t c) -> p t c", c=65)[:, :, 64], AF.Ln, [ppv], [rec])
                act(rec[:], rec[:], AF.Exp, [rec], [rec], scale=-1.0)
                for tq in range(4):
                    fw.op("act", [ppv, rec], [ybq], lambda: nc.scalar.mul(
                        out=ybq[:, tq, h * 64:(h + 1) * 64], in_=ppv[:, tq * 65:tq * 65 + 64], mul=rec[:, tq:tq + 1]))

            def finish(g):
                tok0, J = geo(g)
                for tq in range(4):
                    for c in range(4):
                        tr(pT[:, c, :], ybq[:, tq, c * 128:(c + 1) * 128], identb[:], [ybq, identb], [pT])
                    cp("act", ybT[:, :, tq * 128:(tq + 1) * 128], pT[:, 0:4, :], [pT], [ybT])
                for c in range(4):
                    fw.dma("sp", ybT_d.t.ap()[c * 128:(c + 1) * 128, tok0:tok0 + 512], ybT[:, c, :], ybT_d, ybT)

            def run_masks(g):
                pre, post, tail = mask_chunks(g)
                for h in range(8):
                    if pre[h] is not None:
                        pre[h]()
                    if post[h] is not None:
                        post[h]()
                tail()

            loads(0)
            run_masks(0)
            for g in range(NST):
                pre, post, tail = None, None, None
                defer = False
                if g + 1 < NST:
                    if geo(g + 1)[1] == 0:
                        defer = True
                    else:
                        loads(g + 1)
                        pre, post, tail = mask_chunks(g + 1)
                for h in range(8):
                    if pre is not None and pre[h] is not None:
                        pre[h]()
                    attention_head(g, h)
                    if post is not None and post[h] is not None:
                        post[h]()
                finish(g)
                if tail is not None:
                    tail()
                if defer:
                    loads(g + 1)
                    run_masks(g + 1)
            fw.barrier()

        with ExitStack() as st:
            wbra = fw.sb(st, "wbra", [128, 4, D], BF16)
            woutb = fw.sb(st, "woutb", [128, 8, D], BF16)
            fw.dma("pool", wbra[:], w_br_att.t.ap().rearrange("o (kc p) n -> p (o kc) n", p=128), wbra, w_br_att)
            fw.dma("pool", woutb[:], w_out.t.ap().rearrange("o (kc p) n -> p (o kc) n", p=128), woutb, w_out)
            ybl = [fw.sb(st, "ybl%d" % i, [128, 4, 512], BF16) for i in range(2)]
            epl = [fw.sb(st, "epl%d" % i, [128, 512], F32) for i in range(3)]
            mal = [fw.sb(st, "mal%d" % i, [128, 512], F32) for i in range(3)]
            t1f = [fw.sb(st, "t1f%d" % i, [128, 512], F32) for i in range(2)]
            mixT2 = [fw.sb(st, "mixT%d" % i, [128, 8, 512], BF16) for i in range(2)]
            xr = [fw.sb(st, "xr%d" % i, [128, D], F32) for i in range(3)]
            pQ2 = [fw.ps(st, "pQ2%d" % i, [128, 512], F32) for i in range(3)]
            pS2 = [fw.ps(st, "pS2%d" % i, [128, 512], F32) for i in range(4)]
            k3 = 0
            k4 = 0
            kx = 0
            for g in range(NST):
                tok0 = g * 512
                yb_ = ybl[g % 2]
                mixT = mixT2[g % 2]
                for c in range(4):
                    fw.dma("sp", yb_[:, c, :], ybT_d.t.ap()[c * 128:(c + 1) * 128, tok0:tok0 + 512], yb_, ybT_d)
                for oc in range(8):
                    pq = pQ2[k3 % 3]
                    ep_ = epl[k3 % 3]
                    ma_ = mal[k3 % 3]
                    tf_ = t1f[k3 % 2]
                    k3 += 1
                    fw.dma("sp", ep_[:], epb_d.t.ap()[oc * 128:(oc + 1) * 128, tok0:tok0 + 512], ep_, epb_d)
                    fw.dma("sp", ma_[:], mixA_d.t.ap()[oc * 128:(oc + 1) * 128, tok0:tok0 + 512], ma_, mixA_d)
                    for kc in range(4):
                        mm(pq[:], wbra[:, kc, oc * 128:(oc + 1) * 128], yb_[:, kc, :], kc == 0, kc == 3, [wbra, yb_], [pq])
                    tt("dve", tf_[:], pq[:], ep_[:], ALU.mult, [pq, ep_], [tf_])
                    tt("pool", mixT[:, oc, :], tf_[:], ma_[:], ALU.add, [tf_, ma_], [mixT])
                for tq in range(4):
                    r0 = tok0 + tq * 128
                    xi = xr[kx % 3]
                    kx += 1
                    fw.dma("sp", xi[:], x.t.ap()[r0:r0 + 128, :], xi, x)
                    for hf in range(2):
                        ps_ = pS2[k4 % 4]
                        k4 += 1
                        for kc in range(8):
                            mm(ps_[:], mixT[:, kc, tq * 128:(tq + 1) * 128], woutb[:, kc, hf * 512:(hf + 1) * 512],
                               kc == 0, kc == 7, [mixT, woutb], [ps_])
                        tt("dve", xi[:, hf * 512:(hf + 1) * 512], ps_[:], xi[:, hf * 512:(hf + 1) * 512], ALU.add, [ps_, xi], [xi])
                    fw.dma("sp", out.t.ap()[r0:r0 + 128, :], xi[:], out, xi)
            fw.barrier()

        if dbg == "B":
            return nc, fw, out

        with ExitStack() as st:
            NH = TB // 512
            NTB = TB // 128
            wdb = fw.sb(st, "wdb", [128, NEXP, D], BF16)
            for e0 in range(0, NEXP, 4):
                fw.dma("pool", wdb[:, e0:e0 + 4, :], w_down.t.ap().rearrange("o e k n -> k (o e) n")[:, e0:e0 + 4, :], wdb, w_down)
            gffB = fw.sb(st, "gffB", [128, D], F32)
            fw.dma("sp", gffB[:], AP(g_ffn, 0, [[0, 128], [1, D]]), gffB, g_ffn)
            wr = fw.sb(st, "wr", [128, 8, 36], F32)
            fw.dma("sp", wr[:, :, 0:4], w_rg.t.ap().rearrange("o (kc p) n -> p (o kc) n", p=128), wr, w_rg)
            fw.dma("sp", wr[:, :, 4:36], w_re.t.ap().rearrange("o (kc p) n -> p (o kc) n", p=128), wr, w_re)
            br = fw.sb(st, "br", [1, 36], F32)
            fw.dma("sp", br[:, 0:4], b_rg.t.ap(), br, b_rg)
            fw.dma("sp", br[:, 4:36], b_re.t.ap(), br, b_re)
            selA = fw.sb(st, "selA", [64, NEXP, 128], BF16)
            ms("pool", selA[:], 0.0, [selA])
            for base in (0, -32):
                fw.op("pool", [selA], [selA], lambda: nc.gpsimd.affine_select(
                    out=selA[:], in_=selA[:], pattern=[[-1, NEXP], [0, 128]], compare_op=ALU.not_equal,
                    fill=1.0, base=base, channel_multiplier=1))
            xt = [fw.sb(st, "xtc%d" % i, [128, D], F32) for i in range(2)]
            junk = fw.sb(st, "junkc", [128, D], BF16)
            st1 = [fw.sb(st, "st1c%d" % i, [128, 4], F32) for i in range(2)]
            xnf = [fw.sb(st, "xnf%d" % i, [128, D], F32) for i in range(1)]
            xfT = fw.sb(st, "xfT", [128, 8, 128], F32)
            xn2T = fw.sb(st, "xn2T", [128, 8, TB], BF16)
            lg = fw.sb(st, "lg", [128, 36], F32)
            rt = fw.sb(st, "rt", [128, 16], F32)
            ohg = fw.sb(st, "ohg", [128, 4], F32)
            esel = fw.sb(st, "esel", [128, 8], F32)
            top8 = fw.sb(st, "top8", [128, 8], F32)
            oh1 = fw.sb(st, "oh1", [128, 8], F32)
            oh2 = fw.sb(st, "oh2", [128, 8], F32)
            within = fw.sb(st, "within", [128, 8], F32)
            comb = fw.sb(st, "comb", [128, 32], F32)
            c2b = fw.sb(st, "c2b", [128, 32], BF16)
            c2f = fw.sb(st, "c2f", [128, 64], F32)
            cT2 = fw.sb(st, "cT2", [64, TB], BF16)
            wg = [fw.sb(st, "wg%d" % i, [128, 8, 128], BF16) for i in range(3)]
            wu = [fw.sb(st, "wu%d" % i, [128, 8, 128], BF16) for i in range(3)]
            eg = [fw.sb(st, "eg%d" % i, [128, 512], F32) for i in range(2)]
            t1 = [fw.sb(st, "t1_%d" % i, [128, 512], F32) for i in range(2)]
            hT = fw.sb(st, "hT", [128, NEXP, TB], BF16)
            x2l = [fw.sb(st, "x2l%d" % i, [128, D], F32) for i in range(1)]
            pTf = fw.ps(st, "pTf", [128, 4, 128], F32)
            pR = fw.ps(st, "pR", [128, 512], F32)
            pG = [fw.ps(st, "pG%d" % i, [128, 512], F32) for i in range(2)]
            pU2 = [fw.ps(st, "pU2%d" % i, [128, 512], F32) for i in range(2)]
            pBC = fw.ps(st, "pBC", [128, 512], F32)
            pD = fw.ps(st, "pD", [128, 512], F32)
            wgv = w_gate.t.ap().rearrange("o e (kc p) n -> (o e) p kc n", p=128)
            wuv = w_up.t.ap().rearrange("o e (kc p) n -> (o e) p kc n", p=128)
            if dbg == "C0":
                fw.barrier()
                return nc, fw, out
            wi_ = 0
            for blk in range(NTOK // TB):
                b0 = blk * TB
                for t in range(NTB):
                    xi = xt[t % 2]
                    si = st1[t % 2]
                    xf = xnf[0]
                    r0 = b0 + t * 128
                    cs = slice(t * 128, (t + 1) * 128)
                    fw.dma("sp", xi[:], out.t.ap()[r0:r0 + 128, :], xi, out)
                    act(junk[:], xi[:], AF.Square, [xi], [junk, si], accum_out=si[:, 0:1])
                    rstd_from(si[:, 0:1], si[:, 1:2], 1.0 / D, epsb, [si], [si])
                    stt("dve", xf[:], xi[:], si[:, 1:2], gffB[:], ALU.mult, ALU.mult, [xi, si, gffB], [xf])
                    for half in range(2):
                        for k4 in range(4):
                            kc = half * 4 + k4
                            tr(pTf[:, k4, :], xf[:, kc * 128:(kc + 1) * 128], identf[:], [xf, identf], [pTf])
                        cp("act", xfT[:, half * 4:half * 4 + 4, :], pTf[:], [pTf], [xfT])
                        cp("dve", xn2T[:, half * 4:half * 4 + 4, cs], pTf[:], [pTf], [xn2T])
                    for kc in range(8):
                        mm(pR[:, 0:36], xfT[:, kc, :], wr[:, kc, :], kc == 0, False, [xfT, wr], [pR])
                    mm(pR[:, 0:36], onesf[0:1, :], br[0:1, :], False, True, [onesf, br], [pR])
                    cp("act", lg[:], pR[:, 0:36], [pR], [lg])
                    fw.op("dve", [lg], [rt], lambda: nc.vector.tensor_reduce(out=rt[:, 0:1], in_=lg[:, 0:4], axis=AX.X, op=ALU.max))
                    ts("dve", rt[:, 1:2], rt[:, 0:1], -1.0, None, ALU.mult, None, [rt], [rt])
                    act(ohg[:], lg[:, 0:4], AF.Exp, [lg, rt], [ohg, rt], bias=rt[:, 1:2], accum_out=rt[:, 2:3])
                    rcp(rt[:, 3:4], rt[:, 2:3], [rt], [rt])
                    ts("dve", ohg[:], lg[:, 0:4], rt[:, 0:1], None, ALU.is_ge, None, [lg, rt], [ohg])
                    ts("dve", esel[:], lg[:, 4:12], ohg[:, 0:1], None, ALU.mult, None, [lg, ohg], [esel])
                    for gi in range(1, 4):
                        stt("dve", esel[:], lg[:, 4 + 8 * gi:12 + 8 * gi], ohg[:, gi:gi + 1], esel[:], ALU.mult, ALU.add,
                            [lg, ohg, esel], [esel])
                    fw.op("dve", [esel], [top8], lambda: nc.vector.max(out=top8[:], in_=esel[:]))
                    ts("dve", oh1[:], esel[:], top8[:, 0:1], None, ALU.is_equal, None, [esel, top8], [oh1])
                    ts("dve", oh2[:], esel[:], top8[:, 1:2], None, ALU.is_equal, None, [esel, top8], [oh2])
                    tt("dve", rt[:, 4:5], top8[:, 1:2], top8[:, 0:1], ALU.subtract, [top8], [rt])
                    act(rt[:, 5:6], rt[:, 4:5], AF.Exp, [rt], [rt])
                    ts("dve", rt[:, 5:6], rt[:, 5:6], 1.0, None, ALU.add, None, [rt], [rt])
                    rcp(rt[:, 6:7], rt[:, 5:6], [rt], [rt])
                    tt("dve", rt[:, 7:8], rt[:, 6:7], rt[:, 3:4], ALU.mult, [rt], [rt])
                    tt("dve", rt[:, 8:9], rt[:, 3:4], rt[:, 7:8], ALU.subtract, [rt], [rt])
                    ts("dve", within[:], oh1[:], rt[:, 7:8], None, ALU.mult, None, [oh1, rt], [within])
                    stt("dve", within[:], oh2[:], rt[:, 8:9], within[:], ALU.mult, ALU.add, [oh2, rt, within], [within])
                    for gi in range(4):
                        ts("dve", comb[:, gi * 8:(gi + 1) * 8], within[:], ohg[:, gi:gi + 1], None, ALU.mult, None,
                           [within, ohg], [comb])
                    cp("dve", c2b[:], comb[:], [comb], [c2b])
                    cp("dve", c2f[:, 0:32], c2b[:], [c2b], [c2f])
                    tt("dve", c2f[:, 32:64], comb[:], c2f[:, 0:32], ALU.subtract, [comb, c2f], [c2f])
                    tr(pTf[0:64, 0, :], c2f[:, :], identf[:], [c2f, identf], [pTf])
                    cp("act", cT2[:, cs], pTf[0:64, 0, :], [pTf], [cT2])
                if dbg == "C1":
                    fw.barrier()
                    return nc, fw, out
                for e in range(NEXP):
                    wg_ = wg[wi_ % 3]
                    wu_ = wu[wi_ % 3]
                    wi_ += 1
                    fw.dma("pool", wg_[:], wgv[e], wg_, w_gate)
                    fw.dma("pool", wu_[:], wuv[e], wu_, w_up)
                    for half in range(NH):
                        hs = slice(half * 512, (half + 1) * 512)
                        pg_ = pG[(e * NH + half) % 2]
                        pu_ = pU2[(e * NH + half) % 2]
                        eg_ = eg[(e * NH + half) % 2]
                        t1_ = t1[(e * NH + half) % 2]
                        for kc in range(8):
                            mm(pg_[:], wg_[:, kc, :], xn2T[:, kc, hs], kc == 0, kc == 7, [wg_, xn2T], [pg_])
                        for kc in range(8):
                            mm(pu_[:], wu_[:, kc, :], xn2T[:, kc, hs], kc == 0, kc == 7, [wu_, xn2T], [pu_])
                        pbc_ = pBC if (e * NH + half) % 2 == 0 else pD
                        mm(pbc_[:], selA[:, e, :], cT2[:, hs], True, True, [selA, cT2], [pbc_])
                        act(eg_[:], pg_[:], AF.Silu, [pg_], [eg_])
                        tt("dve", t1_[:], eg_[:], pu_[:], ALU.mult, [eg_, pu_], [t1_])
                        tt("dve", hT[:, e, hs], t1_[:], pbc_[:], ALU.mult, [t1_, pbc_], [hT])
                if dbg == "C2":
                    fw.barrier()
                    return nc, fw, out
                for t in range(NTB):
                    r0 = b0 + t * 128
                    xi = x2l[0]
                    fw.dma("sp", xi[:], out.t.ap()[r0:r0 + 128, :], xi, out)
                    pacc = (pD, pBC)
                    for e in range(NEXP):
                        for hf in range(2):
                            mm(pacc[hf][:], hT[:, e, t * 128:(t + 1) * 128], wdb[:, e, hf * 512:(hf + 1) * 512], e == 0, e == NEXP - 1,
                               [hT, wdb], [pacc[hf]])
                    for hf in range(2):
                        tt("dve", xi[:, hf * 512:(hf + 1) * 512], pacc[hf][:], xi[:, hf * 512:(hf + 1) * 512], ALU.add, [pacc[hf], xi], [xi])
                    fw.dma("sp", out.t.ap()[r0:r0 + 128, :], xi[:], out, xi)
            fw.barrier()
    return nc, fw, out


_E1H = None


def _inputs_for_core(inputs, c, NSEQ, T):
    global _E1H
    if _E1H is None:
        _E1H = _bias_onehot()
    m = {}
    xs = np.ascontiguousarray(inputs["x"][c * NSEQ:(c + 1) * NSEQ]).reshape(NSEQ * T, D)
    m["x"] = xs
    for k, v in inputs.items():
        if k == "x":
            continue
        m[k] = np.ascontiguousarray(np.asarray(v, dtype=np.float32))
    m["e1h"] = _E1H
    return m


def kernel(**inputs):
    x = np.asarray(inputs["x"])
    B, T, _ = x.shape
    NSEQ = B // 8
    _, fw0, _ = build(NSEQ, T)
    nc, fw, out = build(NSEQ, T, needed=fw0.used)
    in_maps = [_inputs_for_core(inputs, c, NSEQ, T) for c in range(8)]
    res = run_bass_kernel_spmd(nc, in_maps, core_ids=list(range(8)))
    outs = [np.asarray(res.results[c]["out"]).reshape(NSEQ, T, D) for c in range(8)]
    return np.concatenate(outs, axis=0).astype(np.float32)
```

```python
import math
from contextlib import ExitStack
import numpy as np
import concourse.bass as bass
import concourse.mybir as mybir
from concourse.bass_utils import run_bass_kernel_spmd

F32 = mybir.dt.float32
BF16 = mybir.dt.bfloat16
ALU = mybir.AluOpType
AF = mybir.ActivationFunctionType
AX = mybir.AxisListType

D = 1024
DIN = 5432
NEXP = 32
EPS = 1e-6
C_QA, C_KA, C_VA, C_RA, C_ALR, C_QB, C_KB, C_VB, C_QI, C_KI, C_WI, C_GA, C_GB = (
    0, 256, 512, 1024, 1536, 1552, 2064, 2576, 3088, 3344, 3376, 3384, 4408)
NBIS = 16
NEG = -3.0e38


class Buf:
    __slots__ = ("name", "t", "w", "r", "dsem", "excl", "isdram")

    def __init__(self, name, t=None):
        self.name = name
        self.t = t
        self.w = {}
        self.r = {}
        self.dsem = None
        self.excl = False
        self.isdram = False

    def __getitem__(self, idx):
        return self.t[idx]


class FW:
    def __init__(self, nc, needed=None):
        self.nc = nc
        self.eng = {"pe": nc.tensor, "dve": nc.vector, "act": nc.scalar,
                    "pool": nc.gpsimd, "sp": nc.sync}
        self.sems = {}
        self.cnt = {}
        self.known = {}
        self.rank = {}
        self.rankof = {}
        self.needed = needed
        self.used = set()
        for k in self.eng:
            self.sems[k] = nc.alloc_semaphore("c_" + k)
            self.cnt[k] = 0
            self.rank[k] = 0
            self.known[k] = {}
        self.nd = 0
        self._dcount = {}
        self.uid = 0

    def sb(self, st, name, shape, dt):
        self.uid += 1
        return Buf(name, st.enter_context(self.nc.sbuf_tensor("%s_%d" % (name, self.uid), list(shape), dt)))

    def ps(self, st, name, shape, dt=F32):
        self.uid += 1
        b = Buf(name, st.enter_context(self.nc.psum_tensor("%s_%d" % (name, self.uid), list(shape), dt)))
        b.excl = True
        return b

    def dram(self, name, shape, dt, kind="Internal"):
        b = Buf(name, self.nc.dram_tensor(name, list(shape), dt, kind=kind))
        b.isdram = True
        return b

    def _dsem(self, b):
        if b.dsem is None:
            key = "d%d" % self.nd
            self.nd += 1
            self.sems[key] = self.nc.alloc_semaphore(key)
            b.dsem = key
        return b.dsem

    def _emit_wait(self, e, k, v):
        if k in self.cnt:
            self.used.add((k, v))
            val = v if self.needed is None else self.rankof[(k, v)]
        else:
            val = v
        self.eng[e].wait_ge(self.sems[k], val)

    def _waits(self, e, reads, writes, skipkey=None):
        need = {}
        for b in reads:
            for k, v in b.w.items():
                if need.get(k, 0) < v:
                    need[k] = v
        for b in writes:
            if b.isdram:
                continue
            for k, v in b.w.items():
                if k != skipkey and need.get(k, 0) < v:
                    need[k] = v
            for k, v in b.r.items():
                if need.get(k, 0) < v:
                    need[k] = v
        kn = self.known[e]
        for k, v in need.items():
            if kn.get(k, 0) >= v:
                continue
            if k == "pe" and e == "pe":
                continue
            self._emit_wait(e, k, v)
            kn[k] = v

    def _count(self, e, ins):
        self.cnt[e] += 1
        o = self.cnt[e]
        if self.needed is None:
            ins.then_inc(self.sems[e], 1)
        elif (e, o) in self.needed:
            ins.then_inc(self.sems[e], 1)
            self.rank[e] += 1
            self.rankof[(e, o)] = self.rank[e]
        return o

    def op(self, e, reads, writes, fn):
        if any(b.excl for b in reads):
            writes = list(writes) + [b for b in reads if b.excl and b not in writes]
            reads = [b for b in reads if not b.excl]
        self._waits(e, reads, writes)
        ins = fn()
        o = self._count(e, ins)
        for b in writes:
            b.w = {e: o}
            b.r = {}
        for b in reads:
            if b not in writes:
                b.r[e] = o
        return ins

    def dma(self, q, out_ap, in_ap, dst, src, **kw):
        owner = src if dst.isdram else dst
        key = self._dsem(owner)
        self._waits(q, [src], [dst], skipkey=key)
        ins = self.eng[q].dma_start(out=out_ap, in_=in_ap, **kw)
        cnt = self._dcount.get(key, 0) + 16
        self._dcount[key] = cnt
        ins.then_inc(self.sems[key], 16)
        if dst.isdram:
            dst.w[key] = cnt
        else:
            if key in dst.w and len(dst.w) == 1:
                dst.w[key] = cnt
            else:
                dst.w = {key: cnt}
            dst.r = {}
        src.r[key] = cnt
        return ins

    def barrier(self):
        sp = self.eng["sp"]
        kn = self.known["sp"]
        for k in list(self.sems.keys()):
            if k == "sp":
                continue
            v = self.cnt[k] if k in self.cnt else self._dcount.get(k, 0)
            if v > 0 and kn.get(k, 0) < v:
                self._emit_wait("sp", k, v)
                kn[k] = v
        ins = sp.nop()
        v = self._count("sp", ins)
        for e in ("pe", "dve", "act", "pool"):
            self._emit_wait(e, "sp", v)
            self.known[e]["sp"] = v
            for k, kv in kn.items():
                if self.known[e].get(k, 0) < kv:
                    self.known[e][k] = kv


def _bucket(rel):
    n = abs(rel)
    if n < 8:
        v = n
    else:
        nf = np.float32(n)
        val = np.float32(np.log(nf / np.float32(8))) / np.float32(math.log(16.0)) * np.float32(8)
        v = min(8 + int(val), 15)
    return (16 if rel > 0 else 0) + v


def _bias_onehot():
    e = np.zeros((32, 512), np.float32)
    for vi, v in enumerate((-1, 0)):
        for y in range(255):
            d = v * 128 + 127 - y
            e[_bucket(d), vi * 256 + y] = 1.0
    return e


def build(NSEQ, T, dbg=False, needed=None):
    NTOK = NSEQ * T
    KTOP = min(256, T // 4)
    NST = NTOK // 512
    STS = T // 512
    TB = min(1024, NTOK)
    nc = bass.Bass("TRN2", target_bir_lowering=False)
    fw = FW(nc, needed)

    def din(name, shape):
        return fw.dram(name, shape, F32, kind="ExternalInput")

    x = din("x", [NTOK, D])
    g_mix = din("g_mix", [1, D])
    w_in = din("w_in", [1, D, DIN])
    w_alpha2 = din("w_alpha2", [1, 16, 256])
    b_alpha = din("b_alpha", [1, 256])
    g_gla = din("g_gla", [1, 128])
    w_br_gla = din("w_br_gla", [1, 512, D])
    g_q = din("g_q", [1, 64])
    g_k = din("g_k", [1, 64])
    rel_bias = din("rel_bias", [32, 8])
    w_br_att = din("w_br_att", [1, 512, D])
    w_out = din("w_out", [1, D, D])
    g_ffn = din("g_ffn", [1, D])
    w_rg = din("w_rg", [1, D, 4])
    b_rg = din("b_rg", [1, 4])
    w_re = din("w_re", [1, D, 32])
    b_re = din("b_re", [1, 32])
    w_gate = din("w_gate", [1, NEXP, D, 128])
    w_up = din("w_up", [1, NEXP, D, 128])
    w_down = din("w_down", [1, NEXP, 128, D])
    e1h = din("e1h", [32, 512])
    out = fw.dram("out", [NTOK, D], F32, kind="ExternalOutput")

    sk = "ExternalOutput" if dbg else "Internal"
    mixA_d = fw.dram("mixA_d", [D, NTOK], F32, kind=sk)
    epb_d = fw.dram("epb_d", [D, NTOK], F32, kind=sk)
    qbT_d = fw.dram("qbT_d", [512, NTOK], BF16, kind=sk)
    kbT_d = fw.dram("kbT_d", [512, NTOK], BF16, kind=sk)
    vb_d = fw.dram("vb_d", [NTOK, 520], BF16, kind=sk)
    qiT_d = fw.dram("qiT_d", [256, NTOK], BF16, kind=sk)
    kiT_d = fw.dram("kiT_d", [32, NTOK], BF16, kind=sk)
    wi_d = fw.dram("wi_d", [NTOK, 8], F32, kind=sk)
    phi_d = fw.dram("phi_d", [8, 512], F32, kind=sk)
    dbgs = {}

    def AP(buf, offset, ap):
        return bass.AP(tensor=buf.t, offset=offset, ap=ap)

    def mm(o, lhsT, rhs, start, stop, R, W):
        fw.op("pe", R, W, lambda: nc.tensor.matmul(o, lhsT=lhsT, rhs=rhs, start=start, stop=stop))

    def tr(o, in_, ident, R, W):
        fw.op("pe", R, W, lambda: nc.tensor.transpose(out=o, in_=in_, identity=ident))

    def act(o, in_, func, R, W, **kw):
        fw.op("act", R, W, lambda: nc.scalar.activation(out=o, in_=in_, func=func, **kw))

    def ts(e, o, in0, s1, s2, op0, op1, R, W, **kw):
        eng = fw.eng[e]
        if op1 is None:
            fw.op(e, R, W, lambda: eng.tensor_scalar(out=o, in0=in0, scalar1=s1, scalar2=None, op0=op0, **kw))
        else:
            fw.op(e, R, W, lambda: eng.tensor_scalar(out=o, in0=in0, scalar1=s1, scalar2=s2, op0=op0, op1=op1, **kw))

    def tt(e, o, in0, in1, op, R, W):
        eng = fw.eng[e]
        fw.op(e, R, W, lambda: eng.tensor_tensor(out=o, in0=in0, in1=in1, op=op))

    def stt(e, o, in0, scalar, in1, op0, op1, R, W):
        eng = fw.eng[e]
        fw.op(e, R, W, lambda: eng.scalar_tensor_tensor(out=o, in0=in0, scalar=scalar, in1=in1, op0=op0, op1=op1))

    def cp(e, o, in_, R, W):
        if e == "act":
            fw.op(e, R, W, lambda: nc.scalar.copy(out=o, in_=in_))
        else:
            eng = fw.eng[e]
            fw.op(e, R, W, lambda: eng.tensor_copy(out=o, in_=in_))

    def ms(e, o, val, W):
        eng = fw.eng[e]
        fw.op(e, [], W, lambda: eng.memset(o, val))

    def rcp(o, in_, R, W):
        fw.op("dve", R, W, lambda: nc.vector.reciprocal(out=o, in_=in_))

    def sigmoid_act(o_ap, in_ap, R, W):
        act(o_ap, in_ap, AF.Exp, R, W, scale=-1.0)
        act(o_ap, o_ap, AF.Ln, W + [oneb], W, bias=oneb[:, 0:1])
        act(o_ap, o_ap, AF.Exp, W, W, scale=-1.0)

    def rstd_from(ssq_ap, o_ap, scale, epsb, R, W):
        act(o_ap, ssq_ap, AF.Ln, R + [epsb], W, scale=scale, bias=epsb[:, 0:1])
        act(o_ap, o_ap, AF.Exp, W, W, scale=-0.5)

    with ExitStack() as gst:
        identb = fw.sb(gst, "identb", [128, 128], BF16)
        identf = fw.sb(gst, "identf", [128, 128], F32)
        antib = fw.sb(gst, "antib", [128, 128], BF16)
        epsb = fw.sb(gst, "epsb", [128, 1], F32)
        oneb = fw.sb(gst, "oneb", [128, 1], F32)
        onesf = fw.sb(gst, "onesf", [128, 128], F32)
        ms("pool", epsb[:], EPS, [epsb])
        ms("pool", oneb[:], 1.0, [oneb])
        ms("pool", onesf[:], 1.0, [onesf])
        ms("pool", identf[:], 0.0, [identf])
        fw.op("pool", [identf], [identf], lambda: nc.gpsimd.affine_select(
            out=identf[:], in_=identf[:], pattern=[[-1, 128]], compare_op=ALU.not_equal,
            fill=1.0, base=0, channel_multiplier=1))
        cp("dve", identb[:], identf[:], [identf], [identb])
        ms("pool", antib[:], 0.0, [antib])
        fw.op("pool", [antib], [antib], lambda: nc.gpsimd.affine_select(
            out=antib[:], in_=antib[:], pattern=[[1, 128]], compare_op=ALU.not_equal,
            fill=1.0, base=-127, channel_multiplier=1))

        with ExitStack() as st:
            winb = fw.sb(st, "winb", [128, 8, DIN], BF16)
            wbrg = fw.sb(st, "wbrg", [128, 4, D], BF16)
            wa2 = fw.sb(st, "wa2", [17, 256], F32)
            gmixB = fw.sb(st, "gmixB", [128, D], F32)
            gglaB = fw.sb(st, "gglaB", [128, 128], F32)
            gq2 = fw.sb(st, "gq2", [128, 1], F32)
            gk2 = fw.sb(st, "gk2", [128, 1], F32)
            Umat = fw.sb(st, "Umat", [128, 128], F32)
            blk1 = fw.sb(st, "blk1", [128, 128], F32)
            win_v = w_in.t.ap().rearrange("o (kc p) n -> p (o kc) n", p=128)
            CH = 776
            for c0 in range(0, DIN, CH):
                fw.dma("pool", winb[:, :, c0:c0 + CH], win_v[:, :, c0:c0 + CH], winb, w_in)
            fw.dma("pool", wbrg[:], w_br_gla.t.ap().rearrange("o (kc p) n -> p (o kc) n", p=128), wbrg, w_br_gla)
            fw.dma("sp", wa2[0:16, :], w_alpha2.t.ap().rearrange("o k n -> (o k) n"), wa2, w_alpha2)
            fw.dma("sp", wa2[16:17, :], b_alpha.t.ap(), wa2, b_alpha)
            fw.dma("sp", gmixB[:], AP(g_mix, 0, [[0, 128], [1, D]]), gmixB, g_mix)
            fw.dma("sp", gglaB[:], AP(g_gla, 0, [[0, 128], [1, 128]]), gglaB, g_gla)
            for hh in range(2):
                fw.dma("sp", gq2[hh * 64:(hh + 1) * 64, :], AP(g_q, 0, [[1, 64], [1, 1]]), gq2, g_q)
                fw.dma("sp", gk2[hh * 64:(hh + 1) * 64, :], AP(g_k, 0, [[1, 64], [1, 1]]), gk2, g_k)
            ts("dve", gq2[:], gq2[:], 0.125, None, ALU.mult, None, [gq2], [gq2])
            ms("pool", Umat[:], 1.0, [Umat])
            fw.op("pool", [Umat], [Umat], lambda: nc.gpsimd.affine_select(
                out=Umat[:], in_=Umat[:], pattern=[[-1, 128]], compare_op=ALU.is_gt,
                fill=0.0, base=0, channel_multiplier=1))
            ms("pool", Umat[64:128, 0:64], 0.0, [Umat])
            ms("pool", blk1[:], 0.0, [blk1])
            ms("pool", blk1[0:64, 0:64], 1.0 / 64, [blk1])
            ms("pool", blk1[64:128, 64:128], 1.0 / 64, [blk1])

            if dbg == "A0":
                fw.barrier()
                return nc, fw, out
            xt = [fw.sb(st, "xt%d" % i, [128, D], F32) for i in range(2)]
            junk = fw.sb(st, "junk", [128, D], BF16)
            st1 = [fw.sb(st, "st1_%d" % i, [128, 4], F32) for i in range(2)]
            xn = [fw.sb(st, "xn%d" % i, [128, D], BF16) for i in range(2)]
            xnT = [fw.sb(st, "xnT%d" % i, [128, 8, 512], BF16) for i in range(2)]
            qAT = fw.sb(st, "qAT", [128, 2, 512], BF16)
            alrT = fw.sb(st, "alrT", [17, 512], F32)
            epa = fw.sb(st, "epa", [128, 8, 512], F32)
            sqf = fw.sb(st, "sqf", [128, 512], F32)
            rsf = fw.sb(st, "rsf", [128, 512], F32)
            stgb = [fw.sb(st, "stgb%d" % i, [128, 512], BF16) for i in range(3)]
            stgf = [fw.sb(st, "stgf%d" % i, [128, 512], F32) for i in range(3)]
            eL = fw.sb(st, "eL", [128, 256], F32)
            Lt = fw.sb(st, "Lt", [128, 256], F32)
            dcy = fw.sb(st, "dcy", [128, 256], F32)
            dec = fw.sb(st, "dec", [128, 4], F32)
            kdec = fw.sb(st, "kdec", [128, 256], BF16)
            vAb = fw.sb(st, "vAb", [128, 512], BF16)
            er = fw.sb(st, "er", [128, 512], F32)
            sr = fw.sb(st, "sr", [128, 512], F32)
            S = fw.sb(st, "S", [128, 2, 256], F32)
            Sb = fw.sb(st, "Sb", [128, 2, 256], BF16)
            ssq4 = fw.sb(st, "ssq4", [128, 4], F32)
            rs4 = fw.sb(st, "rs4", [128, 4], F32)
            og = fw.sb(st, "og", [128, 512], F32)
            of = fw.sb(st, "of", [128, 512], BF16)
            oT = fw.sb(st, "oT", [128, 4, 512], BF16)
            vbst = [fw.sb(st, "vbst%d" % i, [128, 8, 65], BF16) for i in range(2)]
            wist = [fw.sb(st, "wist%d" % i, [128, 8], F32) for i in range(2)]
            pT = fw.ps(st, "pT", [128, 8, 128], BF16)
            pB = [fw.ps(st, "pB%d" % i, [128, 512], F32) for i in range(2)]
            pC = [fw.ps(st, "pC%d" % i, [128, 512], F32) for i in range(2)]
            pX = fw.ps(st, "pX", [128, 512], F32)
            pU = fw.ps(st, "pU", [128, 512], F32)
            pO = fw.ps(st, "pO", [128, 512], F32)
            ms("dve", alrT[:], 1.0, [alrT])
            for i in range(2):
                ms("dve", vbst[i][:], 1.0, [vbst[i]])

            fm_specs = [("qA", C_QA + 128 * j, 128, j) for j in range(2)]
            fm_specs += [("alr", C_ALR, 16, 0)]
            fm_specs += [("qB", C_QB + 128 * j, 128, j) for j in range(4)]
            fm_specs += [("kB", C_KB + 128 * j, 128, j) for j in range(4)]
            fm_specs += [("qi", C_QI + 128 * j, 128, j) for j in range(2)]
            fm_specs += [("ki", C_KI, 32, 0)]
            fm_specs += [("gA", C_GA + 128 * j, 128, j) for j in range(8)]
            fm_specs += [("gB", C_GB + 128 * j, 128, j) for j in range(8)]
            bi = 0
            ci = 0
            sgi = 0
            for g in range(NST):
                tok0 = g * 512
                xs = xnT[g % 2]
                if tok0 % T == 0:
                    ms("pool", S[:], 0.0, [S])
                for t in range(4):
                    xi = xt[t % 2]
                    si = st1[t % 2]
                    xni = xn[t % 2]
                    r0 = tok0 + t * 128
                    fw.dma("sp", xi[:], x.t.ap()[r0:r0 + 128, :], xi, x)
                    act(junk[:], xi[:], AF.Square, [xi], [junk, si], accum_out=si[:, 0:1])
                    rstd_from(si[:, 0:1], si[:, 1:2], 1.0 / D, epsb, [si], [si])
                    stt("dve", xni[:], xi[:], si[:, 1:2], gmixB[:], ALU.mult, ALU.mult, [xi, si, gmixB], [xni])
                    for kc in range(8):
                        tr(pT[:, kc, :], xni[:, kc * 128:(kc + 1) * 128], identb[:], [xni, identb], [pT])
                    cp("act", xs[:, :, t * 128:(t + 1) * 128], pT[:], [pT], [xs])
                if dbg == "A1":
                    fw.barrier()
                    return nc, fw, out
                for (kind, c0, M, j) in fm_specs:
                    pb = pB[bi % 2]
                    bi += 1
                    for kc in range(8):
                        mm(pb[0:M, :], winb[:, kc, c0:c0 + M], xs[:, kc, :], kc == 0, kc == 7, [winb, xs], [pb])
                    if kind == "qA":
                        fw.op("act", [pb], [qAT], lambda: nc.scalar.mul(out=qAT[:, j, :], in_=pb[:], mul=0.125))
                    elif kind == "alr":
                        cp("act", alrT[0:16, :], pb[0:16, :], [pb], [alrT])
                    elif kind in ("qB", "kB"):
                        gsc = gq2 if kind == "qB" else gk2
                        dd = qbT_d if kind == "qB" else kbT_d
                        act(sqf[:], pb[:], AF.Square, [pb], [sqf])
                        mm(pX[:], blk1[:], sqf[:], True, True, [blk1, sqf], [pX])
                        act(rsf[:], pX[:], AF.Ln, [pX, epsb], [rsf], bias=epsb[:, 0:1])
                        act(rsf[:], rsf[:], AF.Exp, [rsf], [rsf], scale=-0.5)
                        sg = stgb[sgi % 3]
                        sgi += 1
                        stt("dve", sg[:], pb[:], gsc[:, 0:1], rsf[:], ALU.mult, ALU.mult, [pb, gsc, rsf], [sg])
                        fw.dma("sp", dd.t.ap()[j * 128:(j + 1) * 128, tok0:tok0 + 512], sg[:], dd, sg)
                    elif kind == "qi":
                        sg = stgb[sgi % 3]
                        sgi += 1
                        cp("act", sg[:], pb[:], [pb], [sg])
                        fw.dma("sp", qiT_d.t.ap()[j * 128:(j + 1) * 128, tok0:tok0 + 512], sg[:], qiT_d, sg)
                    elif kind == "ki":
                        sg = stgb[sgi % 3]
                        sgi += 1
                        cp("act", sg[0:32, :], pb[0:32, :], [pb], [sg])
                        fw.dma("sp", kiT_d.t.ap()[:, tok0:tok0 + 512], sg[0:32, :], kiT_d, sg)
                    elif kind == "gA":
                        sigmoid_act(epa[:, j, :], pb[:], [pb], [epa])
                    elif kind == "gB":
                        sf = stgf[sgi % 3]
                        sgi += 1
                        sigmoid_act(sf[:], pb[:], [pb], [sf])
                        fw.dma("sp", epb_d.t.ap()[j * 128:(j + 1) * 128, tok0:tok0 + 512], sf[:], epb_d, sf)
                if dbg == "A2":
                    fw.barrier()
                    return nc, fw, out
                for t in range(4):
                    cs = slice(t * 128, (t + 1) * 128)
                    r0 = tok0 + t * 128

                    def tokmm(c0, N):
                        nonlocal ci
                        pc = pC[ci % 2]
                        ci += 1
                        for kc in range(8):
                            mm(pc[:, 0:N], xs[:, kc, cs], winb[:, kc, c0:c0 + N], kc == 0, kc == 7, [xs, winb], [pc])
                        return pc
                    pc = tokmm(C_VB, 512)
                    vs_ = vbst[t % 2]
                    cp("act", vs_[:, :, 0:64], pc[:].rearrange("p (h d) -> p h d", h=8), [pc], [vs_])
                    fw.dma("sp", vb_d.t.ap()[r0:r0 + 128, :], vs_[:].rearrange("p h d -> p (h d)"), vb_d, vs_)
                    pc = tokmm(C_WI, 8)
                    ws_ = wist[t % 2]
                    fw.op("act", [pc], [ws_], lambda: nc.scalar.mul(out=ws_[:], in_=pc[:, 0:8], mul=1.0 / 16))
                    fw.dma("sp", wi_d.t.ap()[r0:r0 + 128, :], ws_[:], wi_d, ws_)
                    mm(pX[:, 0:256], alrT[0:17, cs], wa2[0:17, :], True, True, [alrT, wa2], [pX])
                    act(eL[:], pX[:, 0:256], AF.Exp, [pX], [eL], scale=-1.0)
                    act(Lt[:], eL[:], AF.Ln, [eL, oneb], [Lt], bias=oneb[:, 0:1])
                    mm(pX[:, 256:512], Umat[:], Lt[:], True, True, [Umat, Lt], [pX])
                    act(dcy[:], pX[:, 256:512], AF.Exp, [pX], [dcy], scale=-1.0 / 16)
                    for ch in range(2):
                        for pr in range(2):
                            mm(pU[:, 500 + ch * 2 + pr:501 + ch * 2 + pr], Lt[ch * 64:(ch + 1) * 64, pr * 128:(pr + 1) * 128],
                               onesf[ch * 64:(ch + 1) * 64, 0:1], True, True, [Lt, onesf], [pU])
                    act(dec[:], pU[:, 500:504], AF.Exp, [pU], [dec], scale=-1.0 / 16)
                    pc = tokmm(C_KA, 256)
                    tt("dve", kdec[:], pc[:, 0:256], dcy[:], ALU.mult, [pc, dcy], [kdec])
                    pc = tokmm(C_VA, 512)
                    cp("act", vAb[:], pc[:], [pc], [vAb])
                    pc = tokmm(C_RA, 512)
                    sigmoid_act(er[:], pc[:], [pc], [er])
                    tt("dve", sr[:], pc[:], er[:], ALU.mult, [pc, er], [sr])
                    for ch in range(2):
                        rows = slice(ch * 64, (ch + 1) * 64)
                        for pr in range(2):
                            mm(pU[:, 0:256], kdec[rows, pr * 128:(pr + 1) * 128], vAb[rows, pr * 256:(pr + 1) * 256],
                               True, True, [kdec, vAb], [pU])
                            for hh in range(2):
                                prow = slice(hh * 64, (hh + 1) * 64)
                                stt("dve", S[prow, pr, hh * 128:(hh + 1) * 128], S[prow, pr, hh * 128:(hh + 1) * 128],
                                    dec[prow, ch * 2 + pr:ch * 2 + pr + 1], pU[prow, hh * 128:(hh + 1) * 128],
                                    ALU.mult, ALU.add, [S, dec, pU], [S])
                        cp("dve", Sb[:], S[:], [S], [Sb])
                        for pr in range(2):
                            mm(pO[rows, pr * 256:(pr + 1) * 256], qAT[:, pr, t * 128 + ch * 64:t * 128 + (ch + 1) * 64],
                               Sb[:, pr, :], True, True, [qAT, Sb], [pO])
                    act(og[:], pO[:], AF.Square, [pO], [og])
                    fw.op("dve", [og], [ssq4], lambda: nc.vector.tensor_reduce(
                        out=ssq4[:], in_=og[:].rearrange("p (h d) -> p h d", h=4), axis=AX.X, op=ALU.add))
                    rstd_from(ssq4[:], rs4[:], 1.0 / 128, epsb, [ssq4], [rs4])
                    for h in range(4):
                        stt("dve", og[:, h * 128:(h + 1) * 128], pO[:, h * 128:(h + 1) * 128], rs4[:, h:h + 1],
                            gglaB[:], ALU.mult, ALU.mult, [pO, rs4, gglaB], [og])
                    tt("dve", of[:], og[:], sr[:], ALU.mult, [og, sr], [of])
                    for h in range(4):
                        tr(pT[:, h, :], of[:, h * 128:(h + 1) * 128], identb[:], [of, identb], [pT])
                    cp("act", oT[:, :, cs], pT[:, 0:4, :], [pT], [oT])
                if dbg == "A3":
                    fw.barrier()
                    return nc, fw, out
                for oc in range(8):
                    pb = pB[bi % 2]
                    bi += 1
                    for kc in range(4):
                        mm(pb[:], wbrg[:, kc, oc * 128:(oc + 1) * 128], oT[:, kc, :], kc == 0, kc == 3, [wbrg, oT], [pb])
                    sf = stgf[sgi % 3]
                    sgi += 1
                    tt("dve", sf[:], pb[:], epa[:, oc, :], ALU.mult, [pb, epa], [sf])
                    fw.dma("sp", mixA_d.t.ap()[oc * 128:(oc + 1) * 128, tok0:tok0 + 512], sf[:], mixA_d, sf)
            fw.barrier()

        if dbg == "A":
            return nc, fw, out

        ybT_d = fw.dram("ybT_d", [512, NTOK], BF16)
        with ExitStack() as st:
            NBLK = T // 128
            rb15B = fw.sb(st, "rb15B", [128, 8], F32)
            hkb = fw.sb(st, "hkb", [128, 2, 8, 128], BF16)
            pow2 = fw.sb(st, "pow2", [128, NBIS + 2], F32)
            kbT_t = fw.sb(st, "kbT", [128, 4, T], BF16)
            vbs_t = fw.sb(st, "vbs", [128, NBLK, 520], BF16)
            ki3_t = fw.sb(st, "ki3", [96, T], BF16)
            kbTJ = [Buf("kbT%d" % j, kbT_t.t) for j in range(STS)]
            vbsJ = [Buf("vbs%d" % j, vbs_t.t) for j in range(STS)]
            ki3J = [Buf("ki3%d" % j, ki3_t.t) for j in range(STS)]
            kbT, vbs, ki3 = kbT_t, vbs_t, ki3_t
            qbs2 = [fw.sb(st, "qbs%d" % i, [128, 4, 512], BF16) for i in range(2)]
            qi32 = [fw.sb(st, "qi3%d" % i, [96, 3, 512], BF16) for i in range(2)]
            wis2 = [fw.sb(st, "wis%d" % i, [128, 4, 8], F32) for i in range(2)]
            maskT2 = [fw.sb(st, "maskT%d" % i, [128, NBLK, 512], BF16) for i in range(2)]
            diag = fw.sb(st, "diag", [128, 8, 128], BF16)
            Rb = [fw.sb(st, "Rb%d" % i, [128, 512], BF16) for i in range(4)]
            score2 = [fw.sb(st, "score%d" % i, [128, T], F32) for i in range(1)]
            maskq2 = [fw.sb(st, "maskq%d" % i, [128, T], BF16) for i in range(1)]
            bs = fw.sb(st, "bs", [128, 8], F32)
            wcols = fw.sb(st, "wcols", [128, NBIS + 2], F32)
            pex = [fw.sb(st, "pex%d" % i, [128, 512], BF16) for i in range(4)]
            ptm = [fw.sb(st, "ptm%d" % i, [128, 512], BF16) for i in range(4)]
            rec = fw.sb(st, "rec", [128, 4], F32)
            ybq = fw.sb(st, "ybq", [128, 4, 512], BF16)
            ybT = fw.sb(st, "ybT", [128, 4, 512], BF16)
            pS = [fw.ps(st, "pS%d" % i, [128, 512], F32) for i in range(2)]
            pSc = fw.ps(st, "pSc", [128, 512], F32)
            pT = fw.ps(st, "pTb", [128, 8, 128], BF16)
            pQK = [fw.ps(st, "pQK%d" % i, [128, 512], F32) for i in range(2)]
            pPV = [fw.ps(st, "pPV%d" % i, [128, 512], F32) for i in range(2)]

            with ExitStack() as st0:
                rbT = fw.sb(st0, "rbT", [32, 8], F32)
                e1s = fw.sb(st0, "e1s", [32, 512], F32)
                rb15c = fw.sb(st0, "rb15c", [8, 1], F32)
                phis = fw.sb(st0, "phis", [8, 512], F32)
                hk1 = fw.sb(st0, "hk1", [128, 128], F32)
                fw.dma("sp", rbT[:], rel_bias.t.ap(), rbT, rel_bias)
                fw.dma("sp", e1s[:], e1h.t.ap(), e1s, e1h)
                fw.dma("sp", rb15c[:], AP(rel_bias, 15 * 8, [[1, 8], [1, 1]]), rb15c, rel_bias)
                fw.dma("sp", rb15B[:], AP(rel_bias, 15 * 8, [[0, 128], [1, 8]]), rb15B, rel_bias)
                mm(pSc[0:8, :], rbT[:], e1s[:], True, True, [rbT, e1s], [pSc])
                ts("dve", phis[:], pSc[0:8, :], rb15c[:, 0:1], None, ALU.subtract, None, [pSc, rb15c], [phis])
                fw.dma("sp", phi_d.t.ap(), phis[:], phi_d, phis)
                for vi in range(2):
                    for h in range(8):
                        fw.dma("sp", hk1[:], AP(phi_d, h * 512 + vi * 256, [[1, 128], [1, 128]]), hk1, phi_d)
                        cp("dve", hkb[:, vi, h, :], hk1[:], [hk1], [hkb])
                for i in range(NBIS + 2):
                    ms("pool", pow2[:, i:i + 1], 2.0 ** (-i), [pow2])
                fw.barrier()

            cntr = {"qk": 0, "pv": 0, "bank": 0}
            bank4 = pS + pQK

            def next_bank():
                b = bank4[cntr["bank"] % 4]
                cntr["bank"] += 1
                return b

            def geo(g):
                tok0 = g * 512
                return tok0, (tok0 % T) // 512

            def loads(g):
                tok0, J = geo(g)
                qbs, qi3, wis = qbs2[g % 2], qi32[g % 2], wis2[g % 2]
                for c in range(4):
                    fw.dma("sp", kbT[:, c, J * 512:(J + 1) * 512], kbT_d.t.ap()[c * 128:(c + 1) * 128, tok0:tok0 + 512], kbTJ[J], kbT_d)
                    fw.dma("sp", qbs[:, c, :], qbT_d.t.ap()[c * 128:(c + 1) * 128, tok0:tok0 + 512], qbs, qbT_d)
                fw.dma("sp", vbs[:, 4 * J:4 * J + 4, :], vb_d.t.ap()[tok0:tok0 + 512, :].rearrange("(t p) c -> p t c", p=128), vbsJ[J], vb_d)
                for r in range(3):
                    fw.dma("sp", ki3[r * 32:(r + 1) * 32, J * 512:(J + 1) * 512], kiT_d.t.ap()[:, tok0:tok0 + 512], ki3J[J], kiT_d)
                for h in range(8):
                    fw.dma("sp", qi3[(h % 3) * 32:(h % 3) * 32 + 32, h // 3, :], qiT_d.t.ap()[h * 32:(h + 1) * 32, tok0:tok0 + 512], qi3, qiT_d)
                fw.dma("sp", wis[:], wi_d.t.ap()[tok0:tok0 + 512, :].rearrange("(t p) h -> p t h", p=128), wis, wi_d)

            def mask_chunks(g):
                tok0, J = geo(g)
                qi3, wis, maskT = qi32[g % 2], wis2[g % 2], maskT2[g % 2]

                def bis_iter(i, nk, score, maskq):
                    ts("dve", maskq[:, 0:nk], score[:, 0:nk], bs[:, 3:4], 0.0, ALU.is_ge, ALU.add, [score, bs], [maskq, bs],
                       accum_out=bs[:, 4:5])
                    ts("dve", bs[:, 5:6], bs[:, 4:5], KTOP - 0.5, 0.5, ALU.is_ge, ALU.subtract, [bs], [bs])
                    if i < NBIS:
                        stt("dve", bs[:, 3:4], bs[:, 5:6], wcols[:, i:i + 1], bs[:, 3:4], ALU.mult, ALU.add, [bs, wcols], [bs])

                def part1(t):
                    nk = (4 * J + t + 1) * 128
                    score, maskq = score2[0], maskq2[0]
                    for h in range(8):
                        ts("dve", diag[:, h, :], identb[:], wis[:, t, h:h + 1], None, ALU.mult, None, [identb, wis], [diag])
                    for s0 in range(0, nk, 512):
                        ns = min(512, nk - s0)
                        kr = [ki3J[jj] for jj in range(s0 // 512, (s0 + ns - 1) // 512 + 1)]

                        def s_mm(h):
                            ps_ = next_bank()
                            rb_ = Rb[h % 4]
                            r0 = (h % 3) * 32
                            mm(ps_[:, 0:ns], qi3[r0:r0 + 32, h // 3, t * 128:(t + 1) * 128], ki3[r0:r0 + 32, s0:s0 + ns],
                               True, True, [qi3] + kr, [ps_])
                            act(rb_[:, 0:ns], ps_[:, 0:ns], AF.Relu, [ps_], [rb_])
                        s_mm(0)
                        s_mm(1)
                        s_mm(2)
                        for h in range(8):
                            mm(pSc[:, 0:ns], diag[:, h, :], Rb[h % 4][:, 0:ns], h == 0, h == 7, [diag, Rb[h % 4]], [pSc])
                            if h + 3 < 8:
                                s_mm(h + 3)
                        cp("act", score[:, s0:s0 + ns], pSc[:, 0:ns], [pSc], [score])
                    fw.op("dve", [score], [bs], lambda: nc.vector.tensor_reduce(out=bs[:, 0:1], in_=score[:, 0:nk], axis=AX.X, op=ALU.min))
                    fw.op("dve", [score], [bs], lambda: nc.vector.tensor_reduce(out=bs[:, 1:2], in_=score[:, 0:nk], axis=AX.X, op=ALU.max))
                    ms("dve", score[0:64, nk - 64:nk], NEG, [score])
                    tt("dve", bs[:, 2:3], bs[:, 1:2], bs[:, 0:1], ALU.subtract, [bs], [bs])
                    ts("dve", wcols[:], pow2[:], bs[:, 2:3], None, ALU.mult, None, [pow2, bs], [wcols])
                    tt("dve", bs[:, 3:4], bs[:, 0:1], wcols[:, 1:2], ALU.add, [bs, wcols], [bs])
                    for i in range(1, NBIS // 2 + 1):
                        bis_iter(i, nk, score, maskq)

                def part2(t):
                    nk = (4 * J + t + 1) * 128
                    score, maskq = score2[0], maskq2[0]
                    for i in range(NBIS // 2 + 1, NBIS + 1):
                        bis_iter(i, nk, score, maskq)
                    stt("dve", bs[:, 6:7], bs[:, 5:6], wcols[:, NBIS:NBIS + 1], bs[:, 3:4], ALU.mult, ALU.add, [bs, wcols], [bs])
                    tt("dve", bs[:, 6:7], bs[:, 6:7], wcols[:, NBIS + 1:NBIS + 2], ALU.subtract, [bs, wcols], [bs])
                    ts("dve", maskq[:, 0:nk], score[:, 0:nk], bs[:, 6:7], None, ALU.is_ge, None, [score, bs], [maskq])

                def transp(t):
                    nk = (4 * J + t + 1) * 128
                    maskq = maskq2[0]
                    nb = nk // 128
                    for a0 in range(0, nb, 8):
                        na = min(8, nb - a0)
                        for a in range(a0, a0 + na):
                            tr(pT[:, a - a0, :], maskq[:, a * 128:(a + 1) * 128], identb[:], [maskq, identb], [pT])
                        cp("act", maskT[:, a0:a0 + na, t * 128:(t + 1) * 128], pT[:, 0:na, :], [pT], [maskT])

                pre = [lambda: part1(0), lambda: part2(0), lambda: part1(1), lambda: part2(1),
                       lambda: part1(2), lambda: part2(2), lambda: part1(3), lambda: part2(3)]
                post = [None, lambda: transp(0), None, lambda: transp(1), None, lambda: transp(2), None, None]
                return pre, post, (lambda: transp(3))

            def attention_head(g, h):
                tok0, J = geo(g)
                qbs, maskT = qbs2[g % 2], maskT2[g % 2]
                c = h // 2
                pb_ = (h % 2) * 64
                ppv = pPV[cntr["pv"] % 2]
                cntr["pv"] += 1
                na_tot = 4 * J + 4

                def qk_stage(a):
                    u = a - 4 * J
                    q0 = max(0, u) * 128
                    nq = 512 - q0
                    qki = cntr["qk"]
                    pq = next_bank()
                    pe_ = pex[qki % 4]
                    pt_ = ptm[qki % 4]
                    cntr["qk"] += 1
                    near = [(tq, a - 4 * J - tq) for tq in range(q0 // 128, 4) if (a - 4 * J - tq) in (-1, 0)]
                    mm(pq[:, 0:nq], kbT[pb_:pb_ + 64, c, a * 128:(a + 1) * 128], qbs[pb_:pb_ + 64, c, q0:512],
                       True, len(near) == 0, [kbTJ[a // 4], qbs], [pq])
                    for ni, (tq, v) in enumerate(near):
                        mm(pq[:, tq * 128 - q0:(tq + 1) * 128 - q0], antib[:], hkb[:, v + 1, h, :],
                           False, ni == len(near) - 1, [antib, hkb], [pq])
                    act(pe_[:, 0:nq], pq[:, 0:nq], AF.Exp, [pq, rb15B], [pe_], bias=rb15B[:, h:h + 1])
                    tt("pool", pt_[:, 0:nq], pe_[:, 0:nq], maskT[:, a, q0:512], ALU.mult, [pe_, maskT], [pt_])
                    return (a, q0, pt_)

                def pv_stage(st_):
                    a, q0, pt_ = st_
                    for tq in range(q0 // 128, 4):
                        fw.op("pe", [pt_, vbsJ[a // 4]], [ppv], lambda: nc.tensor.matmul(
                            ppv[:, tq * 65:(tq + 1) * 65], lhsT=pt_[:, tq * 128 - q0:(tq + 1) * 128 - q0], rhs=vbs[:, a, h * 65:(h + 1) * 65],
                            start=(a == 0 and tq == 0), stop=(a == 4 * J + tq), skip_group_check=True))
                pend = []
                for a in range(na_tot):
                    pend.append(qk_stage(a))
                    if len(pend) > 2:
                        pv_stage(pend.pop(0))
                while pend:
                    pv_stage(pend.pop(0))
                act(rec[:], ppv[:, 0:260].rearrange("p (t c) -> p t c", c=65)[:, :, 64], AF.Ln, [ppv], [rec])
                act(rec[:], rec[:], AF.Exp, [rec], [rec], scale=-1.0)
                for tq in range(4):
                    fw.op("act", [ppv, rec], [ybq], lambda: nc.scalar.mul(
                        out=ybq[:, tq, h * 64:(h + 1) * 64], in_=ppv[:, tq * 65:tq * 65 + 64], mul=rec[:, tq:tq + 1]))

            def finish(g):
                tok0, J = geo(g)
                for tq in range(4):
                    for c in range(4):
                        tr(pT[:, c, :], ybq[:, tq, c * 128:(c + 1) * 128], identb[:], [ybq, identb], [pT])
                    cp("act", ybT[:, :, tq * 128:(tq + 1) * 128], pT[:, 0:4, :], [pT], [ybT])
                for c in range(4):
                    fw.dma("sp", ybT_d.t.ap()[c * 128:(c + 1) * 128, tok0:tok0 + 512], ybT[:, c, :], ybT_d, ybT)

            def run_masks(g):
                pre, post, tail = mask_chunks(g)
                for h in range(8):
                    if pre[h] is not None:
                        pre[h]()
                    if post[h] is not None:
                        post[h]()
                tail()

            loads(0)
            run_masks(0)
            for g in range(NST):
                pre, post, tail = None, None, None
                defer = False
                if g + 1 < NST:
                    if geo(g + 1)[1] == 0:
                        defer = True
                    else:
                        loads(g + 1)
                        pre, post, tail = mask_chunks(g + 1)
                for h in range(8):
                    if pre is not None and pre[h] is not None:
                        pre[h]()
                    attention_head(g, h)
                    if post is not None and post[h] is not None:
                        post[h]()
                finish(g)
                if tail is not None:
                    tail()
                if defer:
                    loads(g + 1)
                    run_masks(g + 1)
            fw.barrier()

        with ExitStack() as st:
            wbra = fw.sb(st, "wbra", [128, 4, D], BF16)
            woutb = fw.sb(st, "woutb", [128, 8, D], BF16)
            fw.dma("pool", wbra[:], w_br_att.t.ap().rearrange("o (kc p) n -> p (o kc) n", p=128), wbra, w_br_att)
            fw.dma("pool", woutb[:], w_out.t.ap().rearrange("o (kc p) n -> p (o kc) n", p=128), woutb, w_out)
            ybl = [fw.sb(st, "ybl%d" % i, [128, 4, 512], BF16) for i in range(2)]
            epl = [fw.sb(st, "epl%d" % i, [128, 512], F32) for i in range(3)]
            mal = [fw.sb(st, "mal%d" % i, [128, 512], F32) for i in range(3)]
            t1f = [fw.sb(st, "t1f%d" % i, [128, 512], F32) for i in range(2)]
            mixT2 = [fw.sb(st, "mixT%d" % i, [128, 8, 512], BF16) for i in range(2)]
            xr = [fw.sb(st, "xr%d" % i, [128, D], F32) for i in range(3)]
            pQ2 = [fw.ps(st, "pQ2%d" % i, [128, 512], F32) for i in range(3)]
            pS2 = [fw.ps(st, "pS2%d" % i, [128, 512], F32) for i in range(4)]
            k3 = 0
            k4 = 0
            kx = 0
            for g in range(NST):
                tok0 = g * 512
                yb_ = ybl[g % 2]
                mixT = mixT2[g % 2]
                for c in range(4):
                    fw.dma("sp", yb_[:, c, :], ybT_d.t.ap()[c * 128:(c + 1) * 128, tok0:tok0 + 512], yb_, ybT_d)
                for oc in range(8):
                    pq = pQ2[k3 % 3]
                    ep_ = epl[k3 % 3]
                    ma_ = mal[k3 % 3]
                    tf_ = t1f[k3 % 2]
                    k3 += 1
                    fw.dma("sp", ep_[:], epb_d.t.ap()[oc * 128:(oc + 1) * 128, tok0:tok0 + 512], ep_, epb_d)
                    fw.dma("sp", ma_[:], mixA_d.t.ap()[oc * 128:(oc + 1) * 128, tok0:tok0 + 512], ma_, mixA_d)
                    for kc in range(4):
                        mm(pq[:], wbra[:, kc, oc * 128:(oc + 1) * 128], yb_[:, kc, :], kc == 0, kc == 3, [wbra, yb_], [pq])
                    tt("dve", tf_[:], pq[:], ep_[:], ALU.mult, [pq, ep_], [tf_])
                    tt("pool", mixT[:, oc, :], tf_[:], ma_[:], ALU.add, [tf_, ma_], [mixT])
                for tq in range(4):
                    r0 = tok0 + tq * 128
                    xi = xr[kx % 3]
                    kx += 1
                    fw.dma("sp", xi[:], x.t.ap()[r0:r0 + 128, :], xi, x)
                    for hf in range(2):
                        ps_ = pS2[k4 % 4]
                        k4 += 1
                        for kc in range(8):
                            mm(ps_[:], mixT[:, kc, tq * 128:(tq + 1) * 128], woutb[:, kc, hf * 512:(hf + 1) * 512],
                               kc == 0, kc == 7, [mixT, woutb], [ps_])
                        tt("dve", xi[:, hf * 512:(hf + 1) * 512], ps_[:], xi[:, hf * 512:(hf + 1) * 512], ALU.add, [ps_, xi], [xi])
                    fw.dma("sp", out.t.ap()[r0:r0 + 128, :], xi[:], out, xi)
            fw.barrier()

        if dbg == "B":
            return nc, fw, out

        with ExitStack() as st:
            NH = TB // 512
            NTB = TB // 128
            wdb = fw.sb(st, "wdb", [128, NEXP, D], BF16)
            for e0 in range(0, NEXP, 4):
                fw.dma("pool", wdb[:, e0:e0 + 4, :], w_down.t.ap().rearrange("o e k n -> k (o e) n")[:, e0:e0 + 4, :], wdb, w_down)
            gffB = fw.sb(st, "gffB", [128, D], F32)
            fw.dma("sp", gffB[:], AP(g_ffn, 0, [[0, 128], [1, D]]), gffB, g_ffn)
            wr = fw.sb(st, "wr", [128, 8, 36], F32)
            fw.dma("sp", wr[:, :, 0:4], w_rg.t.ap().rearrange("o (kc p) n -> p (o kc) n", p=128), wr, w_rg)
            fw.dma("sp", wr[:, :, 4:36], w_re.t.ap().rearrange("o (kc p) n -> p (o kc) n", p=128), wr, w_re)
            br = fw.sb(st, "br", [1, 36], F32)
            fw.dma("sp", br[:, 0:4], b_rg.t.ap(), br, b_rg)
            fw.dma("sp", br[:, 4:36], b_re.t.ap(), br, b_re)
            selA = fw.sb(st, "selA", [64, NEXP, 128], BF16)
            ms("pool", selA[:], 0.0, [selA])
            for base in (0, -32):
                fw.op("pool", [selA], [selA], lambda: nc.gpsimd.affine_select(
                    out=selA[:], in_=selA[:], pattern=[[-1, NEXP], [0, 128]], compare_op=ALU.not_equal,
                    fill=1.0, base=base, channel_multiplier=1))
            xt = [fw.sb(st, "xtc%d" % i, [128, D], F32) for i in range(2)]
            junk = fw.sb(st, "junkc", [128, D], BF16)
            st1 = [fw.sb(st, "st1c%d" % i, [128, 4], F32) for i in range(2)]
            xnf = [fw.sb(st, "xnf%d" % i, [128, D], F32) for i in range(1)]
            xfT = fw.sb(st, "xfT", [128, 8, 128], F32)
            xn2T = fw.sb(st, "xn2T", [128, 8, TB], BF16)
            lg = fw.sb(st, "lg", [128, 36], F32)
            rt = fw.sb(st, "rt", [128, 16], F32)
            ohg = fw.sb(st, "ohg", [128, 4], F32)
            esel = fw.sb(st, "esel", [128, 8], F32)
            top8 = fw.sb(st, "top8", [128, 8], F32)
            oh1 = fw.sb(st, "oh1", [128, 8], F32)
            oh2 = fw.sb(st, "oh2", [128, 8], F32)
            within = fw.sb(st, "within", [128, 8], F32)
            comb = fw.sb(st, "comb", [128, 32], F32)
            c2b = fw.sb(st, "c2b", [128, 32], BF16)
            c2f = fw.sb(st, "c2f", [128, 64], F32)
            cT2 = fw.sb(st, "cT2", [64, TB], BF16)
            wg = [fw.sb(st, "wg%d" % i, [128, 8, 128], BF16) for i in range(3)]
            wu = [fw.sb(st, "wu%d" % i, [128, 8, 128], BF16) for i in range(3)]
            eg = [fw.sb(st, "eg%d" % i, [128, 512], F32) for i in range(2)]
            t1 = [fw.sb(st, "t1_%d" % i, [128, 512], F32) for i in range(2)]
            hT = fw.sb(st, "hT", [128, NEXP, TB], BF16)
            x2l = [fw.sb(st, "x2l%d" % i, [128, D], F32) for i in range(1)]
            pTf = fw.ps(st, "pTf", [128, 4, 128], F32)
            pR = fw.ps(st, "pR", [128, 512], F32)
            pG = [fw.ps(st, "pG%d" % i, [128, 512], F32) for i in range(2)]
            pU2 = [fw.ps(st, "pU2%d" % i, [128, 512], F32) for i in range(2)]
            pBC = fw.ps(st, "pBC", [128, 512], F32)
            pD = fw.ps(st, "pD", [128, 512], F32)
            wgv = w_gate.t.ap().rearrange("o e (kc p) n -> (o e) p kc n", p=128)
            wuv = w_up.t.ap().rearrange("o e (kc p) n -> (o e) p kc n", p=128)
            if dbg == "C0":
                fw.barrier()
                return nc, fw, out
            wi_ = 0
            for blk in range(NTOK // TB):
                b0 = blk * TB
                for t in range(NTB):
                    xi = xt[t % 2]
                    si = st1[t % 2]
                    xf = xnf[0]
                    r0 = b0 + t * 128
                    cs = slice(t * 128, (t + 1) * 128)
                    fw.dma("sp", xi[:], out.t.ap()[r0:r0 + 128, :], xi, out)
                    act(junk[:], xi[:], AF.Square, [xi], [junk, si], accum_out=si[:, 0:1])
                    rstd_from(si[:, 0:1], si[:, 1:2], 1.0 / D, epsb, [si], [si])
                    stt("dve", xf[:], xi[:], si[:, 1:2], gffB[:], ALU.mult, ALU.mult, [xi, si, gffB], [xf])
                    for half in range(2):
                        for k4 in range(4):
                            kc = half * 4 + k4
                            tr(pTf[:, k4, :], xf[:, kc * 128:(kc + 1) * 128], identf[:], [xf, identf], [pTf])
                        cp("act", xfT[:, half * 4:half * 4 + 4, :], pTf[:], [pTf], [xfT])
                        cp("dve", xn2T[:, half * 4:half * 4 + 4, cs], pTf[:], [pTf], [xn2T])
                    for kc in range(8):
                        mm(pR[:, 0:36], xfT[:, kc, :], wr[:, kc, :], kc == 0, False, [xfT, wr], [pR])
                    mm(pR[:, 0:36], onesf[0:1, :], br[0:1, :], False, True, [onesf, br], [pR])
                    cp("act", lg[:], pR[:, 0:36], [pR], [lg])
                    fw.op("dve", [lg], [rt], lambda: nc.vector.tensor_reduce(out=rt[:, 0:1], in_=lg[:, 0:4], axis=AX.X, op=ALU.max))
                    ts("dve", rt[:, 1:2], rt[:, 0:1], -1.0, None, ALU.mult, None, [rt], [rt])
                    act(ohg[:], lg[:, 0:4], AF.Exp, [lg, rt], [ohg, rt], bias=rt[:, 1:2], accum_out=rt[:, 2:3])
                    rcp(rt[:, 3:4], rt[:, 2:3], [rt], [rt])
                    ts("dve", ohg[:], lg[:, 0:4], rt[:, 0:1], None, ALU.is_ge, None, [lg, rt], [ohg])
                    ts("dve", esel[:], lg[:, 4:12], ohg[:, 0:1], None, ALU.mult, None, [lg, ohg], [esel])
                    for gi in range(1, 4):
                        stt("dve", esel[:], lg[:, 4 + 8 * gi:12 + 8 * gi], ohg[:, gi:gi + 1], esel[:], ALU.mult, ALU.add,
                            [lg, ohg, esel], [esel])
                    fw.op("dve", [esel], [top8], lambda: nc.vector.max(out=top8[:], in_=esel[:]))
                    ts("dve", oh1[:], esel[:], top8[:, 0:1], None, ALU.is_equal, None, [esel, top8], [oh1])
                    ts("dve", oh2[:], esel[:], top8[:, 1:2], None, ALU.is_equal, None, [esel, top8], [oh2])
                    tt("dve", rt[:, 4:5], top8[:, 1:2], top8[:, 0:1], ALU.subtract, [top8], [rt])
                    act(rt[:, 5:6], rt[:, 4:5], AF.Exp, [rt], [rt])
                    ts("dve", rt[:, 5:6], rt[:, 5:6], 1.0, None, ALU.add, None, [rt], [rt])
                    rcp(rt[:, 6:7], rt[:, 5:6], [rt], [rt])
                    tt("dve", rt[:, 7:8], rt[:, 6:7], rt[:, 3:4], ALU.mult, [rt], [rt])
                    tt("dve", rt[:, 8:9], rt[:, 3:4], rt[:, 7:8], ALU.subtract, [rt], [rt])
                    ts("dve", within[:], oh1[:], rt[:, 7:8], None, ALU.mult, None, [oh1, rt], [within])
                    stt("dve", within[:], oh2[:], rt[:, 8:9], within[:], ALU.mult, ALU.add, [oh2, rt, within], [within])
                    for gi in range(4):
                        ts("dve", comb[:, gi * 8:(gi + 1) * 8], within[:], ohg[:, gi:gi + 1], None, ALU.mult, None,
                           [within, ohg], [comb])
                    cp("dve", c2b[:], comb[:], [comb], [c2b])
                    cp("dve", c2f[:, 0:32], c2b[:], [c2b], [c2f])
                    tt("dve", c2f[:, 32:64], comb[:], c2f[:, 0:32], ALU.subtract, [comb, c2f], [c2f])
                    tr(pTf[0:64, 0, :], c2f[:, :], identf[:], [c2f, identf], [pTf])
                    cp("act", cT2[:, cs], pTf[0:64, 0, :], [pTf], [cT2])
                if dbg == "C1":
                    fw.barrier()
                    return nc, fw, out
                for e in range(NEXP):
                    wg_ = wg[wi_ % 3]
                    wu_ = wu[wi_ % 3]
                    wi_ += 1
                    fw.dma("pool", wg_[:], wgv[e], wg_, w_gate)
                    fw.dma("pool", wu_[:], wuv[e], wu_, w_up)
                    for half in range(NH):
                        hs = slice(half * 512, (half + 1) * 512)
                        pg_ = pG[(e * NH + half) % 2]
                        pu_ = pU2[(e * NH + half) % 2]
                        eg_ = eg[(e * NH + half) % 2]
                        t1_ = t1[(e * NH + half) % 2]
                        for kc in range(8):
                            mm(pg_[:], wg_[:, kc, :], xn2T[:, kc, hs], kc == 0, kc == 7, [wg_, xn2T], [pg_])
                        for kc in range(8):
                            mm(pu_[:], wu_[:, kc, :], xn2T[:, kc, hs], kc == 0, kc == 7, [wu_, xn2T], [pu_])
                        pbc_ = pBC if (e * NH + half) % 2 == 0 else pD
                        mm(pbc_[:], selA[:, e, :], cT2[:, hs], True, True, [selA, cT2], [pbc_])
                        act(eg_[:], pg_[:], AF.Silu, [pg_], [eg_])
                        tt("dve", t1_[:], eg_[:], pu_[:], ALU.mult, [eg_, pu_], [t1_])
                        tt("dve", hT[:, e, hs], t1_[:], pbc_[:], ALU.mult, [t1_, pbc_], [hT])
                if dbg == "C2":
                    fw.barrier()
                    return nc, fw, out
                for t in range(NTB):
                    r0 = b0 + t * 128
                    xi = x2l[0]
                    fw.dma("sp", xi[:], out.t.ap()[r0:r0 + 128, :], xi, out)
                    pacc = (pD, pBC)
                    for e in range(NEXP):
                        for hf in range(2):
                            mm(pacc[hf][:], hT[:, e, t * 128:(t + 1) * 128], wdb[:, e, hf * 512:(hf + 1) * 512], e == 0, e == NEXP - 1,
                               [hT, wdb], [pacc[hf]])
                    for hf in range(2):
                        tt("dve", xi[:, hf * 512:(hf + 1) * 512], pacc[hf][:], xi[:, hf * 512:(hf + 1) * 512], ALU.add, [pacc[hf], xi], [xi])
                    fw.dma("sp", out.t.ap()[r0:r0 + 128, :], xi[:], out, xi)
            fw.barrier()
    return nc, fw, out


_E1H = None


def _inputs_for_core(inputs, c, NSEQ, T):
    global _E1H
    if _E1H is None:
        _E1H = _bias_onehot()
    m = {}
    xs = np.ascontiguousarray(inputs["x"][c * NSEQ:(c + 1) * NSEQ]).reshape(NSEQ * T, D)
    m["x"] = xs
    for k, v in inputs.items():
        if k == "x":
            continue
        m[k] = np.ascontiguousarray(np.asarray(v, dtype=np.float32))
    m["e1h"] = _E1H
    return m


def kernel(**inputs):
    x = np.asarray(inputs["x"])
    B, T, _ = x.shape
    NSEQ = B // 8
    _, fw0, _ = build(NSEQ, T)
    nc, fw, out = build(NSEQ, T, needed=fw0.used)
    in_maps = [_inputs_for_core(inputs, c, NSEQ, T) for c in range(8)]
    res = run_bass_kernel_spmd(nc, in_maps, core_ids=list(range(8)))
    outs = [np.asarray(res.results[c]["out"]).reshape(NSEQ, T, D) for c in range(8)]
    return np.concatenate(outs, axis=0).astype(np.float32)
```

```python
import math
from contextlib import ExitStack
import numpy as np
import concourse.bass as bass
import concourse.mybir as mybir
from concourse.bass_utils import run_bass_kernel_spmd

F32 = mybir.dt.float32
BF16 = mybir.dt.bfloat16
ALU = mybir.AluOpType
AF = mybir.ActivationFunctionType
AX = mybir.AxisListType

D = 1024
DIN = 5432
NEXP = 32
EPS = 1e-6
C_QA, C_KA, C_VA, C_RA, C_ALR, C_QB, C_KB, C_VB, C_QI, C_KI, C_WI, C_GA, C_GB = (
    0, 256, 512, 1024, 1536, 1552, 2064, 2576, 3088, 3344, 3376, 3384, 4408)
NBIS = 16
NEG = -3.0e38


class Buf:
    __slots__ = ("name", "t", "w", "r", "dsem", "excl", "isdram")

    def __init__(self, name, t=None):
        self.name = name
        self.t = t
        self.w = {}
        self.r = {}
        self.dsem = None
        self.excl = False
        self.isdram = False

    def __getitem__(self, idx):
        return self.t[idx]


class FW:
    def __init__(self, nc, needed=None):
        self.nc = nc
        self.eng = {"pe": nc.tensor, "dve": nc.vector, "act": nc.scalar,
                    "pool": nc.gpsimd, "sp": nc.sync}
        self.sems = {}
        self.cnt = {}
        self.known = {}
        self.rank = {}
        self.rankof = {}
        self.needed = needed
        self.used = set()
        for k in self.eng:
            self.sems[k] = nc.alloc_semaphore("c_" + k)
            self.cnt[k] = 0
            self.rank[k] = 0
            self.known[k] = {}
        self.nd = 0
        self._dcount = {}
        self.uid = 0

    def sb(self, st, name, shape, dt):
        self.uid += 1
        return Buf(name, st.enter_context(self.nc.sbuf_tensor("%s_%d" % (name, self.uid), list(shape), dt)))

    def ps(self, st, name, shape, dt=F32):
        self.uid += 1
        b = Buf(name, st.enter_context(self.nc.psum_tensor("%s_%d" % (name, self.uid), list(shape), dt)))
        b.excl = True
        return b

    def dram(self, name, shape, dt, kind="Internal"):
        b = Buf(name, self.nc.dram_tensor(name, list(shape), dt, kind=kind))
        b.isdram = True
        return b

    def _dsem(self, b):
        if b.dsem is None:
            key = "d%d" % self.nd
            self.nd += 1
            self.sems[key] = self.nc.alloc_semaphore(key)
            b.dsem = key
        return b.dsem

    def _emit_wait(self, e, k, v):
        if k in self.cnt:
            self.used.add((k, v))
            val = v if self.needed is None else self.rankof[(k, v)]
        else:
            val = v
        self.eng[e].wait_ge(self.sems[k], val)

    def _waits(self, e, reads, writes, skipkey=None):
        need = {}
        for b in reads:
            for k, v in b.w.items():
                if need.get(k, 0) < v:
                    need[k] = v
        for b in writes:
            if b.isdram:
                continue
            for k, v in b.w.items():
                if k != skipkey and need.get(k, 0) < v:
                    need[k] = v
            for k, v in b.r.items():
                if need.get(k, 0) < v:
                    need[k] = v
        kn = self.known[e]
        for k, v in need.items():
            if kn.get(k, 0) >= v:
                continue
            if k == "pe" and e == "pe":
                continue
            self._emit_wait(e, k, v)
            kn[k] = v

    def _count(self, e, ins):
        self.cnt[e] += 1
        o = self.cnt[e]
        if self.needed is None:
            ins.then_inc(self.sems[e], 1)
        elif (e, o) in self.needed:
            ins.then_inc(self.sems[e], 1)
            self.rank[e] += 1
            self.rankof[(e, o)] = self.rank[e]
        return o

    def op(self, e, reads, writes, fn):
        if any(b.excl for b in reads):
            writes = list(writes) + [b for b in reads if b.excl and b not in writes]
            reads = [b for b in reads if not b.excl]
        self._waits(e, reads, writes)
        ins = fn()
        o = self._count(e, ins)
        for b in writes:
            b.w = {e: o}
            b.r = {}
        for b in reads:
            if b not in writes:
                b.r[e] = o
        return ins

    def dma(self, q, out_ap, in_ap, dst, src, **kw):
        owner = src if dst.isdram else dst
        key = self._dsem(owner)
        self._waits(q, [src], [dst], skipkey=key)
        ins = self.eng[q].dma_start(out=out_ap, in_=in_ap, **kw)
        cnt = self._dcount.get(key, 0) + 16
        self._dcount[key] = cnt
        ins.then_inc(self.sems[key], 16)
        if dst.isdram:
            dst.w[key] = cnt
        else:
            if key in dst.w and len(dst.w) == 1:
                dst.w[key] = cnt
            else:
                dst.w = {key: cnt}
            dst.r = {}
        src.r[key] = cnt
        return ins

    def barrier(self):
        sp = self.eng["sp"]
        kn = self.known["sp"]
        for k in list(self.sems.keys()):
            if k == "sp":
                continue
            v = self.cnt[k] if k in self.cnt else self._dcount.get(k, 0)
            if v > 0 and kn.get(k, 0) < v:
                self._emit_wait("sp", k, v)
                kn[k] = v
        ins = sp.nop()
        v = self._count("sp", ins)
        for e in ("pe", "dve", "act", "pool"):
            self._emit_wait(e, "sp", v)
            self.known[e]["sp"] = v
            for k, kv in kn.items():
                if self.known[e].get(k, 0) < kv:
                    self.known[e][k] = kv


def _bucket(rel):
    n = abs(rel)
    if n < 8:
        v = n
    else:
        nf = np.float32(n)
        val = np.float32(np.log(nf / np.float32(8))) / np.float32(math.log(16.0)) * np.float32(8)
        v = min(8 + int(val), 15)
    return (16 if rel > 0 else 0) + v


def _bias_onehot():
    e = np.zeros((32, 512), np.float32)
    for vi, v in enumerate((-1, 0)):
        for y in range(255):
            d = v * 128 + 127 - y
            e[_bucket(d), vi * 256 + y] = 1.0
    return e


def build(NSEQ, T, dbg=False, needed=None):
    NTOK = NSEQ * T
    KTOP = min(256, T // 4)
    NST = NTOK // 512
    STS = T // 512
    TB = min(1024, NTOK)
    nc = bass.Bass("TRN2", target_bir_lowering=False)
    fw = FW(nc, needed)

    def din(name, shape):
        return fw.dram(name, shape, F32, kind="ExternalInput")

    x = din("x", [NTOK, D])
    g_mix = din("g_mix", [1, D])
    w_in = din("w_in", [1, D, DIN])
    w_alpha2 = din("w_alpha2", [1, 16, 256])
    b_alpha = din("b_alpha", [1, 256])
    g_gla = din("g_gla", [1, 128])
    w_br_gla = din("w_br_gla", [1, 512, D])
    g_q = din("g_q", [1, 64])
    g_k = din("g_k", [1, 64])
    rel_bias = din("rel_bias", [32, 8])
    w_br_att = din("w_br_att", [1, 512, D])
    w_out = din("w_out", [1, D, D])
    g_ffn = din("g_ffn", [1, D])
    w_rg = din("w_rg", [1, D, 4])
    b_rg = din("b_rg", [1, 4])
    w_re = din("w_re", [1, D, 32])
    b_re = din("b_re", [1, 32])
    w_gate = din("w_gate", [1, NEXP, D, 128])
    w_up = din("w_up", [1, NEXP, D, 128])
    w_down = din("w_down", [1, NEXP, 128, D])
    e1h = din("e1h", [32, 512])
    out = fw.dram("out", [NTOK, D], F32, kind="ExternalOutput")

    sk = "ExternalOutput" if dbg else "Internal"
    mixA_d = fw.dram("mixA_d", [D, NTOK], F32, kind=sk)
    epb_d = fw.dram("epb_d", [D, NTOK], F32, kind=sk)
    qbT_d = fw.dram("qbT_d", [512, NTOK], BF16, kind=sk)
    kbT_d = fw.dram("kbT_d", [512, NTOK], BF16, kind=sk)
    vb_d = fw.dram("vb_d", [NTOK, 520], BF16, kind=sk)
    qiT_d = fw.dram("qiT_d", [256, NTOK], BF16, kind=sk)
    kiT_d = fw.dram("kiT_d", [32, NTOK], BF16, kind=sk)
    wi_d = fw.dram("wi_d", [NTOK, 8], F32, kind=sk)
    phi_d = fw.dram("phi_d", [8, 512], F32, kind=sk)
    dbgs = {}

    def AP(buf, offset, ap):
        return bass.AP(tensor=buf.t, offset=offset, ap=ap)

    def mm(o, lhsT, rhs, start, stop, R, W):
        fw.op("pe", R, W, lambda: nc.tensor.matmul(o, lhsT=lhsT, rhs=rhs, start=start, stop=stop))

    def tr(o, in_, ident, R, W):
        fw.op("pe", R, W, lambda: nc.tensor.transpose(out=o, in_=in_, identity=ident))

    def act(o, in_, func, R, W, **kw):
        fw.op("act", R, W, lambda: nc.scalar.activation(out=o, in_=in_, func=func, **kw))

    def ts(e, o, in0, s1, s2, op0, op1, R, W, **kw):
        eng = fw.eng[e]
        if op1 is None:
            fw.op(e, R, W, lambda: eng.tensor_scalar(out=o, in0=in0, scalar1=s1, scalar2=None, op0=op0, **kw))
        else:
            fw.op(e, R, W, lambda: eng.tensor_scalar(out=o, in0=in0, scalar1=s1, scalar2=s2, op0=op0, op1=op1, **kw))

    def tt(e, o, in0, in1, op, R, W):
        eng = fw.eng[e]
        fw.op(e, R, W, lambda: eng.tensor_tensor(out=o, in0=in0, in1=in1, op=op))

    def stt(e, o, in0, scalar, in1, op0, op1, R, W):
        eng = fw.eng[e]
        fw.op(e, R, W, lambda: eng.scalar_tensor_tensor(out=o, in0=in0, scalar=scalar, in1=in1, op0=op0, op1=op1))

    def cp(e, o, in_, R, W):
        if e == "act":
            fw.op(e, R, W, lambda: nc.scalar.copy(out=o, in_=in_))
        else:
            eng = fw.eng[e]
            fw.op(e, R, W, lambda: eng.tensor_copy(out=o, in_=in_))

    def ms(e, o, val, W):
        eng = fw.eng[e]
        fw.op(e, [], W, lambda: eng.memset(o, val))

    def rcp(o, in_, R, W):
        fw.op("dve", R, W, lambda: nc.vector.reciprocal(out=o, in_=in_))

    def sigmoid_act(o_ap, in_ap, R, W):
        act(o_ap, in_ap, AF.Exp, R, W, scale=-1.0)
        act(o_ap, o_ap, AF.Ln, W + [oneb], W, bias=oneb[:, 0:1])
        act(o_ap, o_ap, AF.Exp, W, W, scale=-1.0)

    def rstd_from(ssq_ap, o_ap, scale, epsb, R, W):
        act(o_ap, ssq_ap, AF.Ln, R + [epsb], W, scale=scale, bias=epsb[:, 0:1])
        act(o_ap, o_ap, AF.Exp, W, W, scale=-0.5)

    with ExitStack() as gst:
        identb = fw.sb(gst, "identb", [128, 128], BF16)
        identf = fw.sb(gst, "identf", [128, 128], F32)
        antib = fw.sb(gst, "antib", [128, 128], BF16)
        epsb = fw.sb(gst, "epsb", [128, 1], F32)
        oneb = fw.sb(gst, "oneb", [128, 1], F32)
        onesf = fw.sb(gst, "onesf", [128, 128], F32)
        ms("pool", epsb[:], EPS, [epsb])
        ms("pool", oneb[:], 1.0, [oneb])
        ms("pool", onesf[:], 1.0, [onesf])
        ms("pool", identf[:], 0.0, [identf])
        fw.op("pool", [identf], [identf], lambda: nc.gpsimd.affine_select(
            out=identf[:], in_=identf[:], pattern=[[-1, 128]], compare_op=ALU.not_equal,
            fill=1.0, base=0, channel_multiplier=1))
        cp("dve", identb[:], identf[:], [identf], [identb])
        ms("pool", antib[:], 0.0, [antib])
        fw.op("pool", [antib], [antib], lambda: nc.gpsimd.affine_select(
            out=antib[:], in_=antib[:], pattern=[[1, 128]], compare_op=ALU.not_equal,
            fill=1.0, base=-127, channel_multiplier=1))

        with ExitStack() as st:
            winb = fw.sb(st, "winb", [128, 8, DIN], BF16)
            wbrg = fw.sb(st, "wbrg", [128, 4, D], BF16)
            wa2 = fw.sb(st, "wa2", [17, 256], F32)
            gmixB = fw.sb(st, "gmixB", [128, D], F32)
            gglaB = fw.sb(st, "gglaB", [128, 128], F32)
            gq2 = fw.sb(st, "gq2", [128, 1], F32)
            gk2 = fw.sb(st, "gk2", [128, 1], F32)
            Umat = fw.sb(st, "Umat", [128, 128], F32)
            blk1 = fw.sb(st, "blk1", [128, 128], F32)
            win_v = w_in.t.ap().rearrange("o (kc p) n -> p (o kc) n", p=128)
            CH = 776
            for c0 in range(0, DIN, CH):
                fw.dma("pool", winb[:, :, c0:c0 + CH], win_v[:, :, c0:c0 + CH], winb, w_in)
            fw.dma("pool", wbrg[:], w_br_gla.t.ap().rearrange("o (kc p) n -> p (o kc) n", p=128), wbrg, w_br_gla)
            fw.dma("sp", wa2[0:16, :], w_alpha2.t.ap().rearrange("o k n -> (o k) n"), wa2, w_alpha2)
            fw.dma("sp", wa2[16:17, :], b_alpha.t.ap(), wa2, b_alpha)
            fw.dma("sp", gmixB[:], AP(g_mix, 0, [[0, 128], [1, D]]), gmixB, g_mix)
            fw.dma("sp", gglaB[:], AP(g_gla, 0, [[0, 128], [1, 128]]), gglaB, g_gla)
            for hh in range(2):
                fw.dma("sp", gq2[hh * 64:(hh + 1) * 64, :], AP(g_q, 0, [[1, 64], [1, 1]]), gq2, g_q)
                fw.dma("sp", gk2[hh * 64:(hh + 1) * 64, :], AP(g_k, 0, [[1, 64], [1, 1]]), gk2, g_k)
            ts("dve", gq2[:], gq2[:], 0.125, None, ALU.mult, None, [gq2], [gq2])
            ms("pool", Umat[:], 1.0, [Umat])
            fw.op("pool", [Umat], [Umat], lambda: nc.gpsimd.affine_select(
                out=Umat[:], in_=Umat[:], pattern=[[-1, 128]], compare_op=ALU.is_gt,
                fill=0.0, base=0, channel_multiplier=1))
            ms("pool", Umat[64:128, 0:64], 0.0, [Umat])
            ms("pool", blk1[:], 0.0, [blk1])
            ms("pool", blk1[0:64, 0:64], 1.0 / 64, [blk1])
            ms("pool", blk1[64:128, 64:128], 1.0 / 64, [blk1])

            if dbg == "A0":
                fw.barrier()
                return nc, fw, out
            xt = [fw.sb(st, "xt%d" % i, [128, D], F32) for i in range(2)]
            junk = fw.sb(st, "junk", [128, D], BF16)
            st1 = [fw.sb(st, "st1_%d" % i, [128, 4], F32) for i in range(2)]
            xn = [fw.sb(st, "xn%d" % i, [128, D], BF16) for i in range(2)]
            xnT = [fw.sb(st, "xnT%d" % i, [128, 8, 512], BF16) for i in range(2)]
            qAT = fw.sb(st, "qAT", [128, 2, 512], BF16)
            alrT = fw.sb(st, "alrT", [17, 512], F32)
            epa = fw.sb(st, "epa", [128, 8, 512], F32)
            sqf = fw.sb(st, "sqf", [128, 512], F32)
            rsf = fw.sb(st, "rsf", [128, 512], F32)
            stgb = [fw.sb(st, "stgb%d" % i, [128, 512], BF16) for i in range(3)]
            stgf = [fw.sb(st, "stgf%d" % i, [128, 512], F32) for i in range(3)]
            eL = fw.sb(st, "eL", [128, 256], F32)
            Lt = fw.sb(st, "Lt", [128, 256], F32)
            dcy = fw.sb(st, "dcy", [128, 256], F32)
            dec = fw.sb(st, "dec", [128, 4], F32)
            kdec = fw.sb(st, "kdec", [128, 256], BF16)
            vAb = fw.sb(st, "vAb", [128, 512], BF16)
            er = fw.sb(st, "er", [128, 512], F32)
            sr = fw.sb(st, "sr", [128, 512], F32)
            S = fw.sb(st, "S", [128, 2, 256], F32)
            Sb = fw.sb(st, "Sb", [128, 2, 256], BF16)
            ssq4 = fw.sb(st, "ssq4", [128, 4], F32)
            rs4 = fw.sb(st, "rs4", [128, 4], F32)
            og = fw.sb(st, "og", [128, 512], F32)
            of = fw.sb(st, "of", [128, 512], BF16)
            oT = fw.sb(st, "oT", [128, 4, 512], BF16)
            vbst = [fw.sb(st, "vbst%d" % i, [128, 8, 65], BF16) for i in range(2)]
            wist = [fw.sb(st, "wist%d" % i, [128, 8], F32) for i in range(2)]
            pT = fw.ps(st, "pT", [128, 8, 128], BF16)
            pB = [fw.ps(st, "pB%d" % i, [128, 512], F32) for i in range(2)]
            pC = [fw.ps(st, "pC%d" % i, [128, 512], F32) for i in range(2)]
            pX = fw.ps(st, "pX", [128, 512], F32)
            pU = fw.ps(st, "pU", [128, 512], F32)
            pO = fw.ps(st, "pO", [128, 512], F32)
            ms("dve", alrT[:], 1.0, [alrT])
            for i in range(2):
                ms("dve", vbst[i][:], 1.0, [vbst[i]])

            fm_specs = [("qA", C_QA + 128 * j, 128, j) for j in range(2)]
            fm_specs += [("alr", C_ALR, 16, 0)]
            fm_specs += [("qB", C_QB + 128 * j, 128, j) for j in range(4)]
            fm_specs += [("kB", C_KB + 128 * j, 128, j) for j in range(4)]
            fm_specs += [("qi", C_QI + 128 * j, 128, j) for j in range(2)]
            fm_specs += [("ki", C_KI, 32, 0)]
            fm_specs += [("gA", C_GA + 128 * j, 128, j) for j in range(8)]
            fm_specs += [("gB", C_GB + 128 * j, 128, j) for j in range(8)]
            bi = 0
            ci = 0
            sgi = 0
            for g in range(NST):
                tok0 = g * 512
                xs = xnT[g % 2]
                if tok0 % T == 0:
                    ms("pool", S[:], 0.0, [S])
                for t in range(4):
                    xi = xt[t % 2]
                    si = st1[t % 2]
                    xni = xn[t % 2]
                    r0 = tok0 + t * 128
                    fw.dma("sp", xi[:], x.t.ap()[r0:r0 + 128, :], xi, x)
                    act(junk[:], xi[:], AF.Square, [xi], [junk, si], accum_out=si[:, 0:1])
                    rstd_from(si[:, 0:1], si[:, 1:2], 1.0 / D, epsb, [si], [si])
                    stt("dve", xni[:], xi[:], si[:, 1:2], gmixB[:], ALU.mult, ALU.mult, [xi, si, gmixB], [xni])
                    for kc in range(8):
                        tr(pT[:, kc, :], xni[:, kc * 128:(kc + 1) * 128], identb[:], [xni, identb], [pT])
                    cp("act", xs[:, :, t * 128:(t + 1) * 128], pT[:], [pT], [xs])
                if dbg == "A1":
                    fw.barrier()
                    return nc, fw, out
                for (kind, c0, M, j) in fm_specs:
                    pb = pB[bi % 2]
                    bi += 1
                    for kc in range(8):
                        mm(pb[0:M, :], winb[:, kc, c0:c0 + M], xs[:, kc, :], kc == 0, kc == 7, [winb, xs], [pb])
                    if kind == "qA":
                        fw.op("act", [pb], [qAT], lambda: nc.scalar.mul(out=qAT[:, j, :], in_=pb[:], mul=0.125))
                    elif kind == "alr":
                        cp("act", alrT[0:16, :], pb[0:16, :], [pb], [alrT])
                    elif kind in ("qB", "kB"):
                        gsc = gq2 if kind == "qB" else gk2
                        dd = qbT_d if kind == "qB" else kbT_d
                        act(sqf[:], pb[:], AF.Square, [pb], [sqf])
                        mm(pX[:], blk1[:], sqf[:], True, True, [blk1, sqf], [pX])
                        act(rsf[:], pX[:], AF.Ln, [pX, epsb], [rsf], bias=epsb[:, 0:1])
                        act(rsf[:], rsf[:], AF.Exp, [rsf], [rsf], scale=-0.5)
                        sg = stgb[sgi % 3]
                        sgi += 1
                        stt("dve", sg[:], pb[:], gsc[:, 0:1], rsf[:], ALU.mult, ALU.mult, [pb, gsc, rsf], [sg])
                        fw.dma("sp", dd.t.ap()[j * 128:(j + 1) * 128, tok0:tok0 + 512], sg[:], dd, sg)
                    elif kind == "qi":
                        sg = stgb[sgi % 3]
                        sgi += 1
                        cp("act", sg[:], pb[:], [pb], [sg])
                        fw.dma("sp", qiT_d.t.ap()[j * 128:(j + 1) * 128, tok0:tok0 + 512], sg[:], qiT_d, sg)
                    elif kind == "ki":
                        sg = stgb[sgi % 3]
                        sgi += 1
                        cp("act", sg[0:32, :], pb[0:32, :], [pb], [sg])
                        fw.dma("sp", kiT_d.t.ap()[:, tok0:tok0 + 512], sg[0:32, :], kiT_d, sg)
                    elif kind == "gA":
                        sigmoid_act(epa[:, j, :], pb[:], [pb], [epa])
                    elif kind == "gB":
                        sf = stgf[sgi % 3]
                        sgi += 1
                        sigmoid_act(sf[:], pb[:], [pb], [sf])
                        fw.dma("sp", epb_d.t.ap()[j * 128:(j + 1) * 128, tok0:tok0 + 512], sf[:], epb_d, sf)
                if dbg == "A2":
                    fw.barrier()
                    return nc, fw, out
                for t in range(4):
                    cs = slice(t * 128, (t + 1) * 128)
                    r0 = tok0 + t * 128

                    def tokmm(c0, N):
                        nonlocal ci
                        pc = pC[ci % 2]
                        ci += 1
                        for kc in range(8):
                            mm(pc[:, 0:N], xs[:, kc, cs], winb[:, kc, c0:c0 + N], kc == 0, kc == 7, [xs, winb], [pc])
                        return pc
                    pc = tokmm(C_VB, 512)
                    vs_ = vbst[t % 2]
                    cp("act", vs_[:, :, 0:64], pc[:].rearrange("p (h d) -> p h d", h=8), [pc], [vs_])
                    fw.dma("sp", vb_d.t.ap()[r0:r0 + 128, :], vs_[:].rearrange("p h d -> p (h d)"), vb_d, vs_)
                    pc = tokmm(C_WI, 8)
                    ws_ = wist[t % 2]
                    fw.op("act", [pc], [ws_], lambda: nc.scalar.mul(out=ws_[:], in_=pc[:, 0:8], mul=1.0 / 16))
                    fw.dma("sp", wi_d.t.ap()[r0:r0 + 128, :], ws_[:], wi_d, ws_)
                    mm(pX[:, 0:256], alrT[0:17, cs], wa2[0:17, :], True, True, [alrT, wa2], [pX])
                    act(eL[:], pX[:, 0:256], AF.Exp, [pX], [eL], scale=-1.0)
                    act(Lt[:], eL[:], AF.Ln, [eL, oneb], [Lt], bias=oneb[:, 0:1])
                    mm(pX[:, 256:512], Umat[:], Lt[:], True, True, [Umat, Lt], [pX])
                    act(dcy[:], pX[:, 256:512], AF.Exp, [pX], [dcy], scale=-1.0 / 16)
                    for ch in range(2):
                        for pr in range(2):
                            mm(pU[:, 500 + ch * 2 + pr:501 + ch * 2 + pr], Lt[ch * 64:(ch + 1) * 64, pr * 128:(pr + 1) * 128],
                               onesf[ch * 64:(ch + 1) * 64, 0:1], True, True, [Lt, onesf], [pU])
                    act(dec[:], pU[:, 500:504], AF.Exp, [pU], [dec], scale=-1.0 / 16)
                    pc = tokmm(C_KA, 256)
                    tt("dve", kdec[:], pc[:, 0:256], dcy[:], ALU.mult, [pc, dcy], [kdec])
                    pc = tokmm(C_VA, 512)
                    cp("act", vAb[:], pc[:], [pc], [vAb])
                    pc = tokmm(C_RA, 512)
                    sigmoid_act(er[:], pc[:], [pc], [er])
                    tt("dve", sr[:], pc[:], er[:], ALU.mult, [pc, er], [sr])
                    for ch in range(2):
                        rows = slice(ch * 64, (ch + 1) * 64)
                        for pr in range(2):
                            mm(pU[:, 0:256], kdec[rows, pr * 128:(pr + 1) * 128], vAb[rows, pr * 256:(pr + 1) * 256],
                               True, True, [kdec, vAb], [pU])
                            for hh in range(2):
                                prow = slice(hh * 64, (hh + 1) * 64)
                                stt("dve", S[prow, pr, hh * 128:(hh + 1) * 128], S[prow, pr, hh * 128:(hh + 1) * 128],
                                    dec[prow, ch * 2 + pr:ch * 2 + pr + 1], pU[prow, hh * 128:(hh + 1) * 128],
                                    ALU.mult, ALU.add, [S, dec, pU], [S])
                        cp("dve", Sb[:], S[:], [S], [Sb])
                        for pr in range(2):
                            mm(pO[rows, pr * 256:(pr + 1) * 256], qAT[:, pr, t * 128 + ch * 64:t * 128 + (ch + 1) * 64],
                               Sb[:, pr, :], True, True, [qAT, Sb], [pO])
                    act(og[:], pO[:], AF.Square, [pO], [og])
                    fw.op("dve", [og], [ssq4], lambda: nc.vector.tensor_reduce(
                        out=ssq4[:], in_=og[:].rearrange("p (h d) -> p h d", h=4), axis=AX.X, op=ALU.add))
                    rstd_from(ssq4[:], rs4[:], 1.0 / 128, epsb, [ssq4], [rs4])
                    for h in range(4):
                        stt("dve", og[:, h * 128:(h + 1) * 128], pO[:, h * 128:(h + 1) * 128], rs4[:, h:h + 1],
                            gglaB[:], ALU.mult, ALU.mult, [pO, rs4, gglaB], [og])
                    tt("dve", of[:], og[:], sr[:], ALU.mult, [og, sr], [of])
                    for h in range(4):
                        tr(pT[:, h, :], of[:, h * 128:(h + 1) * 128], identb[:], [of, identb], [pT])
                    cp("act", oT[:, :, cs], pT[:, 0:4, :], [pT], [oT])
                if dbg == "A3":
                    fw.barrier()
                    return nc, fw, out
                for oc in range(8):
                    pb = pB[bi % 2]
                    bi += 1
                    for kc in range(4):
                        mm(pb[:], wbrg[:, kc, oc * 128:(oc + 1) * 128], oT[:, kc, :], kc == 0, kc == 3, [wbrg, oT], [pb])
                    sf = stgf[sgi % 3]
                    sgi += 1
                    tt("dve", sf[:], pb[:], epa[:, oc, :], ALU.mult, [pb, epa], [sf])
                    fw.dma("sp", mixA_d.t.ap()[oc * 128:(oc + 1) * 128, tok0:tok0 + 512], sf[:], mixA_d, sf)
            fw.barrier()

        if dbg == "A":
            return nc, fw, out

        ybT_d = fw.dram("ybT_d", [512, NTOK], BF16)
        with ExitStack() as st:
            NBLK = T // 128
            rb15B = fw.sb(st, "rb15B", [128, 8], F32)
            hkb = fw.sb(st, "hkb", [128, 2, 8, 128], BF16)
            pow2 = fw.sb(st, "pow2", [128, NBIS + 2], F32)
            kbT_t = fw.sb(st, "kbT", [128, 4, T], BF16)
            vbs_t = fw.sb(st, "vbs", [128, NBLK, 520], BF16)
            ki3_t = fw.sb(st, "ki3", [96, T], BF16)
            kbTJ = [Buf("kbT%d" % j, kbT_t.t) for j in range(STS)]
            vbsJ = [Buf("vbs%d" % j, vbs_t.t) for j in range(STS)]
            ki3J = [Buf("ki3%d" % j, ki3_t.t) for j in range(STS)]
            kbT, vbs, ki3 = kbT_t, vbs_t, ki3_t
            qbs2 = [fw.sb(st, "qbs%d" % i, [128, 4, 512], BF16) for i in range(2)]
            qi32 = [fw.sb(st, "qi3%d" % i, [96, 3, 512], BF16) for i in range(2)]
            wis2 = [fw.sb(st, "wis%d" % i, [128, 4, 8], F32) for i in range(2)]
            maskT2 = [fw.sb(st, "maskT%d" % i, [128, NBLK, 512], BF16) for i in range(2)]
            diag = fw.sb(st, "diag", [128, 8, 128], BF16)
            Rb = [fw.sb(st, "Rb%d" % i, [128, 512], BF16) for i in range(4)]
            score2 = [fw.sb(st, "score%d" % i, [128, T], F32) for i in range(1)]
            maskq2 = [fw.sb(st, "maskq%d" % i, [128, T], BF16) for i in range(1)]
            bs = fw.sb(st, "bs", [128, 8], F32)
            wcols = fw.sb(st, "wcols", [128, NBIS + 2], F32)
            pex = [fw.sb(st, "pex%d" % i, [128, 512], BF16) for i in range(4)]
            ptm = [fw.sb(st, "ptm%d" % i, [128, 512], BF16) for i in range(4)]
            rec = fw.sb(st, "rec", [128, 4], F32)
            ybq = fw.sb(st, "ybq", [128, 4, 512], BF16)
            ybT = fw.sb(st, "ybT", [128, 4, 512], BF16)
            pS = [fw.ps(st, "pS%d" % i, [128, 512], F32) for i in range(2)]
            pSc = fw.ps(st, "pSc", [128, 512], F32)
            pT = fw.ps(st, "pTb", [128, 8, 128], BF16)
            pQK = [fw.ps(st, "pQK%d" % i, [128, 512], F32) for i in range(2)]
            pPV = [fw.ps(st, "pPV%d" % i, [128, 512], F32) for i in range(2)]

            with ExitStack() as st0:
                rbT = fw.sb(st0, "rbT", [32, 8], F32)
                e1s = fw.sb(st0, "e1s", [32, 512], F32)
                rb15c = fw.sb(st0, "rb15c", [8, 1], F32)
                phis = fw.sb(st0, "phis", [8, 512], F32)
                hk1 = fw.sb(st0, "hk1", [128, 128], F32)
                fw.dma("sp", rbT[:], rel_bias.t.ap(), rbT, rel_bias)
                fw.dma("sp", e1s[:], e1h.t.ap(), e1s, e1h)
                fw.dma("sp", rb15c[:], AP(rel_bias, 15 * 8, [[1, 8], [1, 1]]), rb15c, rel_bias)
                fw.dma("sp", rb15B[:], AP(rel_bias, 15 * 8, [[0, 128], [1, 8]]), rb15B, rel_bias)
                mm(pSc[0:8, :], rbT[:], e1s[:], True, True, [rbT, e1s], [pSc])
                ts("dve", phis[:], pSc[0:8, :], rb15c[:, 0:1], None, ALU.subtract, None, [pSc, rb15c], [phis])
                fw.dma("sp", phi_d.t.ap(), phis[:], phi_d, phis)
                for vi in range(2):
                    for h in range(8):
                        fw.dma("sp", hk1[:], AP(phi_d, h * 512 + vi * 256, [[1, 128], [1, 128]]), hk1, phi_d)
                        cp("dve", hkb[:, vi, h, :], hk1[:], [hk1], [hkb])
                for i in range(NBIS + 2):
                    ms("pool", pow2[:, i:i + 1], 2.0 ** (-i), [pow2])
                fw.barrier()

            cntr = {"qk": 0, "pv": 0, "bank": 0}
            bank4 = pS + [pQK[0]]
            scbanks = [pSc, pQK[1]]
            cntr["sc"] = 0

            def next_bank():
                b = bank4[cntr["bank"] % 3]
                cntr["bank"] += 1
                return b

            def geo(g):
                tok0 = g * 512
                return tok0, (tok0 % T) // 512

            def loads(g):
                tok0, J = geo(g)
                qbs, qi3, wis = qbs2[g % 2], qi32[g % 2], wis2[g % 2]
                for c in range(4):
                    fw.dma("sp", kbT[:, c, J * 512:(J + 1) * 512], kbT_d.t.ap()[c * 128:(c + 1) * 128, tok0:tok0 + 512], kbTJ[J], kbT_d)
                    fw.dma("sp", qbs[:, c, :], qbT_d.t.ap()[c * 128:(c + 1) * 128, tok0:tok0 + 512], qbs, qbT_d)
                fw.dma("sp", vbs[:, 4 * J:4 * J + 4, :], vb_d.t.ap()[tok0:tok0 + 512, :].rearrange("(t p) c -> p t c", p=128), vbsJ[J], vb_d)
                for r in range(3):
                    fw.dma("sp", ki3[r * 32:(r + 1) * 32, J * 512:(J + 1) * 512], kiT_d.t.ap()[:, tok0:tok0 + 512], ki3J[J], kiT_d)
                for h in range(8):
                    fw.dma("sp", qi3[(h % 3) * 32:(h % 3) * 32 + 32, h // 3, :], qiT_d.t.ap()[h * 32:(h + 1) * 32, tok0:tok0 + 512], qi3, qiT_d)
                fw.dma("sp", wis[:], wi_d.t.ap()[tok0:tok0 + 512, :].rearrange("(t p) h -> p t h", p=128), wis, wi_d)

            def mask_chunks(g):
                tok0, J = geo(g)
                qi3, wis, maskT = qi32[g % 2], wis2[g % 2], maskT2[g % 2]

                def bis_iter(i, nk, score, maskq):
                    ts("dve", maskq[:, 0:nk], score[:, 0:nk], bs[:, 3:4], 0.0, ALU.is_ge, ALU.add, [score, bs], [maskq, bs],
                       accum_out=bs[:, 4:5])
                    ts("dve", bs[:, 5:6], bs[:, 4:5], KTOP - 0.5, 0.5, ALU.is_ge, ALU.subtract, [bs], [bs])
                    if i < NBIS:
                        stt("dve", bs[:, 3:4], bs[:, 5:6], wcols[:, i:i + 1], bs[:, 3:4], ALU.mult, ALU.add, [bs, wcols], [bs])

                def part1(t):
                    nk = (4 * J + t + 1) * 128
                    score, maskq = score2[0], maskq2[0]
                    for h in range(8):
                        ts("dve", diag[:, h, :], identb[:], wis[:, t, h:h + 1], None, ALU.mult, None, [identb, wis], [diag])
                    for s0 in range(0, nk, 512):
                        ns = min(512, nk - s0)
                        kr = [ki3J[jj] for jj in range(s0 // 512, (s0 + ns - 1) // 512 + 1)]

                        def s_mm(h):
                            ps_ = next_bank()
                            rb_ = Rb[h % 4]
                            r0 = (h % 3) * 32
                            mm(ps_[:, 0:ns], qi3[r0:r0 + 32, h // 3, t * 128:(t + 1) * 128], ki3[r0:r0 + 32, s0:s0 + ns],
                               True, True, [qi3] + kr, [ps_])
                            act(rb_[:, 0:ns], ps_[:, 0:ns], AF.Relu, [ps_], [rb_])
                        s_mm(0)
                        s_mm(1)
                        s_mm(2)
                        psc_ = scbanks[cntr["sc"] % 2]
                        cntr["sc"] += 1
                        for h in range(8):
                            mm(psc_[:, 0:ns], diag[:, h, :], Rb[h % 4][:, 0:ns], h == 0, h == 7, [diag, Rb[h % 4]], [psc_])
                            if h + 3 < 8:
                                s_mm(h + 3)
                        cp("act", score[:, s0:s0 + ns], psc_[:, 0:ns], [psc_], [score])
                    fw.op("dve", [score], [bs], lambda: nc.vector.tensor_reduce(out=bs[:, 0:1], in_=score[:, 0:nk], axis=AX.X, op=ALU.min))
                    fw.op("dve", [score], [bs], lambda: nc.vector.tensor_reduce(out=bs[:, 1:2], in_=score[:, 0:nk], axis=AX.X, op=ALU.max))
                    ms("dve", score[0:64, nk - 64:nk], NEG, [score])
                    tt("dve", bs[:, 2:3], bs[:, 1:2], bs[:, 0:1], ALU.subtract, [bs], [bs])
                    ts("dve", wcols[:], pow2[:], bs[:, 2:3], None, ALU.mult, None, [pow2, bs], [wcols])
                    tt("dve", bs[:, 3:4], bs[:, 0:1], wcols[:, 1:2], ALU.add, [bs, wcols], [bs])
                    for i in range(1, NBIS // 2 + 1):
                        bis_iter(i, nk, score, maskq)

                def part2(t):
                    nk = (4 * J + t + 1) * 128
                    score, maskq = score2[0], maskq2[0]
                    for i in range(NBIS // 2 + 1, NBIS + 1):
                        bis_iter(i, nk, score, maskq)
                    stt("dve", bs[:, 6:7], bs[:, 5:6], wcols[:, NBIS:NBIS + 1], bs[:, 3:4], ALU.mult, ALU.add, [bs, wcols], [bs])
                    tt("dve", bs[:, 6:7], bs[:, 6:7], wcols[:, NBIS + 1:NBIS + 2], ALU.subtract, [bs, wcols], [bs])
                    ts("dve", maskq[:, 0:nk], score[:, 0:nk], bs[:, 6:7], None, ALU.is_ge, None, [score, bs], [maskq])

                def transp(t):
                    nk = (4 * J + t + 1) * 128
                    maskq = maskq2[0]
                    nb = nk // 128
                    for a0 in range(0, nb, 8):
                        na = min(8, nb - a0)
                        for a in range(a0, a0 + na):
                            tr(pT[:, a - a0, :], maskq[:, a * 128:(a + 1) * 128], identb[:], [maskq, identb], [pT])
                        cp("act", maskT[:, a0:a0 + na, t * 128:(t + 1) * 128], pT[:, 0:na, :], [pT], [maskT])

                pre = [lambda: part1(0), lambda: part2(0), lambda: part1(1), lambda: part2(1),
                       lambda: part1(2), lambda: part2(2), lambda: part1(3), lambda: part2(3)]
                post = [None, lambda: transp(0), None, lambda: transp(1), None, lambda: transp(2), None, None]
                return pre, post, (lambda: transp(3))

            def attention_head(g, h):
                tok0, J = geo(g)
                qbs, maskT = qbs2[g % 2], maskT2[g % 2]
                c = h // 2
                pb_ = (h % 2) * 64
                ppv = pPV[cntr["pv"] % 2]
                cntr["pv"] += 1
                na_tot = 4 * J + 4

                def qk_stage(a):
                    u = a - 4 * J
                    q0 = max(0, u) * 128
                    nq = 512 - q0
                    qki = cntr["qk"]
                    pq = next_bank()
                    pe_ = pex[qki % 4]
                    pt_ = ptm[qki % 4]
                    cntr["qk"] += 1
                    near = [(tq, a - 4 * J - tq) for tq in range(q0 // 128, 4) if (a - 4 * J - tq) in (-1, 0)]
                    mm(pq[:, 0:nq], kbT[pb_:pb_ + 64, c, a * 128:(a + 1) * 128], qbs[pb_:pb_ + 64, c, q0:512],
                       True, len(near) == 0, [kbTJ[a // 4], qbs], [pq])
                    for ni, (tq, v) in enumerate(near):
                        mm(pq[:, tq * 128 - q0:(tq + 1) * 128 - q0], antib[:], hkb[:, v + 1, h, :],
                           False, ni == len(near) - 1, [antib, hkb], [pq])
                    act(pe_[:, 0:nq], pq[:, 0:nq], AF.Exp, [pq, rb15B], [pe_], bias=rb15B[:, h:h + 1])
                    tt("pool", pt_[:, 0:nq], pe_[:, 0:nq], maskT[:, a, q0:512], ALU.mult, [pe_, maskT], [pt_])
                    return (a, q0, pt_)

                def pv_stage(st_):
                    a, q0, pt_ = st_
                    for tq in range(q0 // 128, 4):
                        fw.op("pe", [pt_, vbsJ[a // 4]], [ppv], lambda: nc.tensor.matmul(
                            ppv[:, tq * 65:(tq + 1) * 65], lhsT=pt_[:, tq * 128 - q0:(tq + 1) * 128 - q0], rhs=vbs[:, a, h * 65:(h + 1) * 65],
                            start=(a == 0 and tq == 0), stop=(a == 4 * J + tq), skip_group_check=True))
                pend = []
                for a in range(na_tot):
                    pend.append(qk_stage(a))
                    if len(pend) > 2:
                        pv_stage(pend.pop(0))
                while pend:
                    pv_stage(pend.pop(0))
                act(rec[:], ppv[:, 0:260].rearrange("p (t c) -> p t c", c=65)[:, :, 64], AF.Ln, [ppv], [rec])
                act(rec[:], rec[:], AF.Exp, [rec], [rec], scale=-1.0)
                for tq in range(4):
                    fw.op("act", [ppv, rec], [ybq], lambda: nc.scalar.mul(
                        out=ybq[:, tq, h * 64:(h + 1) * 64], in_=ppv[:, tq * 65:tq * 65 + 64], mul=rec[:, tq:tq + 1]))

            def finish(g):
                tok0, J = geo(g)
                for tq in range(4):
                    for c in range(4):
                        tr(pT[:, c, :], ybq[:, tq, c * 128:(c + 1) * 128], identb[:], [ybq, identb], [pT])
                    cp("act", ybT[:, :, tq * 128:(tq + 1) * 128], pT[:, 0:4, :], [pT], [ybT])
                for c in range(4):
                    fw.dma("sp", ybT_d.t.ap()[c * 128:(c + 1) * 128, tok0:tok0 + 512], ybT[:, c, :], ybT_d, ybT)

            def run_masks(g):
                pre, post, tail = mask_chunks(g)
                for h in range(8):
                    if pre[h] is not None:
                        pre[h]()
                    if post[h] is not None:
                        post[h]()
                tail()

            loads(0)
            run_masks(0)
            for g in range(NST):
                pre, post, tail = None, None, None
                defer = False
                if g + 1 < NST:
                    if geo(g + 1)[1] == 0:
                        defer = True
                    else:
                        loads(g + 1)
                        pre, post, tail = mask_chunks(g + 1)
                for h in range(8):
                    if pre is not None and pre[h] is not None:
                        pre[h]()
                    attention_head(g, h)
                    if post is not None and post[h] is not None:
                        post[h]()
                finish(g)
                if tail is not None:
                    tail()
                if defer:
                    loads(g + 1)
                    run_masks(g + 1)
            fw.barrier()

        with ExitStack() as st:
            wbra = fw.sb(st, "wbra", [128, 4, D], BF16)
            woutb = fw.sb(st, "woutb", [128, 8, D], BF16)
            fw.dma("pool", wbra[:], w_br_att.t.ap().rearrange("o (kc p) n -> p (o kc) n", p=128), wbra, w_br_att)
            fw.dma("pool", woutb[:], w_out.t.ap().rearrange("o (kc p) n -> p (o kc) n", p=128), woutb, w_out)
            ybl = [fw.sb(st, "ybl%d" % i, [128, 4, 512], BF16) for i in range(2)]
            epl = [fw.sb(st, "epl%d" % i, [128, 512], F32) for i in range(3)]
            mal = [fw.sb(st, "mal%d" % i, [128, 512], F32) for i in range(3)]
            t1f = [fw.sb(st, "t1f%d" % i, [128, 512], F32) for i in range(2)]
            mixT2 = [fw.sb(st, "mixT%d" % i, [128, 8, 512], BF16) for i in range(2)]
            xr = [fw.sb(st, "xr%d" % i, [128, D], F32) for i in range(3)]
            pQ2 = [fw.ps(st, "pQ2%d" % i, [128, 512], F32) for i in range(3)]
            pS2 = [fw.ps(st, "pS2%d" % i, [128, 512], F32) for i in range(4)]
            k3 = 0
            k4 = 0
            kx = 0
            for g in range(NST):
                tok0 = g * 512
                yb_ = ybl[g % 2]
                mixT = mixT2[g % 2]
                for c in range(4):
                    fw.dma("sp", yb_[:, c, :], ybT_d.t.ap()[c * 128:(c + 1) * 128, tok0:tok0 + 512], yb_, ybT_d)
                for oc in range(8):
                    pq = pQ2[k3 % 3]
                    ep_ = epl[k3 % 3]
                    ma_ = mal[k3 % 3]
                    tf_ = t1f[k3 % 2]
                    k3 += 1
                    fw.dma("sp", ep_[:], epb_d.t.ap()[oc * 128:(oc + 1) * 128, tok0:tok0 + 512], ep_, epb_d)
                    fw.dma("sp", ma_[:], mixA_d.t.ap()[oc * 128:(oc + 1) * 128, tok0:tok0 + 512], ma_, mixA_d)
                    for kc in range(4):
                        mm(pq[:], wbra[:, kc, oc * 128:(oc + 1) * 128], yb_[:, kc, :], kc == 0, kc == 3, [wbra, yb_], [pq])
                    tt("dve", tf_[:], pq[:], ep_[:], ALU.mult, [pq, ep_], [tf_])
                    tt("pool", mixT[:, oc, :], tf_[:], ma_[:], ALU.add, [tf_, ma_], [mixT])
                for tq in range(4):
                    r0 = tok0 + tq * 128
                    xi = xr[kx % 3]
                    kx += 1
                    fw.dma("sp", xi[:], x.t.ap()[r0:r0 + 128, :], xi, x)
                    for hf in range(2):
                        ps_ = pS2[k4 % 4]
                        k4 += 1
                        for kc in range(8):
                            mm(ps_[:], mixT[:, kc, tq * 128:(tq + 1) * 128], woutb[:, kc, hf * 512:(hf + 1) * 512],
                               kc == 0, kc == 7, [mixT, woutb], [ps_])
                        tt("dve", xi[:, hf * 512:(hf + 1) * 512], ps_[:], xi[:, hf * 512:(hf + 1) * 512], ALU.add, [ps_, xi], [xi])
                    fw.dma("sp", out.t.ap()[r0:r0 + 128, :], xi[:], out, xi)
            fw.barrier()

        if dbg == "B":
            return nc, fw, out

        with ExitStack() as st:
            NH = TB // 512
            NTB = TB // 128
            wdb = fw.sb(st, "wdb", [128, NEXP, D], BF16)
            for e0 in range(0, NEXP, 4):
                fw.dma("pool", wdb[:, e0:e0 + 4, :], w_down.t.ap().rearrange("o e k n -> k (o e) n")[:, e0:e0 + 4, :], wdb, w_down)
            gffB = fw.sb(st, "gffB", [128, D], F32)
            fw.dma("sp", gffB[:], AP(g_ffn, 0, [[0, 128], [1, D]]), gffB, g_ffn)
            wr = fw.sb(st, "wr", [128, 8, 36], F32)
            fw.dma("sp", wr[:, :, 0:4], w_rg.t.ap().rearrange("o (kc p) n -> p (o kc) n", p=128), wr, w_rg)
            fw.dma("sp", wr[:, :, 4:36], w_re.t.ap().rearrange("o (kc p) n -> p (o kc) n", p=128), wr, w_re)
            br = fw.sb(st, "br", [1, 36], F32)
            fw.dma("sp", br[:, 0:4], b_rg.t.ap(), br, b_rg)
            fw.dma("sp", br[:, 4:36], b_re.t.ap(), br, b_re)
            selA = fw.sb(st, "selA", [64, NEXP, 128], BF16)
            ms("pool", selA[:], 0.0, [selA])
            for base in (0, -32):
                fw.op("pool", [selA], [selA], lambda: nc.gpsimd.affine_select(
                    out=selA[:], in_=selA[:], pattern=[[-1, NEXP], [0, 128]], compare_op=ALU.not_equal,
                    fill=1.0, base=base, channel_multiplier=1))
            xt = [fw.sb(st, "xtc%d" % i, [128, D], F32) for i in range(2)]
            junk = fw.sb(st, "junkc", [128, D], BF16)
            st1 = [fw.sb(st, "st1c%d" % i, [128, 4], F32) for i in range(2)]
            xnf = [fw.sb(st, "xnf%d" % i, [128, D], F32) for i in range(1)]
            xfT = fw.sb(st, "xfT", [128, 8, 128], F32)
            xn2T = fw.sb(st, "xn2T", [128, 8, TB], BF16)
            lg = fw.sb(st, "lg", [128, 36], F32)
            rt = fw.sb(st, "rt", [128, 16], F32)
            ohg = fw.sb(st, "ohg", [128, 4], F32)
            esel = fw.sb(st, "esel", [128, 8], F32)
            top8 = fw.sb(st, "top8", [128, 8], F32)
            oh1 = fw.sb(st, "oh1", [128, 8], F32)
            oh2 = fw.sb(st, "oh2", [128, 8], F32)
            within = fw.sb(st, "within", [128, 8], F32)
            comb = fw.sb(st, "comb", [128, 32], F32)
            c2b = fw.sb(st, "c2b", [128, 32], BF16)
            c2f = fw.sb(st, "c2f", [128, 64], F32)
            cT2 = fw.sb(st, "cT2", [64, TB], BF16)
            wg = [fw.sb(st, "wg%d" % i, [128, 8, 128], BF16) for i in range(3)]
            wu = [fw.sb(st, "wu%d" % i, [128, 8, 128], BF16) for i in range(3)]
            eg = [fw.sb(st, "eg%d" % i, [128, 512], F32) for i in range(2)]
            t1 = [fw.sb(st, "t1_%d" % i, [128, 512], F32) for i in range(2)]
            hT = fw.sb(st, "hT", [128, NEXP, TB], BF16)
            x2l = [fw.sb(st, "x2l%d" % i, [128, D], F32) for i in range(1)]
            pTf = fw.ps(st, "pTf", [128, 4, 128], F32)
            pR = fw.ps(st, "pR", [128, 512], F32)
            pG = [fw.ps(st, "pG%d" % i, [128, 512], F32) for i in range(2)]
            pU2 = [fw.ps(st, "pU2%d" % i, [128, 512], F32) for i in range(2)]
            pBC = fw.ps(st, "pBC", [128, 512], F32)
            pD = fw.ps(st, "pD", [128, 512], F32)
            wgv = w_gate.t.ap().rearrange("o e (kc p) n -> (o e) p kc n", p=128)
            wuv = w_up.t.ap().rearrange("o e (kc p) n -> (o e) p kc n", p=128)
            if dbg == "C0":
                fw.barrier()
                return nc, fw, out
            wi_ = 0
            for blk in range(NTOK // TB):
                b0 = blk * TB
                for t in range(NTB):
                    xi = xt[t % 2]
                    si = st1[t % 2]
                    xf = xnf[0]
                    r0 = b0 + t * 128
                    cs = slice(t * 128, (t + 1) * 128)
                    fw.dma("sp", xi[:], out.t.ap()[r0:r0 + 128, :], xi, out)
                    act(junk[:], xi[:], AF.Square, [xi], [junk, si], accum_out=si[:, 0:1])
                    rstd_from(si[:, 0:1], si[:, 1:2], 1.0 / D, epsb, [si], [si])
                    stt("dve", xf[:], xi[:], si[:, 1:2], gffB[:], ALU.mult, ALU.mult, [xi, si, gffB], [xf])
                    for half in range(2):
                        for k4 in range(4):
                            kc = half * 4 + k4
                            tr(pTf[:, k4, :], xf[:, kc * 128:(kc + 1) * 128], identf[:], [xf, identf], [pTf])
                        cp("act", xfT[:, half * 4:half * 4 + 4, :], pTf[:], [pTf], [xfT])
                        cp("dve", xn2T[:, half * 4:half * 4 + 4, cs], pTf[:], [pTf], [xn2T])
                    for kc in range(8):
                        mm(pR[:, 0:36], xfT[:, kc, :], wr[:, kc, :], kc == 0, False, [xfT, wr], [pR])
                    mm(pR[:, 0:36], onesf[0:1, :], br[0:1, :], False, True, [onesf, br], [pR])
                    cp("act", lg[:], pR[:, 0:36], [pR], [lg])
                    fw.op("dve", [lg], [rt], lambda: nc.vector.tensor_reduce(out=rt[:, 0:1], in_=lg[:, 0:4], axis=AX.X, op=ALU.max))
                    ts("dve", rt[:, 1:2], rt[:, 0:1], -1.0, None, ALU.mult, None, [rt], [rt])
                    act(ohg[:], lg[:, 0:4], AF.Exp, [lg, rt], [ohg, rt], bias=rt[:, 1:2], accum_out=rt[:, 2:3])
                    rcp(rt[:, 3:4], rt[:, 2:3], [rt], [rt])
                    ts("dve", ohg[:], lg[:, 0:4], rt[:, 0:1], None, ALU.is_ge, None, [lg, rt], [ohg])
                    ts("dve", esel[:], lg[:, 4:12], ohg[:, 0:1], None, ALU.mult, None, [lg, ohg], [esel])
                    for gi in range(1, 4):
                        stt("dve", esel[:], lg[:, 4 + 8 * gi:12 + 8 * gi], ohg[:, gi:gi + 1], esel[:], ALU.mult, ALU.add,
                            [lg, ohg, esel], [esel])
                    fw.op("dve", [esel], [top8], lambda: nc.vector.max(out=top8[:], in_=esel[:]))
                    ts("dve", oh1[:], esel[:], top8[:, 0:1], None, ALU.is_equal, None, [esel, top8], [oh1])
                    ts("dve", oh2[:], esel[:], top8[:, 1:2], None, ALU.is_equal, None, [esel, top8], [oh2])
                    tt("dve", rt[:, 4:5], top8[:, 1:2], top8[:, 0:1], ALU.subtract, [top8], [rt])
                    act(rt[:, 5:6], rt[:, 4:5], AF.Exp, [rt], [rt])
                    ts("dve", rt[:, 5:6], rt[:, 5:6], 1.0, None, ALU.add, None, [rt], [rt])
                    rcp(rt[:, 6:7], rt[:, 5:6], [rt], [rt])
                    tt("dve", rt[:, 7:8], rt[:, 6:7], rt[:, 3:4], ALU.mult, [rt], [rt])
                    tt("dve", rt[:, 8:9], rt[:, 3:4], rt[:, 7:8], ALU.subtract, [rt], [rt])
                    ts("dve", within[:], oh1[:], rt[:, 7:8], None, ALU.mult, None, [oh1, rt], [within])
                    stt("dve", within[:], oh2[:], rt[:, 8:9], within[:], ALU.mult, ALU.add, [oh2, rt, within], [within])
                    for gi in range(4):
                        ts("dve", comb[:, gi * 8:(gi + 1) * 8], within[:], ohg[:, gi:gi + 1], None, ALU.mult, None,
                           [within, ohg], [comb])
                    cp("dve", c2b[:], comb[:], [comb], [c2b])
                    cp("dve", c2f[:, 0:32], c2b[:], [c2b], [c2f])
                    tt("dve", c2f[:, 32:64], comb[:], c2f[:, 0:32], ALU.subtract, [comb, c2f], [c2f])
                    tr(pTf[0:64, 0, :], c2f[:, :], identf[:], [c2f, identf], [pTf])
                    cp("act", cT2[:, cs], pTf[0:64, 0, :], [pTf], [cT2])
                if dbg == "C1":
                    fw.barrier()
                    return nc, fw, out
                for e in range(NEXP):
                    wg_ = wg[wi_ % 3]
                    wu_ = wu[wi_ % 3]
                    wi_ += 1
                    fw.dma("pool", wg_[:], wgv[e], wg_, w_gate)
                    fw.dma("pool", wu_[:], wuv[e], wu_, w_up)
                    for half in range(NH):
                        hs = slice(half * 512, (half + 1) * 512)
                        pg_ = pG[(e * NH + half) % 2]
                        pu_ = pU2[(e * NH + half) % 2]
                        eg_ = eg[(e * NH + half) % 2]
                        t1_ = t1[(e * NH + half) % 2]
                        for kc in range(8):
                            mm(pg_[:], wg_[:, kc, :], xn2T[:, kc, hs], kc == 0, kc == 7, [wg_, xn2T], [pg_])
                        for kc in range(8):
                            mm(pu_[:], wu_[:, kc, :], xn2T[:, kc, hs], kc == 0, kc == 7, [wu_, xn2T], [pu_])
                        pbc_ = pBC if (e * NH + half) % 2 == 0 else pD
                        mm(pbc_[:], selA[:, e, :], cT2[:, hs], True, True, [selA, cT2], [pbc_])
                        act(eg_[:], pg_[:], AF.Silu, [pg_], [eg_])
                        tt("dve", t1_[:], eg_[:], pu_[:], ALU.mult, [eg_, pu_], [t1_])
                        tt("dve", hT[:, e, hs], t1_[:], pbc_[:], ALU.mult, [t1_, pbc_], [hT])
                if dbg == "C2":
                    fw.barrier()
                    return nc, fw, out
                for t in range(NTB):
                    r0 = b0 + t * 128
                    xi = x2l[0]
                    fw.dma("sp", xi[:], out.t.ap()[r0:r0 + 128, :], xi, out)
                    pacc = (pD, pBC)
                    for e in range(NEXP):
                        for hf in range(2):
                            mm(pacc[hf][:], hT[:, e, t * 128:(t + 1) * 128], wdb[:, e, hf * 512:(hf + 1) * 512], e == 0, e == NEXP - 1,
                               [hT, wdb], [pacc[hf]])
                    for hf in range(2):
                        tt("dve", xi[:, hf * 512:(hf + 1) * 512], pacc[hf][:], xi[:, hf * 512:(hf + 1) * 512], ALU.add, [pacc[hf], xi], [xi])
                    fw.dma("sp", out.t.ap()[r0:r0 + 128, :], xi[:], out, xi)
            fw.barrier()
    return nc, fw, out


_E1H = None


def _inputs_for_core(inputs, c, NSEQ, T):
    global _E1H
    if _E1H is None:
        _E1H = _bias_onehot()
    m = {}
    xs = np.ascontiguousarray(inputs["x"][c * NSEQ:(c + 1) * NSEQ]).reshape(NSEQ * T, D)
    m["x"] = xs
    for k, v in inputs.items():
        if k == "x":
            continue
        m[k] = np.ascontiguousarray(np.asarray(v, dtype=np.float32))
    m["e1h"] = _E1H
    return m


def kernel(**inputs):
    x = np.asarray(inputs["x"])
    B, T, _ = x.shape
    NSEQ = B // 8
    _, fw0, _ = build(NSEQ, T)
    nc, fw, out = build(NSEQ, T, needed=fw0.used)
    in_maps = [_inputs_for_core(inputs, c, NSEQ, T) for c in range(8)]
    res = run_bass_kernel_spmd(nc, in_maps, core_ids=list(range(8)))
    outs = [np.asarray(res.results[c]["out"]).reshape(NSEQ, T, D) for c in range(8)]
    return np.concatenate(outs, axis=0).astype(np.float32)
```
